# Optimizing a Trainium2 kernel written in Bass

```python
import jax, jax.numpy as jnp
from jax import lax
import numpy as np

D_MODEL = 2048
BATCH = 4
SEQ = 2048
DEPTH = 1

D_MIX = D_MODEL
NSA_WIDTH = D_MIX // 2
POOL_WIDTH = D_MIX - NSA_WIDTH
HEAD_DIM = 128
N_HEADS = NSA_WIDTH // HEAD_DIM
N_KV = 2
GQA = N_HEADS // N_KV
KV_WIDTH = N_KV * HEAD_DIM
N_GATES = 3 * N_HEADS
ROT_DIM = HEAD_DIM // 4
ROPE_THETA = 500000.0
CMP_LEN = 32
CMP_STRIDE = 16
CMP_HIDDEN = 2 * HEAD_DIM
SEL_LEN = 64
SEL_TOPK = 16
N_LOCAL = 2
WINDOW = 512
WIN_QBLOCK = 128
SEL_QBLOCK = 64
POOL_SIZES = (2, 4, 8, 16)
N_POOL_GROUPS = 4
POOL_GROUP = POOL_WIDTH // N_POOL_GROUPS
N_EXPERT_GROUPS = 4
EXPERTS_PER_GROUP = 8
N_EXPERTS = N_EXPERT_GROUPS * EXPERTS_PER_GROUP
TOP_K_INNER = 2
D_FF_EXPERT = D_MODEL // 4
MOE_BLOCK = 128
EPS = 1e-6
NEG = -1e30
BIG = 1e30
IN_WIDTH = NSA_WIDTH + 6 * KV_WIDTH + N_GATES + POOL_WIDTH

kernel_name = "hymba_nsa_pool_hmoe_layer"


def rmsnorm(x, g):
    xf = x.astype(jnp.float32)
    y = xf * lax.rsqrt(jnp.mean(xf * xf, axis=-1, keepdims=True) + EPS)
    return (y * g.astype(jnp.float32)).astype(x.dtype)


def rope_tables(positions, dtype):
    inv_freq = ROPE_THETA ** (-jnp.arange(0, ROT_DIM, 2, dtype=jnp.float32) / ROT_DIM)
    ang = positions.astype(jnp.float32)[..., None] * inv_freq
    return jnp.cos(ang)[:, None].astype(dtype), jnp.sin(ang)[:, None].astype(dtype)


def apply_partial_rope(x, cos, sin):
    xr, xp = x[..., :ROT_DIM], x[..., ROT_DIM:]
    x1, x2 = xr[..., : ROT_DIM // 2], xr[..., ROT_DIM // 2:]
    rot = jnp.concatenate([x1 * cos - x2 * sin, x2 * cos + x1 * sin], axis=-1)
    return jnp.concatenate([rot, xp], axis=-1)


def gather_blocks(blocks, idx):
    return jax.vmap(jax.vmap(lambda b_, i_: b_[i_]))(blocks, idx)


def cmp_to_sel_weights(n_cmp, n_sel):
    cs = jnp.arange(n_cmp)[:, None] * CMP_STRIDE
    ss = jnp.arange(n_sel)[None, :] * SEL_LEN
    ov = jnp.clip(jnp.minimum(cs + CMP_LEN, ss + SEL_LEN) - jnp.maximum(cs, ss), 0)
    return ov.astype(jnp.float32) / CMP_LEN


def nsa_mixer(q, kc, vc, ks, vs, kw, vw, gates, positions,
              pe_k, w_ck1, w_ck2, pe_v, w_cv1, w_cv2):
    B, S = q.shape[0], q.shape[1]
    scale = HEAD_DIM ** -0.5
    def heads(t, n):
        return t.reshape(B, S, n, HEAD_DIM).transpose(0, 2, 1, 3)
    q = heads(q, N_HEADS)
    kc, vc, ks, vs, kw, vw = [heads(t, N_KV) for t in (kc, vc, ks, vs, kw, vw)]
    cos, sin = rope_tables(positions, q.dtype)
    q_rope = apply_partial_rope(q, cos, sin)
    ks = apply_partial_rope(ks, cos, sin)
    kw = apply_partial_rope(kw, cos, sin)
    qg = q.reshape(B, N_KV, GQA, S, HEAD_DIM)
    qrg = q_rope.reshape(B, N_KV, GQA, S, HEAD_DIM)
    t_idx = jnp.arange(S)

    n_cmp = (S - CMP_LEN) // CMP_STRIDE + 1
    cmp_tok = jnp.arange(n_cmp)[:, None] * CMP_STRIDE + jnp.arange(CMP_LEN)[None, :]
    def compress(t, pe, w1, w2):
        blk = (t[:, :, cmp_tok] + pe).reshape(B, N_KV, n_cmp, CMP_LEN * HEAD_DIM)
        return jax.nn.gelu(blk @ w1) @ w2
    k_cmp = compress(kc, pe_k, w_ck1, w_ck2)
    v_cmp = compress(vc, pe_v, w_cv1, w_cv2)
    cmp_end = jnp.arange(n_cmp) * CMP_STRIDE + CMP_LEN - 1
    cmp_ok = cmp_end[None, :] <= t_idx[:, None]
    s_cmp = jnp.einsum('bgrsd,bgnd->bgrsn', qg, k_cmp).astype(jnp.float32) * scale
    s_cmp = jnp.where(cmp_ok, s_cmp, NEG)
    p_cmp = jnp.where(cmp_ok, jax.nn.softmax(s_cmp, axis=-1), 0.0)
    o_cmp = jnp.einsum('bgrsn,bgnd->bgrsd', p_cmp.astype(v_cmp.dtype), v_cmp)

    n_sel = S // SEL_LEN
    imp = jnp.einsum('bgrsn,nj->bgsj', p_cmp, cmp_to_sel_weights(n_cmp, n_sel))
    blk_j = jnp.arange(n_sel)[None, :]
    cur = (t_idx // SEL_LEN)[:, None]
    sel_ok = blk_j * SEL_LEN <= t_idx[:, None]
    forced = (blk_j == 0) | ((cur - blk_j >= 0) & (cur - blk_j < N_LOCAL))
    imp = jnp.where(sel_ok & forced, BIG, jnp.where(sel_ok, imp, -BIG))
    k_top = min(SEL_TOPK, n_sel)
    top_val, top_idx = lax.top_k(imp, k_top)
    top_ok = top_val >= 0.0
    ks_blocks = ks.reshape(B, N_KV, n_sel, SEL_LEN, HEAD_DIM)
    vs_blocks = vs.reshape(B, N_KV, n_sel, SEL_LEN, HEAD_DIM)
    nq = S // SEL_QBLOCK
    q_chunks = jnp.moveaxis(qrg.reshape(B, N_KV, GQA, nq, SEL_QBLOCK, HEAD_DIM), 3, 0)
    i_chunks = jnp.moveaxis(top_idx.reshape(B, N_KV, nq, SEL_QBLOCK, k_top), 2, 0)
    ok_chunks = jnp.moveaxis(top_ok.reshape(B, N_KV, nq, SEL_QBLOCK, k_top), 2, 0)
    t_chunks = t_idx.reshape(nq, SEL_QBLOCK)
    def sel_block(args):
        qb, ib, okb, tb = args
        flat = ib.reshape(B, N_KV, SEL_QBLOCK * k_top)
        kb = gather_blocks(ks_blocks, flat).reshape(B, N_KV, SEL_QBLOCK, k_top * SEL_LEN, HEAD_DIM)
        vb = gather_blocks(vs_blocks, flat).reshape(B, N_KV, SEL_QBLOCK, k_top * SEL_LEN, HEAD_DIM)
        kpos = (ib[..., None] * SEL_LEN + jnp.arange(SEL_LEN)).reshape(B, N_KV, SEL_QBLOCK, k_top * SEL_LEN)
        ok = jnp.repeat(okb, SEL_LEN, axis=-1) & (kpos <= tb[:, None])
        s = jnp.einsum('bgrqd,bgqld->bgrql', qb, kb).astype(jnp.float32) * scale
        s = jnp.where(ok[:, :, None], s, NEG)
        p = jax.nn.softmax(s, axis=-1).astype(vb.dtype)
        return jnp.einsum('bgrql,bgqld->bgrqd', p, vb)
    o_sel = lax.map(sel_block, (q_chunks, i_chunks, ok_chunks, t_chunks))
    o_sel = jnp.moveaxis(o_sel, 0, 3).reshape(B, N_KV, GQA, S, HEAD_DIM)

    nwb = S // WIN_QBLOCK
    kw_pad = jnp.pad(kw, ((0, 0), (0, 0), (WINDOW, 0), (0, 0)))
    vw_pad = jnp.pad(vw, ((0, 0), (0, 0), (WINDOW, 0), (0, 0)))
    win_idx = jnp.arange(nwb)[:, None] * WIN_QBLOCK + jnp.arange(WIN_QBLOCK + WINDOW)[None, :]
    kwb = kw_pad[:, :, win_idx]
    vwb = vw_pad[:, :, win_idx]
    kpos = (win_idx - WINDOW)[:, None, :]
    qpos = t_idx.reshape(nwb, WIN_QBLOCK)[:, :, None]
    w_ok = (kpos <= qpos) & (kpos > qpos - WINDOW) & (kpos >= 0)
    qw = qrg.reshape(B, N_KV, GQA, nwb, WIN_QBLOCK, HEAD_DIM)
    s_w = jnp.einsum('bgrnqd,bgnkd->bgrnqk', qw, kwb).astype(jnp.float32) * scale
    s_w = jnp.where(w_ok, s_w, NEG)
    p_w = jax.nn.softmax(s_w, axis=-1).astype(vwb.dtype)
    o_win = jnp.einsum('bgrnqk,bgnkd->bgrnqd', p_w, vwb).reshape(B, N_KV, GQA, S, HEAD_DIM)

    g = jax.nn.sigmoid(gates.reshape(B, S, N_HEADS, 3).transpose(0, 2, 1, 3))
    g = g.reshape(B, N_KV, GQA, S, 3)
    o = g[..., 0:1] * o_cmp + g[..., 1:2] * o_sel + g[..., 2:3] * o_win
    return o.reshape(B, N_HEADS, S, HEAD_DIM).transpose(0, 2, 1, 3).reshape(B, S, NSA_WIDTH)


def pool_mixer(u, w_pool, b_pool, pool_scale):
    B, S, C = u.shape
    uf = u.astype(jnp.float32)
    c0 = jnp.pad(jnp.cumsum(uf, axis=1), ((0, 0), (1, 0), (0, 0)))
    t1 = jnp.arange(1, S + 1, dtype=jnp.float32)[:, None]
    outs = []
    for gi, w in enumerate(POOL_SIZES):
        sl = slice(gi * POOL_GROUP, (gi + 1) * POOL_GROUP)
        cg = c0[:, :, sl]
        prev = jnp.pad(cg[:, : S + 1 - w], ((0, 0), (w - 1, 0), (0, 0)))
        mean = (cg[:, 1:] - prev) / jnp.minimum(t1, float(w))
        outs.append(mean - uf[:, :, sl])
    d = jnp.stack(outs, axis=2).astype(u.dtype)
    y = jnp.einsum('bsgc,gcd->bsgd', d, w_pool).reshape(B, S, C) + b_pool
    return y * pool_scale


def hier_moe(h, w_rg, b_rg, w_re, b_re, w_gate, w_up, w_down):
    B, S, D = h.shape
    N = B * S
    hf = h.reshape(N, D)
    rows = jnp.arange(N)
    lg = (hf @ w_rg).astype(jnp.float32) + b_rg.astype(jnp.float32)
    pg = jax.nn.softmax(lg, axis=-1)
    g_sel = jnp.argmax(lg, axis=-1)
    p_g = pg[rows, g_sel]
    le = jnp.einsum('nd,gde->nge', hf, w_re).astype(jnp.float32) + b_re.astype(jnp.float32)
    pe = jax.nn.softmax(le[rows, g_sel], axis=-1)
    top_p, top_e = lax.top_k(pe, TOP_K_INNER)
    gate = p_g[:, None] * top_p / jnp.sum(top_p, axis=-1, keepdims=True)
    expert = g_sel[:, None] * EXPERTS_PER_GROUP + top_e
    na = N * TOP_K_INNER
    e_flat = expert.reshape(na)
    tok_flat = jnp.repeat(rows, TOP_K_INNER)
    g_flat = gate.reshape(na)
    order = jnp.argsort(e_flat)
    e_s, tok_s, g_s = e_flat[order], tok_flat[order], g_flat[order]
    counts = jnp.bincount(e_flat, length=N_EXPERTS)
    starts = jnp.cumsum(counts) - counts
    pcounts = (counts + MOE_BLOCK - 1) // MOE_BLOCK * MOE_BLOCK
    pends = jnp.cumsum(pcounts)
    pstarts = pends - pcounts
    dest = pstarts[e_s] + jnp.arange(na) - starts[e_s]
    n_blocks = (na + N_EXPERTS * (MOE_BLOCK - 1) + MOE_BLOCK - 1) // MOE_BLOCK
    cap = n_blocks * MOE_BLOCK
    buf_tok = jnp.full((cap,), N, dtype=jnp.int32).at[dest].set(tok_s.astype(jnp.int32))
    buf_gate = jnp.zeros((cap,), jnp.float32).at[dest].set(g_s)
    blk_expert = jnp.minimum(jnp.searchsorted(pends, jnp.arange(n_blocks) * MOE_BLOCK, side='right'),
                             N_EXPERTS - 1)
    h_pad = jnp.concatenate([hf, jnp.zeros((1, D), hf.dtype)], axis=0)
    def run_block(args):
        tk, gt, e = args
        xb = h_pad[tk]
        y = (jax.nn.silu(xb @ w_gate[e]) * (xb @ w_up[e])) @ w_down[e]
        return y * gt[:, None].astype(y.dtype)
    ys = lax.map(run_block, (buf_tok.reshape(n_blocks, MOE_BLOCK),
                             buf_gate.reshape(n_blocks, MOE_BLOCK), blk_expert))
    out = jnp.zeros((N + 1, D), h.dtype).at[buf_tok].add(ys.reshape(cap, D))[:N]
    return out.reshape(B, S, D)


def setup_inputs(seed: int = 0) -> dict:
    key = jax.random.key(seed)
    ks = jax.random.split(key, 24)
    f32 = jnp.float32
    def nrm(k, shape, fan_in):
        return jax.random.normal(k, shape, f32) * (fan_in ** -0.5)
    def gain(k, shape):
        return 1.0 + 0.1 * jax.random.normal(k, shape, f32)
    L = DEPTH
    return {
        "x": jax.random.normal(ks[0], (BATCH, SEQ, D_MODEL), f32),
        "positions": (jnp.arange(SEQ, dtype=jnp.int32)[None, :]
                      + jax.random.randint(ks[1], (BATCH, 1), 0, 1024, dtype=jnp.int32)),
        "ln_mix": gain(ks[2], (L, D_MODEL)),
        "w_in": nrm(ks[3], (L, D_MODEL, IN_WIDTH), D_MODEL),
        "pe_cmp_k": 0.1 * jax.random.normal(ks[4], (L, CMP_LEN, HEAD_DIM), f32),
        "w_cmp_k1": nrm(ks[5], (L, CMP_LEN * HEAD_DIM, CMP_HIDDEN), CMP_LEN * HEAD_DIM),
        "w_cmp_k2": nrm(ks[6], (L, CMP_HIDDEN, HEAD_DIM), CMP_HIDDEN),
        "pe_cmp_v": 0.1 * jax.random.normal(ks[7], (L, CMP_LEN, HEAD_DIM), f32),
        "w_cmp_v1": nrm(ks[8], (L, CMP_LEN * HEAD_DIM, CMP_HIDDEN), CMP_LEN * HEAD_DIM),
        "w_cmp_v2": nrm(ks[9], (L, CMP_HIDDEN, HEAD_DIM), CMP_HIDDEN),
        "w_pool": nrm(ks[10], (L, N_POOL_GROUPS, POOL_GROUP, POOL_GROUP), POOL_GROUP),
        "b_pool": 0.01 * jax.random.normal(ks[11], (L, POOL_WIDTH), f32),
        "pool_scale": gain(ks[12], (L, POOL_WIDTH)),
        "gn_nsa": gain(ks[13], (L, NSA_WIDTH)),
        "gn_pool": gain(ks[14], (L, POOL_WIDTH)),
        "w_out": nrm(ks[15], (L, D_MIX, D_MODEL), D_MIX),
        "ln_moe": gain(ks[16], (L, D_MODEL)),
        "w_router_group": nrm(ks[17], (L, D_MODEL, N_EXPERT_GROUPS), D_MODEL),
        "b_router_group": 0.01 * jax.random.normal(ks[18], (L, N_EXPERT_GROUPS), f32),
        "w_router_expert": nrm(ks[19], (L, N_EXPERT_GROUPS, D_MODEL, EXPERTS_PER_GROUP), D_MODEL),
        "b_router_expert": 0.01 * jax.random.normal(ks[20], (L, N_EXPERT_GROUPS, EXPERTS_PER_GROUP), f32),
        "w_gate": nrm(ks[21], (L, N_EXPERTS, D_MODEL, D_FF_EXPERT), D_MODEL),
        "w_up": nrm(ks[22], (L, N_EXPERTS, D_MODEL, D_FF_EXPERT), D_MODEL),
        "w_down": nrm(ks[23], (L, N_EXPERTS, D_FF_EXPERT, D_MODEL), D_FF_EXPERT),
        "ln_final": gain(jax.random.fold_in(key, 99), (D_MODEL,)),
    }


def reference(x, positions, ln_mix, w_in, pe_cmp_k, w_cmp_k1, w_cmp_k2, pe_cmp_v, w_cmp_v1,
              w_cmp_v2, w_pool, b_pool, pool_scale, gn_nsa, gn_pool, w_out, ln_moe,
              w_router_group, b_router_group, w_router_expert, b_router_expert,
              w_gate, w_up, w_down, ln_final):
    sizes = (NSA_WIDTH, KV_WIDTH, KV_WIDTH, KV_WIDTH, KV_WIDTH, KV_WIDTH, KV_WIDTH, N_GATES, POOL_WIDTH)
    cuts, acc = [], 0
    for s_ in sizes[:-1]:
        acc += s_
        cuts.append(acc)
    for l in range(DEPTH):
        h = rmsnorm(x, ln_mix[l])
        proj = h @ w_in[l]
        q, kc, vc, ksel, vsel, kwin, vwin, gates, u = jnp.split(proj, cuts, axis=-1)
        o_nsa = nsa_mixer(q, kc, vc, ksel, vsel, kwin, vwin, gates, positions,
                          pe_cmp_k[l], w_cmp_k1[l], w_cmp_k2[l],
                          pe_cmp_v[l], w_cmp_v1[l], w_cmp_v2[l])
        o_pool = pool_mixer(u, w_pool[l], b_pool[l], pool_scale[l])
        mix = jnp.concatenate([rmsnorm(o_nsa, gn_nsa[l]), rmsnorm(o_pool, gn_pool[l])], axis=-1)
        x = x + mix @ w_out[l]
        h2 = rmsnorm(x, ln_moe[l])
        x = x + hier_moe(h2, w_router_group[l], b_router_group[l], w_router_expert[l],
                         b_router_expert[l], w_gate[l], w_up[l], w_down[l])
    return rmsnorm(x, ln_final)
```

```python
import numpy as np
import ml_dtypes
import concourse.bass as bass
import concourse.mybir as mybir
from concourse.bass_utils import run_bass_kernel_spmd

F32 = mybir.dt.float32
BF16 = mybir.dt.bfloat16
I32 = mybir.dt.int32
AF = mybir.ActivationFunctionType
ALU = mybir.AluOpType
AX = mybir.AxisListType

D = 2048
S = 2048
NB = 4
HD = 128
NH = 8
EPS = 1e-6
SCALE = HD ** -0.5
NEGB = -30000.0
BIG = 1e30
NEXP = 32
DFF = 512
POOL_SIZES = (2, 4, 8, 16)
DT_SIZE = {F32: 4, BF16: 2, I32: 4}


class Tok:
    __slots__ = ("w", "r")

    def __init__(self, fence=None):
        self.w = None
        self.r = dict(fence) if fence else {}


class KB:
    def __init__(self, nc):
        self.nc = nc
        self.eng = dict(pe=nc.tensor, dve=nc.vector, act=nc.scalar, pool=nc.gpsimd, sp=nc.sync)
        self.sems = {}
        self.cnt = {}
        for k in self.eng:
            self.sems[k] = nc.alloc_semaphore("p_" + k)
            self.cnt[k] = 0
        self.waited = {k: {} for k in self.eng}
        self.dring = {}
        self.dpos = {}
        for q, n in (("sp", 12), ("pool", 12), ("act", 4)):
            ks = []
            for i in range(n):
                key = "d_%s%d" % (q, i)
                self.sems[key] = nc.alloc_semaphore(key)
                self.cnt[key] = 0
                ks.append(key)
            self.dring[q] = ks
            self.dpos[q] = 0
        self.base = (nc.sbuf_base + 63) // 64 * 64
        self.top = nc.sbuf_top
        self.live = []
        self.fences = []
        self.nalloc = 0
        self.ents = {}

    def alloc_tok(self, name, shape, dtype, ntok=1):
        n = 1
        for s in shape[1:]:
            n *= s
        size = (n * DT_SIZE[dtype] + 63) // 64 * 64
        off = self.base
        for (o, s_, _) in sorted(self.live):
            if off + size <= o:
                break
            off = max(off, o + s_)
        if off + size > self.top:
            raise RuntimeError("SBUF overflow allocating %s (%d bytes) live=%s" % (name, size, sorted(self.live)))
        self.nalloc += 1
        ent = (off, size, "%s_%d" % (name, self.nalloc))
        self.live.append(ent)
        t = self.nc.alloc_sbuf_tensor_at(ent[2], list(shape), dtype, offset=off)
        fence = {}
        for (o, s_, deps) in self.fences:
            if o < off + size and off < o + s_:
                for k, v in deps.items():
                    if fence.get(k, 0) < v:
                        fence[k] = v
        toks = [Tok(fence) for _ in range(ntok)]
        self.ents[id(t)] = (ent, t)
        return (t, toks[0]) if ntok == 1 else (t, toks)

    def free(self, t, toks):
        ent, _ = self.ents.pop(id(t))
        self.live.remove(ent)
        deps = {}
        for tk in toks:
            if tk.w:
                k, v = tk.w
                if deps.get(k, 0) < v:
                    deps[k] = v
            for k, v in tk.r.items():
                if deps.get(k, 0) < v:
                    deps[k] = v
        self.fences.append((ent[0], ent[1], deps))

    def _deps(self, reads, writes):
        d = {}
        for b in reads:
            if b.w:
                k, v = b.w
                if d.get(k, 0) < v:
                    d[k] = v
        for b in writes:
            if b.w:
                k, v = b.w
                if d.get(k, 0) < v:
                    d[k] = v
            for k, v in b.r.items():
                if d.get(k, 0) < v:
                    d[k] = v
        return d

    def _wait(self, X, deps):
        w = self.waited[X]
        for key, val in deps.items():
            if val <= 0:
                continue
            if key == X and X == "pe":
                continue
            if w.get(key, 0) >= val:
                continue
            self.eng[X].wait_ge(self.sems[key], val)
            w[key] = val

    def op(self, X, fn, reads=(), writes=(), guard=False):
        self._wait(X, self._deps(reads, writes))
        inst = fn(self.eng[X])
        self.cnt[X] += 1
        inst.then_inc(self.sems[X], 1)
        if guard:
            g = self.gbuf
            inst2 = self.eng[X].copy(out=g[:, 0:1], in_=g[:, 1:2])
            self.cnt[X] += 1
            inst2.then_inc(self.sems[X], 1)
        c = self.cnt[X]
        for b in reads:
            if b.r.get(X, 0) < c:
                b.r[X] = c
        for b in writes:
            b.w = (X, c)
            b.r = {}
        return inst

    def dma(self, Q, out, in_, reads=(), writes=()):
        ring = self.dring[Q]
        i = self.dpos[Q]
        self.dpos[Q] = (i + 1) % len(ring)
        key = ring[i]
        deps = self._deps(reads, writes)
        if self.cnt[key] > 0:
            deps[key] = max(deps.get(key, 0), self.cnt[key])
        self._wait(Q, deps)
        inst = self.eng[Q].dma_start(out=out, in_=in_)
        self.cnt[key] += 16
        inst.then_inc(self.sems[key], 16)
        c = self.cnt[key]
        for b in reads:
            b.r[key] = c
        for b in writes:
            b.w = (key, c)
            b.r = {}
        return inst

    def finish(self):
        deps = {}
        for k, v in self.cnt.items():
            if v > 0:
                deps[k] = v
        self._wait("sp", deps)


def build(stage=99, taps=(), poison=False):
    nc = bass.Bass("TRN2", target_bir_lowering=False)
    kb = KB(nc)
    if poison:
        nel = (kb.top - kb.base) // 4
        parena = nc.alloc_sbuf_tensor_at("poison_arena", [128, nel], F32, offset=kb.base)
        pt_ = Tok()
        kb.op("dve", lambda e: e.memset(parena[:], float("nan")), [], [pt_])
        for X in ("pe", "act", "pool", "sp"):
            kb._wait(X, {"dve": 1})

    def din(name, shape, dt=F32):
        return nc.dram_tensor(name, list(shape), dt, kind="ExternalInput").ap()

    def dout(name, shape, dt=F32):
        return nc.dram_tensor(name, list(shape), dt, kind="ExternalOutput").ap()

    xc = din("xc", [2048, D])
    w_in = din("w_in", [D, 3608])
    posT_d = din("posT", [128, 16], I32)
    invf_d = din("invf", [128, 16])
    valid_d = din("valid", [128, 16])
    identb_d = din("identb", [128, 128], BF16)
    identf_d = din("identf", [128, 128])
    lnmix_d = din("lnmix", [128, D])
    pekT_d = din("pekT", [128, 32])
    pevT_d = din("pevT", [128, 32])
    w1k_d = din("w1k", [4096, 256])
    w1v_d = din("w1v", [4096, 256])
    w2k_d = din("w2k", [256, 128])
    w2v_d = din("w2v", [256, 128])
    cmpbias_d = din("cmpbias", [128, 1024], BF16)
    wcs_d = din("wcs", [128, 32], BF16)
    emat_d = din("emat", [128, 2048], BF16)
    causb_d = din("causb", [128, 4, 512], BF16)
    winb_d = din("winb", [128, 8, 512], BF16)
    amul_d = din("amul", [128, 8, 32])
    aadd_d = din("aadd", [128, 8, 32])
    invcnt_d = din("invcnt", [128, 4, 16])
    wpool_d = din("wpool", [4, 256, 256])
    bpool_d = din("bpool", [128, 1024])
    pscale_d = din("pscale", [128, 1024])
    gnnsa_d = din("gnnsa", [128, 1024])
    gnpool_d = din("gnpool", [128, 1024])
    wout_d = din("wout", [D, D])
    lnmoe_d = din("lnmoe", [128, D])
    wr_d = din("wr", [D, 36])
    br_d = din("br", [128, 36])
    if stage >= 7:
        wg_d = din("wg", [NEXP, D, DFF])
        wu_d = din("wu", [NEXP, D, DFF])
        wd_d = din("wd", [NEXP, DFF, D])
    lnfin_d = din("lnfin", [128, D])
    out_d = dout("out", [1024, D])
    tap_d = {}
    for (nm, shp, tdt) in taps:
        tap_d[nm] = dout("tap_" + nm, shp, tdt)

    def tap(nm, src_ap, toks, dst=None):
        if nm in tap_d:
            kb.dma("sp", out=(tap_d[nm] if dst is None else dst), in_=src_ap, reads=toks)

    gb_, gb_t_ = kb.alloc_tok("gbuf", [128, 2], F32)
    kb.gbuf = gb_
    kb.op("dve", lambda e: e.memset(gb_[:], 0.0), [], [gb_t_])
    kb._wait("act", {"dve": kb.cnt["dve"]})
    psb = [nc.alloc_psum_tensor("ps%d" % i, [128, 512], F32) for i in range(8)]
    pst = [Tok() for _ in range(8)]

    identb, identb_t = kb.alloc_tok("identb", [128, 128], BF16)
    identf, identf_t = kb.alloc_tok("identf", [128, 128], F32)
    kb.dma("sp", out=identb[:], in_=identb_d, writes=[identb_t])
    kb.dma("sp", out=identf[:], in_=identf_d, writes=[identf_t])
    small, small_t = kb.alloc_tok("small", [128, 256], F32)
    valid_sb, valid_t = kb.alloc_tok("valid", [128, 16], F32)
    kb.dma("sp", out=valid_sb[:], in_=valid_d, writes=[valid_t])

    cs4, cs4_t = kb.alloc_tok("cs4", [128, 2, 16, 4, 16], F32)
    rt, rt_t = kb.alloc_tok("ropetmp", [128, 4, 4, 16], F32)

    def rope(ps_ap, H, ti, out_ap, ps_tok, out_tok):
        sin = cs4[:, 0, ti, 0:H, :]
        cos = cs4[:, 1, ti, 0:H, :]
        x1 = ps_ap[:, :, 0:16]
        x2 = ps_ap[:, :, 16:32]
        kb.op("dve", lambda e: e.tensor_tensor(out=rt[:, 0, 0:H, :], in0=x1, in1=cos, op=ALU.mult), [ps_tok, cs4_t], [rt_t])
        kb.op("dve", lambda e: e.tensor_tensor(out=rt[:, 1, 0:H, :], in0=x2, in1=sin, op=ALU.mult), [ps_tok, cs4_t], [rt_t])
        kb.op("dve", lambda e: e.tensor_tensor(out=rt[:, 2, 0:H, :], in0=x2, in1=cos, op=ALU.mult), [ps_tok, cs4_t], [rt_t])
        kb.op("dve", lambda e: e.tensor_tensor(out=rt[:, 3, 0:H, :], in0=x1, in1=sin, op=ALU.mult), [ps_tok, cs4_t], [rt_t])
        kb.op("dve", lambda e: e.tensor_tensor(out=out_ap[:, :, 0:16], in0=rt[:, 0, 0:H, :], in1=rt[:, 1, 0:H, :],
                                               op=ALU.subtract), [rt_t], [out_tok])
        kb.op("dve", lambda e: e.tensor_tensor(out=out_ap[:, :, 16:32], in0=rt[:, 2, 0:H, :], in1=rt[:, 3, 0:H, :],
                                               op=ALU.add), [rt_t], [out_tok])
        kb.op("act", lambda e: e.copy(out=out_ap[:, :, 32:128], in_=ps_ap[:, :, 32:128]), [ps_tok], [out_tok])

    NSLOT = 3
    ring = []
    ring_t = []
    for i in range(NSLOT):
        t, tk = kb.alloc_tok("ring%d" % i, [128, 8192], BF16)
        ring.append(t)
        ring_t.append(tk)
    ring_pos = [0]

    def ring_load(src_ap, shape_str, **kw):
        i = ring_pos[0]
        ring_pos[0] = (i + 1) % NSLOT
        a, b = src_ap.shape[1], src_ap.shape[2]
        view = ring[i][:, 0:a * b].rearrange("p (a b) -> p a b", a=a)
        kb.dma("pool", out=view, in_=src_ap, writes=[ring_t[i]])
        return view, ring_t[i]

    wkv = []
    for cg in range(3):
        v, tk = ring_load(w_in[:, 1024 + cg * 512:1024 + (cg + 1) * 512].rearrange("(kc p) n -> p kc n", p=128), "")
        wkv.append((v, tk))
    junk, junk_t = kb.alloc_tok("junk", [128, D], BF16)
    hb, hb_t = kb.alloc_tok("hb", [128, D], BF16)
    KsT, KsT_t = kb.alloc_tok("KsT", [128, 2, 2048], BF16, ntok=16)
    KwT, KwT_t = kb.alloc_tok("KwT", [128, 2, 2048], BF16, ntok=16)
    Vs, Vs_t = kb.alloc_tok("Vs", [128, 16, 2, 129], BF16, ntok=16)
    Vw, Vw_t = kb.alloc_tok("Vw", [128, 16, 2, 129], BF16, ntok=16)
    tkm, tkm_t = kb.alloc_tok("tkm", [128, 512], BF16)
    tkm2, tkm2_t = kb.alloc_tok("tkm2", [128, 512], BF16)
    hTo, hTo_t = kb.alloc_tok("hTown", [128, 16, 1024], BF16, ntok=8)
    hTh, hTh_t = kb.alloc_tok("hThalo", [128, 16, 16], BF16)
    kvcT, kvcT_t = kb.alloc_tok("kvcT", [128, 4, 2048], BF16)
    lnmix, lnmix_t = kb.alloc_tok("lnmix", [128, D], F32)
    kb.dma("sp", out=lnmix[:], in_=lnmix_d, writes=[lnmix_t])
    xt = []
    xt_t = []
    for i in range(2):
        t, tk = kb.alloc_tok("xt%d" % i, [128, D], F32)
        xt.append(t)
        xt_t.append(tk)
    hTt = []
    hTt_t = []
    for i in range(2):
        t, tk = kb.alloc_tok("hTt%d" % i, [128, 16, 128], BF16)
        hTt.append(t)
        hTt_t.append(tk)
    posi, posi_t = kb.alloc_tok("posi", [128, 16], I32)
    invf, invf_t = kb.alloc_tok("invf", [128, 16], F32)
    kb.dma("sp", out=posi[:], in_=posT_d, writes=[posi_t])
    kb.dma("sp", out=invf[:], in_=invf_d, writes=[invf_t])
    posf, posf_t = kb.alloc_tok("posf", [128, 16], F32)
    ang, ang_t = kb.alloc_tok("ang", [128, 2, 16, 16], F32)
    rtmp, rtmp_t = kb.alloc_tok("rtmp", [128, 2, 16, 16], F32)
    rki, rki_t = kb.alloc_tok("rki", [128, 2, 16, 16], I32)
    kb.op("dve", lambda e: e.tensor_copy(out=posf[:], in_=posi[:]), [posi_t], [posf_t])
    for ti in range(16):
        kb.op("dve", lambda e: e.tensor_scalar(out=ang[:, 0, ti, :], in0=invf[:], scalar1=posf[:, ti:ti + 1],
                                               scalar2=None, op0=ALU.mult), [invf_t, posf_t], [ang_t])
    kb.op("dve", lambda e: e.tensor_scalar(out=ang[:, 1], in0=ang[:, 0], scalar1=float(np.pi / 2), scalar2=None,
                                           op0=ALU.add), [ang_t], [ang_t])
    TWO_PI = float(2 * np.pi)
    kb.op("dve", lambda e: e.tensor_scalar(out=rtmp[:], in0=ang[:], scalar1=1.0 / TWO_PI, scalar2=None,
                                           op0=ALU.mult), [ang_t], [rtmp_t])
    kb.op("dve", lambda e: e.tensor_copy(out=rki[:], in_=rtmp[:]), [rtmp_t], [rki_t])
    kb.op("dve", lambda e: e.tensor_copy(out=rtmp[:], in_=rki[:]), [rki_t], [rtmp_t])
    C1 = 6.28125
    C2 = TWO_PI - C1
    kb.op("dve", lambda e: e.scalar_tensor_tensor(out=ang[:], in0=rtmp[:], scalar=-C1, in1=ang[:],
                                                  op0=ALU.mult, op1=ALU.add), [rtmp_t, ang_t], [ang_t])
    kb.op("dve", lambda e: e.scalar_tensor_tensor(out=ang[:], in0=rtmp[:], scalar=-C2, in1=ang[:],
                                                  op0=ALU.mult, op1=ALU.add), [rtmp_t, ang_t], [ang_t])
    PI = float(np.pi)
    kb.op("dve", lambda e: e.tensor_scalar(out=rtmp[:], in0=ang[:], scalar1=PI, scalar2=-TWO_PI,
                                           op0=ALU.is_gt, op1=ALU.mult), [ang_t], [rtmp_t])
    kb.op("dve", lambda e: e.tensor_tensor(out=ang[:], in0=ang[:], in1=rtmp[:], op=ALU.add), [ang_t, rtmp_t], [ang_t])
    kb.op("dve", lambda e: e.tensor_scalar(out=rtmp[:], in0=ang[:], scalar1=-PI, scalar2=TWO_PI,
                                           op0=ALU.is_lt, op1=ALU.mult), [ang_t], [rtmp_t])
    kb.op("dve", lambda e: e.tensor_tensor(out=ang[:], in0=ang[:], in1=rtmp[:], op=ALU.add), [ang_t, rtmp_t], [ang_t])
    kb.op("dve", lambda e: e.tensor_scalar(out=ang[:], in0=ang[:], scalar1=3.141592, scalar2=-3.141592,
                                           op0=ALU.min, op1=ALU.max), [ang_t], [ang_t])
    kb.op("act", lambda e: e.activation(out=rtmp[:], in_=ang[:], func=AF.Sin), [ang_t], [rtmp_t])
    for hh in range(4):
        kb.op("dve", lambda e: e.tensor_copy(out=cs4[:, :, :, hh, :], in_=rtmp[:]), [rtmp_t], [cs4_t])
    if "cs" in tap_d:
        tap("cs", rtmp[:].rearrange("p a t f -> p (a t f)"), [rtmp_t])

    for _t, _k in ((posi, posi_t), (invf, invf_t), (posf, posf_t), (ang, ang_t), (rtmp, rtmp_t), (rki, rki_t)):
        kb.free(_t, [_k])
    for g in range(2):
        kb.op("dve", lambda e: e.tensor_copy(out=Vs[:, :, g, 128], in_=valid_sb[:]), [valid_t], Vs_t)
        kb.op("dve", lambda e: e.tensor_copy(out=Vw[:, :, g, 128], in_=valid_sb[:]), [valid_t], Vw_t)

    psrot = {}

    def nextps(lo=0, hi=8):
        i = psrot.get((lo, hi), lo)
        psrot[(lo, hi)] = lo + (i + 1 - lo) % (hi - lo)
        return i

    def bfview(bank):
        return psb[bank][:].bitcast(BF16)

    sm1, sm1_t = kb.alloc_tok("sm1", [128, 16, 4], F32, ntok=16)
    hbB, hbB_t = kb.alloc_tok("hbB", [128, D], BF16)
    hbs = [(hb, hb_t), (hbB, hbB_t)]
    tkmB, tkmB_t = kb.alloc_tok("tkmB", [128, 512], BF16)
    tkm2B, tkm2B_t = kb.alloc_tok("tkm2B", [128, 512], BF16)
    tk2s = [(tkm2, tkm2_t), (tkm2B, tkm2B_t)]

    def norm_tile(lnrep, lnrep_t, ti, xbuf, xbuf_t, hbuf, hbuf_t):
        s_, st_ = sm1[:, ti, :], sm1_t[ti]
        kb.op("act", lambda e: e.activation(out=junk[:], in_=xbuf[:], func=AF.Square, accum_out=s_[:, 0:1]),
              [xbuf_t], [junk_t, st_], guard=True)
        kb.op("dve", lambda e: e.tensor_scalar(out=s_[:, 1:2], in0=s_[:, 0:1], scalar1=1.0 / D,
                                               scalar2=EPS, op0=ALU.mult, op1=ALU.add), [st_], [st_])
        kb.op("act", lambda e: e.activation(out=s_[:, 2:3], in_=s_[:, 1:2], func=AF.Sqrt), [st_], [st_])
        kb.op("dve", lambda e: e.reciprocal(out=s_[:, 3:4], in_=s_[:, 2:3]), [st_], [st_])
        kb.op("dve", lambda e: e.scalar_tensor_tensor(out=hbuf[:], in0=xbuf[:], scalar=s_[:, 3:4],
                                                      in1=lnrep[:], op0=ALU.mult, op1=ALU.mult),
              [xbuf_t, st_, lnrep_t], [hbuf_t])

    def transpose16(src, src_t, dst_ap_fn, dst_toks, banks=(6, 7)):
        for half in range(2):
            bk = banks[half]
            pv = bfview(bk)
            for j in range(8):
                kc = half * 8 + j
                kb.op("pe", lambda e: e.transpose(out=pv[:, j * 128:(j + 1) * 128], in_=src[:, kc * 128:(kc + 1) * 128],
                                                  identity=identb[:]), [src_t, identb_t], [pst[bk]])
            kb.op("act", lambda e: e.copy(out=dst_ap_fn(half), in_=pv[:, 0:1024].rearrange("p (a b) -> p a b", a=8)),
                  [pst[bk]], dst_toks)

    def p1_norm(ti):
        hbuf, hbuf_t = hbs[ti % 2]
        norm_tile(lnmix, lnmix_t, ti, xt[ti % 2], xt_t[ti % 2], hbuf, hbuf_t)

    def p1_hinfo(ti):
        if ti >= 8:
            ot = ti - 8
            return hTo_t[ot], (lambda kc: hTo[:, kc, ot * 128:(ot + 1) * 128]), (lambda half: hTo[:, half * 8:(half + 1) * 8, ot * 128:(ot + 1) * 128])
        hcur = hTt[ti % 2]
        return hTt_t[ti % 2], (lambda kc: hcur[:, kc, :]), (lambda half: hcur[:, half * 8:(half + 1) * 8, :])

    def p1_tr(ti):
        hbuf, hbuf_t = hbs[ti % 2]
        hs_t, lhs, dstf = p1_hinfo(ti)
        transpose16(hbuf, hbuf_t, dstf, [hs_t])
        if ti == 7:
            kb.op("dve", lambda e: e.tensor_copy(out=hTh[:], in_=hTt[1][:, :, 112:128]), [hs_t], [hTh_t])

    def p1_mm(ti):
        hs_t, lhs, dstf = p1_hinfo(ti)
        banks = []
        for cg in range(3):
            bk = nextps(0, 6)
            wv, wtk = wkv[cg]
            for kc in range(16):
                kb.op("pe", lambda e: e.matmul(out=psb[bk][:, 0:512], lhsT=lhs(kc), rhs=wv[:, kc, :], start=(kc == 0),
                                               stop=(kc == 15)), [hs_t, wtk], [pst[bk]])
            banks.append(bk)
        return banks

    t2tok = [[Tok(), Tok()], [Tok(), Tok()]]
    tkms = [(tkm, tkm_t), (tkmB, tkmB_t)]

    def p1_evac_a(ti, banks):
        par = ti % 2
        t1, t1_t = tkms[par]
        t2 = tk2s[par][0]
        for cg in range(3):
            bk = banks[cg]
            if cg == 0:
                kb.op("act", lambda e: e.copy(out=t1[:], in_=psb[bk][:, 0:512]), [pst[bk]], [t1_t])
            else:
                V, V_t = (Vs, Vs_t) if cg == 1 else (Vw, Vw_t)
                co = (cg - 1) * 256
                rope(psb[bk][:, 0:256].rearrange("p (h d) -> p h d", h=2), 2, ti,
                     t2[:, co:co + 256].rearrange("p (h d) -> p h d", h=2), pst[bk], t2tok[par][cg - 1])
                kb.op("act", lambda e: e.copy(out=V[:, ti, :, 0:128], in_=psb[bk][:, 256:512].rearrange("p (h d) -> p h d", h=2)),
                      [pst[bk]], [V_t[ti]])

    def p1_evac_b(ti):
        par = ti % 2
        t1, t1_t = tkms[par]
        t2 = tk2s[par][0]
        tb = 7
        pv = bfview(tb)
        for j in range(4):
            kb.op("pe", lambda e: e.transpose(out=pv[:, j * 128:(j + 1) * 128], in_=t1[:, j * 128:(j + 1) * 128],
                                              identity=identb[:]), [t1_t, identb_t], [pst[tb]])
        kb.op("dve", lambda e: e.tensor_copy(out=kvcT[:, :, ti * 128:(ti + 1) * 128],
                                             in_=pv[:, 0:512].rearrange("p (a b) -> p a b", a=4)), [pst[tb]], [kvcT_t])
        tb = 6
        pv = bfview(tb)
        for cg in (1, 2):
            co = (cg - 1) * 256
            for j in range(2):
                kb.op("pe", lambda e: e.transpose(out=pv[:, co + j * 128:co + (j + 1) * 128], in_=t2[:, co + j * 128:co + (j + 1) * 128],
                                                  identity=identb[:]), [t2tok[par][cg - 1], identb_t], [pst[tb]])
        kb.op("dve", lambda e: e.tensor_copy(out=KsT[:, :, ti * 128:(ti + 1) * 128],
                                             in_=pv[:, 0:256].rearrange("p (a b) -> p a b", a=2)), [pst[tb]], [KsT_t[ti]])
        kb.op("dve", lambda e: e.tensor_copy(out=KwT[:, :, ti * 128:(ti + 1) * 128],
                                             in_=pv[:, 256:512].rearrange("p (a b) -> p a b", a=2)), [pst[tb]], [KwT_t[ti]])

    kb.dma("sp", out=xt[0][:], in_=xc[0:128, :], writes=[xt_t[0]])
    kb.dma("sp", out=xt[1][:], in_=xc[128:256, :], writes=[xt_t[1]])
    p1_norm(0)
    p1_tr(0)
    for ti in range(16):
        if ti + 1 < 16:
            p1_norm(ti + 1)
        banks = p1_mm(ti)
        if ti + 2 < 16:
            kb.dma("sp", out=xt[ti % 2][:], in_=xc[(ti + 2) * 128:(ti + 3) * 128, :], writes=[xt_t[ti % 2]])
        p1_evac_a(ti, banks)
        if ti >= 1:
            p1_evac_b(ti - 1)
        if ti + 1 < 16:
            p1_tr(ti + 1)
    p1_evac_b(15)
    if "kvcT" in tap_d:
        tap("kvcT", kvcT[:].rearrange("p a t -> p (a t)"), [kvcT_t])
        tap("KsT", KsT[:].rearrange("p a t -> p (a t)"), KsT_t)
        tap("Vw", Vw[:].rearrange("p a g d -> p (a g d)"), Vw_t)
    if stage <= 1:
        kb.finish()
        return nc

    for i in range(2):
        kb.free(xt[i], [xt_t[i]])
        kb.free(hTt[i], [hTt_t[i]])
    kb.free(lnmix, [lnmix_t])
    kb.free(hbB, [hbB_t])
    kb.free(sm1, sm1_t)
    w1 = []
    for kind, src_d in ((0, w1k_d), (1, w1v_d)):
        w1.append(ring_load(src_d.rearrange("(l d) n -> d l n", d=128), ""))
    w2sb, w2_t = kb.alloc_tok("w2sb", [128, 2, 2, 128], BF16)
    kb.dma("pool", out=w2sb[:, 0], in_=w2k_d.rearrange("(hc p) n -> p hc n", p=128), writes=[w2_t])
    kb.dma("pool", out=w2sb[:, 1], in_=w2v_d.rearrange("(hc p) n -> p hc n", p=128), writes=[w2_t])
    peT, peT_t = kb.alloc_tok("peT", [128, 2, 32], BF16)
    kb.dma("pool", out=peT[:, 0], in_=pekT_d, writes=[peT_t])
    kb.dma("pool", out=peT[:, 1], in_=pevT_d, writes=[peT_t])
    kcmpT, kcmpT_t = kb.alloc_tok("kcmpT", [128, 2, 128], BF16)
    Vc, Vc_t = kb.alloc_tok("Vc", [128, 2, 162], BF16)
    for g in range(2):
        kb.op("dve", lambda e: e.memset(Vc[:, g, 128:129], 1.0), [], [Vc_t])
        kb.dma("sp", out=Vc[:, g, 129:161], in_=wcs_d, writes=[Vc_t])
    gx, gx_t = kb.alloc_tok("gx", [128, 128], F32)
    gu, gu_t = kb.alloc_tok("gu", [128, 128], F32)
    gs, gs_t = kb.alloc_tok("gs", [128, 128], F32)
    gT, gT_t = kb.alloc_tok("gT", [128, 2, 128], BF16)
    for kind in range(2):
        wv, wtk = w1[kind]
        for g in range(2):
            srcv = kvcT[:, kind * 2 + g, :].rearrange("p (j s) -> p j s", s=16)
            for hc in range(2):
                bk = nextps(0, 4)
                for l in range(32):
                    rhs = srcv[:, 0:127, l] if l < 16 else srcv[:, 1:128, l - 16]
                    kb.op("pe", lambda e: e.matmul(out=psb[bk][:, 0:127], lhsT=wv[:, l, hc * 128:(hc + 1) * 128], rhs=rhs,
                                                   start=(l == 0), stop=(l == 31)), [kvcT_t, wtk], [pst[bk]])
                for l in range(32):
                    kb.op("pe", lambda e: e.matmul(out=psb[bk][:, 128:129], lhsT=wv[:, l, hc * 128:(hc + 1) * 128],
                                                   rhs=peT[:, kind, l:l + 1], start=(l == 0), stop=(l == 31)),
                          [peT_t, wtk], [pst[bk]])
                kb.op("dve", lambda e: e.tensor_copy(out=small[:, 64:65], in_=psb[bk][:, 128:129]), [pst[bk]], [small_t])
                kb.op("dve", lambda e: e.tensor_scalar(out=gx[:, 0:127], in0=psb[bk][:, 0:127], scalar1=small[:, 64:65], scalar2=None,
                                                       op0=ALU.add), [pst[bk], small_t], [gx_t])
                kb.op("dve", lambda e: e.tensor_tensor(out=gu[:, 0:127], in0=gx[:, 0:127], in1=gx[:, 0:127], op=ALU.mult), [gx_t], [gu_t])
                kb.op("dve", lambda e: e.tensor_scalar(out=gu[:, 0:127], in0=gu[:, 0:127], scalar1=0.044715, scalar2=1.0,
                                                       op0=ALU.mult, op1=ALU.add), [gu_t], [gu_t])
                kb.op("dve", lambda e: e.tensor_tensor(out=gu[:, 0:127], in0=gu[:, 0:127], in1=gx[:, 0:127], op=ALU.mult), [gu_t, gx_t], [gu_t])
                kb.op("act", lambda e: e.activation(out=gs[:, 0:127], in_=gu[:, 0:127], func=AF.Sigmoid, scale=1.5957691216057308),
                      [gu_t], [gs_t])
                kb.op("dve", lambda e: e.tensor_tensor(out=gT[:, hc, 0:127], in0=gx[:, 0:127], in1=gs[:, 0:127], op=ALU.mult),
                      [gx_t, gs_t], [gT_t])
            bk = nextps(0, 4)
            if kind == 0:
                for hc in range(2):
                    kb.op("pe", lambda e: e.matmul(out=psb[bk][:, 0:127], lhsT=w2sb[:, 0, hc, :], rhs=gT[:, hc, 0:127],
                                                   start=(hc == 0), stop=(hc == 1)), [w2_t, gT_t], [pst[bk]])
                kb.op("act", lambda e: e.copy(out=kcmpT[:, g, 0:127], in_=psb[bk][:, 0:127]), [pst[bk]], [kcmpT_t])
            else:
                for hc in range(2):
                    kb.op("pe", lambda e: e.matmul(out=psb[bk][0:127, 0:128], lhsT=gT[:, hc, 0:127], rhs=w2sb[:, 1, hc, :],
                                                   start=(hc == 0), stop=(hc == 1)), [w2_t, gT_t], [pst[bk]])
                kb.op("act", lambda e: e.copy(out=Vc[0:127, g, 0:128], in_=psb[bk][0:127, 0:128]), [pst[bk]], [Vc_t])
    kb.free(kvcT, [kvcT_t])
    kb.free(gx, [gx_t])
    kb.free(gu, [gu_t])
    kb.free(gs, [gs_t])
    kb.free(gT, [gT_t])
    QT2, QT2_t = kb.alloc_tok("QT2", [128, 2, 8, 1024], BF16, ntok=4)
    qw = [ring_load(w_in[:, qc * 512:(qc + 1) * 512].rearrange("(kc p) n -> p kc n", p=128), "") for qc in range(2)]

    def q_mm(qc, ot):
        wv, wtk = qw[qc]
        bk = nextps(0, 4)
        for kc in range(16):
            kb.op("pe", lambda e: e.matmul(out=psb[bk][:, 0:512], lhsT=hTo[:, kc, ot * 128:(ot + 1) * 128], rhs=wv[:, kc, :],
                                           start=(kc == 0), stop=(kc == 15)), [hTo_t[ot], wtk], [pst[bk]])
        return bk

    def q_evac(qc, ot, bk, par):
        t1, t1_t = (tkm, tkm_t) if par == 0 else (tkmB, tkmB_t)
        t2, t2_t = tk2s[par]
        rope(psb[bk][:, 0:512].rearrange("p (h d) -> p h d", h=4), 4, 8 + ot,
             t2[:, 0:512].rearrange("p (h d) -> p h d", h=4), pst[bk], t2_t)
        kb.op("act", lambda e: e.copy(out=t1[:], in_=psb[bk][:, 0:512]), [pst[bk]], [t1_t])
        tb = 4 + par
        pv = bfview(tb)
        for j in range(8):
            srcb, srct = (t1, t1_t) if j < 4 else (t2, t2_t)
            jj = j % 4
            kb.op("pe", lambda e: e.transpose(out=pv[:, j * 128:(j + 1) * 128], in_=srcb[:, jj * 128:(jj + 1) * 128],
                                              identity=identb[:]), [srct, identb_t], [pst[tb]])
        kb.op("dve", lambda e: e.tensor_copy(out=QT2[:, :, 4 * qc:4 * qc + 4, ot * 128:(ot + 1) * 128],
                                             in_=pv[:, 0:1024].rearrange("p (a h d) -> p a h d", a=2, h=4)),
              [pst[tb]], [QT2_t[qc * 2 + ot // 4]])

    qitems = [(qc, ot) for qc in range(2) for ot in range(8)]
    pend = q_mm(*qitems[0])
    for i, (qc, ot) in enumerate(qitems):
        bk = pend
        if i + 1 < len(qitems):
            pend = q_mm(*qitems[i + 1])
        q_evac(qc, ot, bk, i % 2)
    wga, wga_t = kb.alloc_tok("wga", [128, 16, 24], BF16)
    kb.dma("pool", out=wga[:], in_=w_in[:, 2560:2584].rearrange("(kc p) n -> p kc n", p=128), writes=[wga_t])
    gate_sb, gate_t = kb.alloc_tok("gate", [128, 8, 24], F32)
    for ot in range(8):
        bk = nextps(0, 4)
        for kc in range(16):
            kb.op("pe", lambda e: e.matmul(out=psb[bk][:, 0:24], lhsT=hTo[:, kc, ot * 128:(ot + 1) * 128], rhs=wga[:, kc, :],
                                           start=(kc == 0), stop=(kc == 15)), [hTo_t[ot], wga_t], [pst[bk]])
        kb.op("act", lambda e: e.activation(out=gate_sb[:, ot, :], in_=psb[bk][:, 0:24], func=AF.Sigmoid), [pst[bk]], [gate_t])
    ub, ub_t = kb.alloc_tok("ub", [128, 1040], F32)
    sa, sa_t = kb.alloc_tok("sa", [128, 1040], F32)
    sbb, sbb_t = kb.alloc_tok("sbb", [128, 1040], F32)
    invcnt, invcnt_t = kb.alloc_tok("invcnt", [128, 4, 16], F32)
    kb.dma("sp", out=invcnt[:], in_=invcnt_d, writes=[invcnt_t])
    dT, dT_t = kb.alloc_tok("dT", [128, 8, 1024], BF16, ntok=8)
    for uc in range(2):
        wv, wtk = ring_load(w_in[:, 2584 + uc * 512:2584 + (uc + 1) * 512].rearrange("(kc p) n -> p kc n", p=128), "")
        for c4 in range(4):
            c8 = uc * 4 + c4
            wi = c8 // 2
            w = POOL_SIZES[wi]
            bk = nextps(0, 4)
            for kc in range(16):
                kb.op("pe", lambda e: e.matmul(out=psb[bk][:, 0:16], lhsT=wv[:, kc, c4 * 128:(c4 + 1) * 128], rhs=hTh[:, kc, :],
                                               start=(kc == 0), stop=(kc == 15)), [hTh_t, wtk], [pst[bk]])
            kb.op("act", lambda e: e.copy(out=ub[:, 0:16], in_=psb[bk][:, 0:16]), [pst[bk]], [ub_t])
            for tq in range(2):
                bk = nextps(0, 4)
                for kc in range(16):
                    kb.op("pe", lambda e: e.matmul(out=psb[bk][:, 0:512], lhsT=wv[:, kc, c4 * 128:(c4 + 1) * 128],
                                                   rhs=hTo[:, kc, tq * 512:(tq + 1) * 512], start=(kc == 0), stop=(kc == 15)),
                          hTo_t[4 * tq:4 * tq + 4] + [wtk], [pst[bk]])
                kb.op("act", lambda e: e.copy(out=ub[:, 16 + tq * 512:16 + (tq + 1) * 512], in_=psb[bk][:, 0:512]), [pst[bk]], [ub_t])
            cur, cur_t = ub, ub_t
            bufs = [(sa, sa_t), (sbb, sbb_t)]
            step = 1
            bi = 0
            while step < w:
                nb, nb_t = bufs[bi]
                lo = 2 * step - 1
                kb.op("dve", lambda e: e.tensor_tensor(out=nb[:, lo:1040], in0=cur[:, lo:1040], in1=cur[:, lo - step:1040 - step],
                                                       op=ALU.add), [cur_t], [nb_t])
                cur, cur_t = nb, nb_t
                bi ^= 1
                step *= 2
            kb.op("dve", lambda e: e.scalar_tensor_tensor(out=dT[:, c8, :], in0=cur[:, 16:1040], scalar=1.0 / w, in1=ub[:, 16:1040],
                                                          op0=ALU.mult, op1=ALU.subtract), [cur_t, ub_t], [dT_t[c8]])
            kb.op("dve", lambda e: e.tensor_tensor(out=rt[:, 0, 0, :], in0=cur[:, 16:32], in1=invcnt[:, wi, :], op=ALU.mult),
                  [cur_t, invcnt_t], [rt_t])
            kb.op("dve", lambda e: e.tensor_tensor(out=dT[:, c8, 0:16], in0=rt[:, 0, 0, :], in1=ub[:, 16:32], op=ALU.subtract),
                  [rt_t, ub_t], [dT_t[c8]])
    kb.free(hTo, hTo_t)
    kb.free(hTh, [hTh_t])
    kb.free(ub, [ub_t])
    kb.free(sa, [sa_t])
    kb.free(sbb, [sbb_t])
    kb.free(wga, [wga_t])
    if "QT2" in tap_d:
        tap("QT2", QT2[:].rearrange("p a h q -> p (a h q)"), QT2_t)
        tap("dT", dT[:].rearrange("p a q -> p (a q)"), dT_t)
        tap("gate", gate_sb[:].rearrange("p a q -> p (a q)"), [gate_t])
        tap("kcmpT", kcmpT[:].rearrange("p a q -> p (a q)"), [kcmpT_t])
        tap("Vc", Vc[:].rearrange("p a q -> p (a q)"), [Vc_t])
    if stage <= 2:
        kb.finish()
        return nc

    for i in range(NSLOT):
        kb.free(ring[i], [ring_t[i]])

    def rms(src_ap, src_toks, n, ln_ap, ln_tok, out_ap, out_toks, sm, sm_t, col):
        kb.op("act", lambda e: e.activation(out=junk[:, 0:n], in_=src_ap, func=AF.Square, accum_out=sm[:, col:col + 1]),
              src_toks, [junk_t, sm_t], guard=True)
        kb.op("dve", lambda e: e.tensor_scalar(out=sm[:, col + 1:col + 2], in0=sm[:, col:col + 1], scalar1=1.0 / n,
                                               scalar2=EPS, op0=ALU.mult, op1=ALU.add), [sm_t], [sm_t])
        kb.op("act", lambda e: e.activation(out=sm[:, col + 2:col + 3], in_=sm[:, col + 1:col + 2], func=AF.Sqrt), [sm_t], [sm_t])
        kb.op("dve", lambda e: e.reciprocal(out=sm[:, col + 3:col + 4], in_=sm[:, col + 2:col + 3]), [sm_t], [sm_t])
        kb.op("dve", lambda e: e.scalar_tensor_tensor(out=out_ap, in0=src_ap, scalar=sm[:, col + 3:col + 4], in1=ln_ap,
                                                      op0=ALU.mult, op1=ALU.mult), list(src_toks) + [sm_t, ln_tok], out_toks)

    mixtok, mixtok_t = kb.alloc_tok("mixtok", [128, 8, 2048], BF16, ntok=8)
    wpool_sb, wpool_t = kb.alloc_tok("wpool", [128, 4, 2, 256], BF16)
    for G in range(4):
        kb.dma("pool", out=wpool_sb[:, G], in_=wpool_d[G].rearrange("(k p) n -> p k n", p=128), writes=[wpool_t])
    bpool_sb, bpool_t = kb.alloc_tok("bpool", [128, 1024], F32)
    pscale_sb, pscale_t = kb.alloc_tok("pscale", [128, 1024], F32)
    gnpool_sb, gnpool_t = kb.alloc_tok("gnpool", [128, 1024], F32)
    gnnsa_sb, gnnsa_t = kb.alloc_tok("gnnsa", [128, 1024], F32)
    kb.dma("sp", out=bpool_sb[:], in_=bpool_d, writes=[bpool_t])
    kb.dma("sp", out=pscale_sb[:], in_=pscale_d, writes=[pscale_t])
    kb.dma("sp", out=gnpool_sb[:], in_=gnpool_d, writes=[gnpool_t])
    kb.dma("sp", out=gnnsa_sb[:], in_=gnnsa_d, writes=[gnnsa_t])
    opool, opool_t = kb.alloc_tok("opool", [128, 1024], F32)
    sm2, sm2_t = kb.alloc_tok("sm2", [128, 64], F32)
    for ot in range(8):
        pb0 = (ot % 2) * 2
        for G in range(4):
            bk = pb0 + G // 2
            for k2 in range(2):
                kb.op("pe", lambda e: e.matmul(out=psb[bk][:, (G % 2) * 256:(G % 2) * 256 + 256],
                                               lhsT=dT[:, 2 * G + k2, ot * 128:(ot + 1) * 128], rhs=wpool_sb[:, G, k2, :],
                                               start=(k2 == 0), stop=(k2 == 1)), [dT_t[2 * G + k2], wpool_t], [pst[bk]])
        for half in range(2):
            kb.op("dve", lambda e: e.tensor_tensor(out=opool[:, half * 512:(half + 1) * 512], in0=psb[pb0 + half][:, 0:512],
                                                   in1=bpool_sb[:, half * 512:(half + 1) * 512], op=ALU.add),
                  [pst[pb0 + half], bpool_t], [opool_t])
        kb.op("dve", lambda e: e.tensor_tensor(out=opool[:], in0=opool[:], in1=pscale_sb[:], op=ALU.mult), [opool_t, pscale_t], [opool_t])
        if ot == 0 and "opool" in tap_d:
            tap("opool", opool[:], [opool_t])
        rms(opool[:], [opool_t], 1024, gnpool_sb[:], gnpool_t, mixtok[:, ot, 1024:2048], [mixtok_t[ot]], sm2, sm2_t, 0)
    if stage <= 2.5:
        kb.finish()
        return nc
    kb.free(dT, dT_t)
    kb.free(bpool_sb, [bpool_t])
    kb.free(pscale_sb, [pscale_t])
    kb.free(wpool_sb, [wpool_t])
    cmpb_sb, cmpb_t = kb.alloc_tok("cmpb", [128, 1024], BF16)
    emat_sb, emat_t = kb.alloc_tok("emat", [128, 2048], BF16)
    causb_sb, causb_t = kb.alloc_tok("causb", [128, 4, 512], BF16)
    winb_sb, winb_t = kb.alloc_tok("winb", [128, 8, 512], BF16)
    amul_sb, amul_t = kb.alloc_tok("amul", [128, 8, 32], F32)
    aadd_sb, aadd_t = kb.alloc_tok("aadd", [128, 8, 32], F32)
    kb.dma("sp", out=cmpb_sb[:], in_=cmpbias_d, writes=[cmpb_t])
    kb.dma("sp", out=emat_sb[:], in_=emat_d, writes=[emat_t])
    kb.dma("sp", out=causb_sb[:], in_=causb_d, writes=[causb_t])
    kb.dma("sp", out=winb_sb[:], in_=winb_d, writes=[winb_t])
    kb.dma("sp", out=amul_sb[:], in_=amul_d, writes=[amul_t])
    kb.dma("sp", out=aadd_sb[:], in_=aadd_d, writes=[aadd_t])
    PT = []
    PT_t = []
    for i in range(3):
        t, tk = kb.alloc_tok("PT%d" % i, [128, 512], BF16)
        PT.append(t)
        PT_t.append(tk)
    ptpos = [0]
    selbT, selbT_t = kb.alloc_tok("selbT", [128, 2, 512], BF16, ntok=2)
    kb.op("dve", lambda e: e.memset(selbT[:], 0.0), [], selbT_t)
    oacc, oacc_t = kb.alloc_tok("oacc", [128, 4, 1024], F32, ntok=4)
    impsb, imp_t = kb.alloc_tok("impsb", [128, 4, 32], F32, ntok=4)
    impm, impm_t = kb.alloc_tok("impm", [128, 32], F32)
    imp2, imp2_t = kb.alloc_tok("imp2", [128, 32], F32)
    m8, m8_t = kb.alloc_tok("m8", [128, 16], F32)
    selb, selb_t = kb.alloc_tok("selb", [128, 32], BF16)
    sm3 = []
    sm3_t = []
    for i in range(4):
        t, tk = kb.alloc_tok("sm3_%d" % i, [128, 8], F32)
        sm3.append(t)
        sm3_t.append(tk)
    smpos = [0]

    def evac_multi(jobs):
        ss = []
        for _ in jobs:
            i = smpos[0]
            smpos[0] = (i + 1) % 4
            ss.append((sm3[i], sm3_t[i]))
        for (job, (s, st)) in zip(jobs, ss):
            kb.op("dve", lambda e: e.tensor_scalar(out=s[:, 0:1], in0=job[1], scalar1=1e-30, scalar2=None, op0=ALU.max), [job[2]], [st])
        for (job, (s, st)) in zip(jobs, ss):
            kb.op("dve", lambda e: e.reciprocal(out=s[:, 1:2], in_=s[:, 0:1]), [st], [st])
        for (job, (s, st)) in zip(jobs, ss):
            tile, gcol = job[4], job[6]
            kb.op("dve", lambda e: e.tensor_tensor(out=s[:, 2:3], in0=s[:, 1:2], in1=gate_sb[:, tile, gcol:gcol + 1], op=ALU.mult),
                  [st, gate_t], [st])
        for (job, (s, st)) in zip(jobs, ss):
            ps_ap_num, _, ps_tok, j, tile, head, gcol, first, imp_ap, imp_first = job
            dst = oacc[:, j, head * 128:(head + 1) * 128]
            if first:
                kb.op("dve", lambda e: e.tensor_scalar(out=dst, in0=ps_ap_num, scalar1=s[:, 2:3], scalar2=None, op0=ALU.mult),
                      [ps_tok, st], [oacc_t[j]])
            else:
                kb.op("dve", lambda e: e.scalar_tensor_tensor(out=dst, in0=ps_ap_num, scalar=s[:, 2:3], in1=dst, op0=ALU.mult,
                                                              op1=ALU.add), [ps_tok, st, oacc_t[j]], [oacc_t[j]])
        for (job, (s, st)) in zip(jobs, ss):
            ps_ap_num, _, ps_tok, j, tile, head, gcol, first, imp_ap, imp_first = job
            if imp_ap is not None:
                if imp_first:
                    kb.op("dve", lambda e: e.tensor_scalar(out=impsb[:, j, :], in0=imp_ap, scalar1=s[:, 1:2], scalar2=None, op0=ALU.mult),
                          [ps_tok, st], [imp_t[j]])
                else:
                    kb.op("dve", lambda e: e.scalar_tensor_tensor(out=impsb[:, j, :], in0=imp_ap, scalar=s[:, 1:2], in1=impsb[:, j, :],
                                                                  op0=ALU.mult, op1=ALU.add), [ps_tok, st, imp_t[j]], [imp_t[j]])

    def expo(bk, np_):
        i = ptpos[0]
        ptpos[0] = (i + 1) % 3
        kb.op("act", lambda e: e.activation(out=PT[i][0:np_, :], in_=psb[bk][0:np_, 0:512], func=AF.Exp, scale=SCALE),
              [pst[bk]], [PT_t[i]])
        return PT[i], PT_t[i]

    accpair = [0]
    for qc in range(2):
        qsl = slice(qc * 512, (qc + 1) * 512)
        qtoks = lambda head: [QT2_t[(head // 4) * 2 + qc]]
        for g in range(2):
            for r in range(4):
                head = 4 * g + r
                bk = nextps(0, 3)
                kb.op("pe", lambda e: e.matmul(out=psb[bk][0:127, 0:512], lhsT=kcmpT[:, g, 0:127], rhs=QT2[:, 0, head, qsl],
                                               start=True, stop=False), [kcmpT_t] + qtoks(head), [pst[bk]])
                kb.op("pe", lambda e: e.matmul(out=psb[bk][0:127, 0:512], lhsT=identb[0:127, 0:127], rhs=cmpb_sb[0:127, qsl],
                                               start=False, stop=True), [identb_t, cmpb_t], [pst[bk]])
                P, P_t = expo(bk, 127)
                jobs = []
                for j in range(4):
                    ob = nextps(3, 8)
                    kb.op("pe", lambda e: e.matmul(out=psb[ob][:, 0:161], lhsT=P[0:127, j * 128:(j + 1) * 128], rhs=Vc[0:127, g, 0:161],
                                                   start=True, stop=True), [P_t, Vc_t], [pst[ob]])
                    jobs.append((psb[ob][:, 0:128], psb[ob][:, 128:129], pst[ob], j, 4 * qc + j, head, 3 * head + 0, True,
                                 psb[ob][:, 129:161], r == 0))
                evac_multi(jobs)
            for j in range(4):
                tile = 4 * qc + j
                kb.op("dve", lambda e: e.tensor_tensor(out=impm[:], in0=impsb[:, j, :], in1=amul_sb[:, tile, :], op=ALU.mult),
                      [imp_t[j], amul_t], [impm_t])
                kb.op("dve", lambda e: e.tensor_tensor(out=impm[:], in0=impm[:], in1=aadd_sb[:, tile, :], op=ALU.add),
                      [impm_t, aadd_t], [impm_t])
                if "impm" in tap_d and g == 0 and j == 0 and qc == 0:
                    tap("impm", impm[:], [impm_t])
                kb.op("dve", lambda e: e.max(out=m8[:, 0:8], in_=impm[:]), [impm_t], [m8_t])
                kb.op("dve", lambda e: e.match_replace(out=imp2[:], in_to_replace=m8[:, 0:8], in_values=impm[:], imm_value=-3.0e38),
                      [m8_t, impm_t], [imp2_t])
                kb.op("dve", lambda e: e.max(out=m8[:, 8:16], in_=imp2[:]), [imp2_t], [m8_t])
                kb.op("dve", lambda e: e.tensor_scalar(out=m8[:, 15:16], in0=m8[:, 15:16], scalar1=0.0, scalar2=None, op0=ALU.max),
                      [m8_t], [m8_t])
                kb.op("dve", lambda e: e.tensor_scalar(out=imp2[:], in0=impm[:], scalar1=m8[:, 15:16], scalar2=None, op0=ALU.is_ge),
                      [impm_t, m8_t], [imp2_t])
                kb.op("dve", lambda e: e.tensor_scalar(out=selb[:], in0=imp2[:], scalar1=-1.0, scalar2=-NEGB, op0=ALU.add, op1=ALU.mult),
                      [imp2_t], [selb_t])
                if "selb" in tap_d and g == 0 and j == 0 and qc == 0:
                    tap("selb", imp2[:], [imp2_t])
                tb = 6
                pv = bfview(tb)
                kb.op("pe", lambda e: e.transpose(out=pv[0:32, 0:128], in_=selb[:, 0:32], identity=identb[:]), [selb_t, identb_t], [pst[tb]])
                kb.op("act", lambda e: e.copy(out=selbT[0:32, g, j * 128:(j + 1) * 128], in_=pv[0:32, 0:128]), [pst[tb]], [selbT_t[g]])
        if "oacc_cmp" in tap_d and qc == 0:
            tap("oacc_cmp", oacc[:].rearrange("p a q -> p (a q)"), oacc_t)
        if stage <= 2.7:
            kb.finish()
            return nc
        items = []
        for g in range(2):
            for r in range(4):
                head = 4 * g + r
                for br in range(2):
                    kts = list(range(12 + 4 * qc)) if br == 0 else [4 + 4 * qc + i for i in range(8)]
                    for ii, kt in enumerate(kts):
                        items.append((g, head, br, ii, kt, ii == 0, ii == len(kts) - 1))

        def emitS(it):
            g, head, br, ii, kt, _, _ = it
            KT, KT_t = (KsT, KsT_t) if br == 0 else (KwT, KwT_t)
            bk = nextps(0, 3)
            kb.op("pe", lambda e: e.matmul(out=psb[bk][:, 0:512], lhsT=KT[:, g, kt * 128:(kt + 1) * 128], rhs=QT2[:, 1, head, qsl],
                                           start=True, stop=False), [KT_t[kt]] + qtoks(head), [pst[bk]])
            if br == 0:
                diag = kt - (8 + 4 * qc)
                kb.op("pe", lambda e: e.matmul(out=psb[bk][:, 0:512], lhsT=emat_sb[:, kt * 128:(kt + 1) * 128],
                                               rhs=selbT[:, g, :], start=False, stop=(diag < 0)),
                      [emat_t, selbT_t[g]], [pst[bk]])
                if diag >= 0:
                    kb.op("pe", lambda e: e.matmul(out=psb[bk][:, 0:512], lhsT=identb[:], rhs=causb_sb[:, diag, :],
                                                   start=False, stop=True), [identb_t, causb_t], [pst[bk]])
            else:
                kb.op("pe", lambda e: e.matmul(out=psb[bk][:, 0:512], lhsT=identb[:], rhs=winb_sb[:, ii, :],
                                               start=False, stop=True), [identb_t, winb_t], [pst[bk]])
            return bk

        pendq = [emitS(items[0]), emitS(items[1])]
        accb = None
        for idx, it in enumerate(items):
            g, head, br, ii, kt, first_, last_ = it
            bk = pendq.pop(0)
            if idx + 2 < len(items):
                pendq.append(emitS(items[idx + 2]))
            if first_:
                accb = [nextps(3, 8) for _ in range(4)]
            V, V_t = (Vs, Vs_t) if br == 0 else (Vw, Vw_t)
            if br == 0:
                jr = [(j, kt == 0, kt == 8 + 4 * qc + j) for j in range(4) if kt <= 8 + 4 * qc + j]
            else:
                jr = [(j, ii == j, ii == j + 4) for j in range(4) if j <= ii <= j + 4]
            P, P_t = expo(bk, 128)
            for (j, st_, sp_) in jr:
                ab = accb[j]
                kb.op("pe", lambda e: e.matmul(out=psb[ab][:, 0:129], lhsT=P[:, j * 128:(j + 1) * 128],
                                               rhs=V[:, kt, g, 0:129], start=st_, stop=sp_), [P_t, V_t[kt]], [pst[ab]])
            if last_:
                evac_multi([(psb[accb[j]][:, 0:128], psb[accb[j]][:, 128:129], pst[accb[j]], j, 4 * qc + j, head, 3 * head + 1 + br,
                             False, None, False) for j in range(4)])
        if "oacc" in tap_d and qc == 0:
            tap("oacc", oacc[:].rearrange("p a q -> p (a q)"), oacc_t)
        for j in range(4):
            tile = 4 * qc + j
            rms(oacc[:, j, :], [oacc_t[j]], 1024, gnnsa_sb[:], gnnsa_t, mixtok[:, tile, 0:1024], [mixtok_t[tile]], sm2, sm2_t, 8)
    if "mixtok" in tap_d:
        tap("mixtok", mixtok[:].rearrange("p a q -> p (a q)"), mixtok_t)
    if stage <= 3:
        kb.finish()
        return nc

    for _t, _k in ((QT2, QT2_t), (KsT, KsT_t), (KwT, KwT_t), (Vs, Vs_t), (Vw, Vw_t), (kcmpT, [kcmpT_t]), (Vc, [Vc_t]),
                   (cmpb_sb, [cmpb_t]), (emat_sb, [emat_t]), (causb_sb, [causb_t]), (winb_sb, [winb_t]), (amul_sb, [amul_t]),
                   (aadd_sb, [aadd_t]), (PT[0], [PT_t[0]]), (PT[1], [PT_t[1]]), (PT[2], [PT_t[2]]), (selbT, selbT_t), (oacc, oacc_t), (impsb, imp_t),
                   (impm, [impm_t]), (imp2, [imp2_t]), (m8, [m8_t]), (selb, [selb_t]), (gate_sb, [gate_t]), (gnpool_sb, [gnpool_t]),
                   (gnnsa_sb, [gnnsa_t]), (opool, [opool_t]), (w2sb, [w2_t]), (peT, [peT_t]), (tkm, [tkm_t]), (tkm2, [tkm2_t]), (tkmB, [tkmB_t]), (tkm2B, [tkm2B_t]),
                   (cs4, [cs4_t]), (rt, [rt_t]), (invcnt, [invcnt_t])):
        kb.free(_t, _k)
    for i in range(4):
        kb.free(sm3[i], [sm3_t[i]])
    x1, x1_t = kb.alloc_tok("x1", [128, 8, D], F32, ntok=8)
    for ot in range(8):
        kb.dma("sp", out=x1[:, ot, :], in_=xc[1024 + ot * 128:1024 + (ot + 1) * 128, :], writes=[x1_t[ot]])
    mixT, mixT_t = kb.alloc_tok("mixT", [128, 16, 1024], BF16, ntok=8)
    ring = []
    ring_t = []
    NSLOT = 3
    for i in range(NSLOT):
        t, tk = kb.alloc_tok("ringb%d" % i, [128, 8192], BF16)
        ring.append(t)
        ring_t.append(tk)
    rp2 = [0]

    def ring_load2(src_ap, nslot):
        i = rp2[0]
        rp2[0] = (i + 1) % nslot
        a, b = src_ap.shape[1], src_ap.shape[2]
        view = ring[i][:, 0:a * b].rearrange("p (a b) -> p a b", a=a)
        kb.dma("pool", out=view, in_=src_ap, writes=[ring_t[i]])
        return view, ring_t[i]

    for ot in range(8):
        transpose16(mixtok[:, ot, :], mixtok_t[ot], lambda half: mixT[:, half * 8:(half + 1) * 8, ot * 128:(ot + 1) * 128], [mixT_t[ot]])
    for oc in range(4):
        wv, wtk = ring_load2(wout_d[:, oc * 512:(oc + 1) * 512].rearrange("(kc p) n -> p kc n", p=128), 3)
        for ot in range(8):
            bk = nextps(0, 4)
            for kc in range(16):
                kb.op("pe", lambda e: e.matmul(out=psb[bk][:, 0:512], lhsT=mixT[:, kc, ot * 128:(ot + 1) * 128], rhs=wv[:, kc, :],
                                               start=(kc == 0), stop=(kc == 15)), [mixT_t[ot], wtk], [pst[bk]])
            kb.op("dve", lambda e: e.tensor_tensor(out=x1[:, ot, oc * 512:(oc + 1) * 512], in0=psb[bk][:, 0:512],
                                                   in1=x1[:, ot, oc * 512:(oc + 1) * 512], op=ALU.add), [pst[bk], x1_t[ot]], [x1_t[ot]])
    if "x1" in tap_d:
        tap("x1", x1[:].rearrange("p a q -> p (a q)"), x1_t)
    if stage <= 4:
        kb.finish()
        return nc
    kb.free(mixtok, mixtok_t)
    kb.free(mixT, mixT_t)
    for i in range(3):
        kb.free(ring[i], [ring_t[i]])
    h2T, h2T_t = kb.alloc_tok("h2T", [128, 16, 1024], BF16, ntok=8)
    ring = []
    ring_t = []
    for i in range(4):
        t, tk = kb.alloc_tok("ringc%d" % i, [128, 8192], BF16)
        ring.append(t)
        ring_t.append(tk)
    rp2[0] = 0
    moe_pref = []
    if stage >= 7:
        moe_pref.append(ring_load2(wg_d[0].rearrange("(kc p) n -> p kc n", p=128), 4))
        moe_pref.append(ring_load2(wu_d[0].rearrange("(kc p) n -> p kc n", p=128), 4))
        moe_pref.append(ring_load2(wd_d[0].rearrange("(fc p) n -> p fc n", p=128), 4))
    lnmoe_sb, lnmoe_t = kb.alloc_tok("lnmoe", [128, D], F32)
    kb.dma("sp", out=lnmoe_sb[:], in_=lnmoe_d, writes=[lnmoe_t])
    wr_sb, wr_t = kb.alloc_tok("wr", [128, 16, 36], BF16)
    kb.dma("pool", out=wr_sb[:], in_=wr_d.rearrange("(kc p) n -> p kc n", p=128), writes=[wr_t])
    br_sb, br_t = kb.alloc_tok("br", [128, 36], F32)
    kb.dma("sp", out=br_sb[:], in_=br_d, writes=[br_t])
    gmat, gmat_t = kb.alloc_tok("gmat", [128, 8, 32], F32)
    lgb, lgb_t = kb.alloc_tok("lgb", [128, 36], F32)
    lem, lem_t = kb.alloc_tok("lem", [128, 32], F32)
    rs, rs_t = kb.alloc_tok("rs", [128, 32], F32)
    g1b, g1b_t = kb.alloc_tok("g1b", [128, 32], F32)
    mr8, mr8_t = kb.alloc_tok("mr8", [128, 8], F32)
    hbC, hbC_t = kb.alloc_tok("hbC", [128, D], BF16)
    hb2 = [(hb, hb_t), (hbC, hbC_t)]
    sm4, sm4_t = kb.alloc_tok("sm4", [128, 8, 4], F32, ntok=8)

    lgbA, lgbA_t = kb.alloc_tok("lgbA", [128, 8, 36], F32, ntok=8)
    def router_front(ot):
        hbx, hbx_t = hb2[ot % 2]
        rms(x1[:, ot, :], [x1_t[ot]], D, lnmoe_sb[:], lnmoe_t, hbx[:], [hbx_t], sm4[:, ot, :], sm4_t[ot], 0)
        transpose16(hbx, hbx_t, lambda half: h2T[:, half * 8:(half + 1) * 8, ot * 128:(ot + 1) * 128], [h2T_t[ot]],
                    banks=(4 + 2 * (ot % 2), 5 + 2 * (ot % 2)))
        bk = nextps(0, 4)
        for kc in range(16):
            kb.op("pe", lambda e: e.matmul(out=psb[bk][:, 0:36], lhsT=h2T[:, kc, ot * 128:(ot + 1) * 128], rhs=wr_sb[:, kc, :],
                                           start=(kc == 0), stop=(kc == 15)), [h2T_t[ot], wr_t], [pst[bk]])
        kb.op("dve", lambda e: e.tensor_tensor(out=lgbA[:, ot, :], in0=psb[bk][:, 0:36], in1=br_sb[:], op=ALU.add),
              [pst[bk], br_t], [lgbA_t[ot]])
        return bk

    lemA, lemA_t = kb.alloc_tok("lemA", [128, 8, 32], F32, ntok=8)
    rsA, rsA_t = kb.alloc_tok("rsA", [128, 8, 32], F32, ntok=8)
    g1A, g1A_t = kb.alloc_tok("g1A", [128, 8, 32], F32, ntok=8)
    mrA, mrA_t = kb.alloc_tok("mrA", [128, 8, 8], F32, ntok=8)
    gmat_tt = [Tok() for _ in range(8)]
    rbanks = [router_front(ot) for ot in range(8)]
    T8 = range(8)
    for ot in T8:
        kb.op("dve", lambda e: e.tensor_reduce(out=rsA[:, ot, 0:1], in_=lgbA[:, ot, 0:4], axis=AX.X, op=ALU.max), [lgbA_t[ot]], [rsA_t[ot]])
    for ot in T8:
        kb.op("dve", lambda e: e.tensor_scalar(out=rsA[:, ot, 4:8], in0=lgbA[:, ot, 0:4], scalar1=rsA[:, ot, 0:1], scalar2=None,
                                               op0=ALU.is_ge), [lgbA_t[ot], rsA_t[ot]], [rsA_t[ot]])
    for ot in T8:
        kb.op("dve", lambda e: e.tensor_scalar(out=rsA[:, ot, 1:2], in0=rsA[:, ot, 0:1], scalar1=-1.0, scalar2=None, op0=ALU.mult),
              [rsA_t[ot]], [rsA_t[ot]])
    for ot in T8:
        kb.op("act", lambda e: e.activation(out=rsA[:, ot, 8:12], in_=lgbA[:, ot, 0:4], func=AF.Exp, bias=rsA[:, ot, 1:2], scale=1.0,
                                            accum_out=rsA[:, ot, 2:3]), [lgbA_t[ot], rsA_t[ot]], [rsA_t[ot]], guard=True)
    for ot in T8:
        kb.op("dve", lambda e: e.reciprocal(out=rsA[:, ot, 3:4], in_=rsA[:, ot, 2:3]), [rsA_t[ot]], [rsA_t[ot]])
    for ot in T8:
        kb.op("dve", lambda e: e.tensor_scalar(out=rsA[:, ot, 12:16], in0=rsA[:, ot, 4:8], scalar1=-1.0, scalar2=BIG, op0=ALU.add,
                                               op1=ALU.mult), [rsA_t[ot]], [rsA_t[ot]])
    for g in range(4):
        for ot in T8:
            kb.op("dve", lambda e: e.tensor_scalar(out=lemA[:, ot, g * 8:(g + 1) * 8], in0=lgbA[:, ot, 4 + 8 * g:12 + 8 * g],
                                                   scalar1=rsA[:, ot, 12 + g:13 + g], scalar2=None, op0=ALU.add),
                  [lgbA_t[ot], rsA_t[ot]], [lemA_t[ot]])
    for ot in T8:
        kb.op("dve", lambda e: e.max(out=mrA[:, ot, :], in_=lemA[:, ot, :]), [lemA_t[ot]], [mrA_t[ot]])
    for ot in T8:
        kb.op("dve", lambda e: e.tensor_tensor(out=rsA[:, ot, 16:17], in0=mrA[:, ot, 1:2], in1=mrA[:, ot, 0:1], op=ALU.subtract),
              [mrA_t[ot]], [rsA_t[ot]])
    for ot in T8:
        kb.op("act", lambda e: e.activation(out=rsA[:, ot, 17:18], in_=rsA[:, ot, 16:17], func=AF.Exp), [rsA_t[ot]], [rsA_t[ot]])
    for ot in T8:
        kb.op("dve", lambda e: e.tensor_scalar(out=rsA[:, ot, 18:19], in0=rsA[:, ot, 17:18], scalar1=1.0, scalar2=None, op0=ALU.add),
              [rsA_t[ot]], [rsA_t[ot]])
    for ot in T8:
        kb.op("dve", lambda e: e.reciprocal(out=rsA[:, ot, 19:20], in_=rsA[:, ot, 18:19]), [rsA_t[ot]], [rsA_t[ot]])
    for ot in T8:
        kb.op("dve", lambda e: e.tensor_tensor(out=rsA[:, ot, 20:21], in0=rsA[:, ot, 19:20], in1=rsA[:, ot, 3:4], op=ALU.mult),
              [rsA_t[ot]], [rsA_t[ot]])
    for ot in T8:
        kb.op("dve", lambda e: e.tensor_tensor(out=rsA[:, ot, 21:22], in0=rsA[:, ot, 3:4], in1=rsA[:, ot, 20:21], op=ALU.subtract),
              [rsA_t[ot]], [rsA_t[ot]])
    for ot in T8:
        kb.op("dve", lambda e: e.tensor_scalar(out=g1A[:, ot, :], in0=lemA[:, ot, :], scalar1=mrA[:, ot, 0:1], scalar2=rsA[:, ot, 20:21],
                                               op0=ALU.is_equal, op1=ALU.mult), [lemA_t[ot], mrA_t[ot], rsA_t[ot]], [g1A_t[ot]])
    for ot in T8:
        kb.op("dve", lambda e: e.tensor_scalar(out=gmat[:, ot, :], in0=lemA[:, ot, :], scalar1=mrA[:, ot, 1:2], scalar2=rsA[:, ot, 21:22],
                                               op0=ALU.is_equal, op1=ALU.mult), [lemA_t[ot], mrA_t[ot], rsA_t[ot]], [gmat_tt[ot]])
    for ot in T8:
        kb.op("dve", lambda e: e.tensor_tensor(out=gmat[:, ot, :], in0=gmat[:, ot, :], in1=g1A[:, ot, :], op=ALU.add),
              [gmat_tt[ot], g1A_t[ot]], [gmat_tt[ot]])
    gmat_t.w = gmat_tt[7].w
    if "gmat" in tap_d:
        tap("gmat", gmat[:].rearrange("p a q -> p (a q)"), [gmat_t])
    if stage <= 5:
        kb.finish()
        return nc
    kb.free(lnmoe_sb, [lnmoe_t])
    kb.free(hbC, [hbC_t])
    kb.free(sm4, sm4_t)
    kb.free(lgbA, lgbA_t)
    kb.free(lemA, lemA_t)
    kb.free(rsA, rsA_t)
    kb.free(g1A, g1A_t)
    kb.free(mrA, mrA_t)
    AT, AT_t = kb.alloc_tok("AT", [128, 4, 1024], BF16, ntok=8)
    sgt = []
    sgt_t = []
    for i in range(2):
        t, tk = kb.alloc_tok("sg%d" % i, [128, 512], F32)
        sgt.append(t)
        sgt_t.append(tk)
    sgp = 0
    nexp = NEXP if stage >= 7 else 0
    for ex in range(nexp):
        if ex == 0:
            (wgv, wg_t), (wuv, wu_t), (wdv, wd_t) = moe_pref
        else:
            wgv, wg_t = ring_load2(wg_d[ex].rearrange("(kc p) n -> p kc n", p=128), 4)
            wuv, wu_t = ring_load2(wu_d[ex].rearrange("(kc p) n -> p kc n", p=128), 4)
            wdv, wd_t = ring_load2(wd_d[ex].rearrange("(fc p) n -> p fc n", p=128), 4)
        for fc in range(4):
            for tq in range(2):
                bg = nextps(0, 4)
                bu = nextps(0, 4)
                for (bk_, wv_, wt_) in ((bg, wgv, wg_t), (bu, wuv, wu_t)):
                    for kc in range(16):
                        kb.op("pe", lambda e: e.matmul(out=psb[bk_][:, 0:512], lhsT=wv_[:, kc, fc * 128:(fc + 1) * 128],
                                                       rhs=h2T[:, kc, tq * 512:(tq + 1) * 512], start=(kc == 0), stop=(kc == 15)),
                              h2T_t[4 * tq:4 * tq + 4] + [wt_], [pst[bk_]])
                sg, sg_t = sgt[sgp], sgt_t[sgp]
                sgp ^= 1
                kb.op("act", lambda e: e.activation(out=sg[:], in_=psb[bg][:, 0:512], func=AF.Silu), [pst[bg]], [sg_t])
                kb.op("dve", lambda e: e.tensor_tensor(out=AT[:, fc, tq * 512:(tq + 1) * 512], in0=sg[:], in1=psb[bu][:, 0:512],
                                                       op=ALU.mult), [sg_t, pst[bu]], [AT_t[fc * 2 + tq]])
        for ot in range(8):
            for dc in range(4):
                by = nextps(4, 8)
                for fc in range(4):
                    kb.op("pe", lambda e: e.matmul(out=psb[by][:, 0:512], lhsT=AT[:, fc, ot * 128:(ot + 1) * 128],
                                                   rhs=wdv[:, fc, dc * 512:(dc + 1) * 512], start=(fc == 0), stop=(fc == 3)),
                          [AT_t[fc * 2 + ot // 4], wd_t], [pst[by]])
                kb.op("dve", lambda e: e.scalar_tensor_tensor(out=x1[:, ot, dc * 512:(dc + 1) * 512], in0=psb[by][:, 0:512],
                                                              scalar=gmat[:, ot, ex:ex + 1], in1=x1[:, ot, dc * 512:(dc + 1) * 512],
                                                              op0=ALU.mult, op1=ALU.add), [pst[by], gmat_t, x1_t[ot]], [x1_t[ot]])
    for i in range(4):
        kb.free(ring[i], [ring_t[i]])
    kb.free(AT, AT_t)
    kb.free(h2T, h2T_t)
    lnfin_sb, lnfin_t = kb.alloc_tok("lnfin", [128, D], F32)
    kb.dma("sp", out=lnfin_sb[:], in_=lnfin_d, writes=[lnfin_t])
    of = []
    of_t = []
    for i in range(2):
        t, tk = kb.alloc_tok("of%d" % i, [128, D], F32)
        of.append(t)
        of_t.append(tk)
    for ot in range(8):
        rms(x1[:, ot, :], [x1_t[ot]], D, lnfin_sb[:], lnfin_t, of[ot % 2][:], [of_t[ot % 2]], sm2, sm2_t, 24)
        kb.dma("sp", out=out_d[ot * 128:(ot + 1) * 128, :], in_=of[ot % 2][:], reads=[of_t[ot % 2]])
    kb.finish()
    return nc


def _bf(a):
    return np.ascontiguousarray(a.astype(ml_dtypes.bfloat16))


def _rep(v, n=128):
    return np.ascontiguousarray(np.broadcast_to(np.asarray(v, np.float32).reshape(1, -1), (n, v.size)))


def const_tables(h):
    off = -1024 + 1024 * h
    t = {}
    c = np.arange(2048)
    t["valid"] = np.ascontiguousarray(((c + off) >= 0).astype(np.float32).reshape(16, 128).T)
    t["identb"] = _bf(np.eye(128, dtype=np.float32))
    t["identf"] = np.eye(128, dtype=np.float32)
    inv = (500000.0 ** (-np.arange(0, 32, 2, dtype=np.float32) / np.float32(32))).astype(np.float32)
    t["invf"] = _rep(inv)
    j = np.arange(128)[:, None]
    cq = 1024 + np.arange(1024)[None, :]
    ok = (16 * j + off >= 0) & (16 * j + 31 <= cq) & (j < 127)
    t["cmpbias"] = _bf(np.where(ok, 0.0, NEGB).astype(np.float32))
    cs = np.arange(128)[:, None] * 16
    ss = np.arange(32)[None, :] * 64
    ov = np.clip(np.minimum(cs + 32, ss + 64) - np.maximum(cs, ss), 0, None)
    t["wcs"] = _bf((ov / 32.0).astype(np.float32))
    key = np.arange(2048)[None, :]
    t["emat"] = _bf((key // 64 == np.arange(128)[:, None]).astype(np.float32))
    k = np.arange(128)[:, None, None]
    q = np.arange(512)[None, None, :]
    i4 = np.arange(4)[None, :, None]
    t["causb"] = _bf(np.where(128 * i4 + k <= q, 0.0, NEGB).astype(np.float32))
    i8 = np.arange(8)[None, :, None]
    kk = 128 * i8 + k
    t["winb"] = _bf(np.where((kk > q) & (kk <= q + 512), 0.0, NEGB).astype(np.float32))
    tq = (1024 + np.arange(1024) + off)[:, None]
    jg = np.arange(32)[None, :] + off // 64
    okb = (jg >= 0) & (jg * 64 <= tq)
    cur = tq // 64
    forced = (jg == 0) | ((cur - jg >= 0) & (cur - jg < 2))
    amul = (okb & ~forced).astype(np.float32)
    aadd = np.where(okb & forced, BIG, np.where(okb, 0.0, -BIG)).astype(np.float32)
    t["amul"] = np.ascontiguousarray(amul.reshape(8, 128, 32).transpose(1, 0, 2))
    t["aadd"] = np.ascontiguousarray(aadd.reshape(8, 128, 32).transpose(1, 0, 2))
    tg = 1024 + np.arange(16) + off
    ic = np.stack([1.0 / np.minimum(tg + 1, w) for w in POOL_SIZES], 0).astype(np.float32)
    t["invcnt"] = np.ascontiguousarray(np.broadcast_to(ic[None], (128, 4, 16)))
    return t


def prep_inputs(inp, stage=99):
    L = 0
    shared = {}
    shared["w_in"] = np.ascontiguousarray(inp["w_in"][L])
    shared["lnmix"] = _rep(inp["ln_mix"][L])
    shared["pekT"] = np.ascontiguousarray(inp["pe_cmp_k"][L].T)
    shared["pevT"] = np.ascontiguousarray(inp["pe_cmp_v"][L].T)
    shared["w1k"] = np.ascontiguousarray(inp["w_cmp_k1"][L])
    shared["w1v"] = np.ascontiguousarray(inp["w_cmp_v1"][L])
    shared["w2k"] = np.ascontiguousarray(inp["w_cmp_k2"][L])
    shared["w2v"] = np.ascontiguousarray(inp["w_cmp_v2"][L])
    shared["wpool"] = np.ascontiguousarray(inp["w_pool"][L])
    shared["bpool"] = _rep(inp["b_pool"][L])
    shared["pscale"] = _rep(inp["pool_scale"][L])
    shared["gnnsa"] = _rep(inp["gn_nsa"][L])
    shared["gnpool"] = _rep(inp["gn_pool"][L])
    shared["wout"] = np.ascontiguousarray(inp["w_out"][L])
    shared["lnmoe"] = _rep(inp["ln_moe"][L])
    wr = np.concatenate([inp["w_router_group"][L]] + [inp["w_router_expert"][L][g] for g in range(4)], axis=1)
    shared["wr"] = np.ascontiguousarray(wr.astype(np.float32))
    br = np.concatenate([inp["b_router_group"][L].reshape(-1), inp["b_router_expert"][L].reshape(-1)])
    shared["br"] = _rep(br)
    if stage >= 7:
        shared["wg"] = np.ascontiguousarray(inp["w_gate"][L])
        shared["wu"] = np.ascontiguousarray(inp["w_up"][L])
        shared["wd"] = np.ascontiguousarray(inp["w_down"][L])
    shared["lnfin"] = _rep(inp["ln_final"])
    tabs = [const_tables(0), const_tables(1)]
    x = np.asarray(inp["x"], np.float32)
    pos = np.asarray(inp["positions"], np.int32)
    maps = []
    for c in range(8):
        b, h = c // 2, c % 2
        m = dict(shared)
        m.update(tabs[h])
        if h == 0:
            xcx = np.concatenate([np.zeros((1024, D), np.float32), x[b, 0:1024]], 0)
            pc = np.concatenate([np.zeros((1024,), np.int32), pos[b, 0:1024]], 0)
        else:
            xcx = x[b]
            pc = pos[b]
        m["xc"] = np.ascontiguousarray(xcx)
        m["posT"] = np.ascontiguousarray(pc.reshape(16, 128).T.astype(np.int32))
        maps.append(m)
    return maps


_NC_CACHE = {}


def kernel(**inputs):
    inp = {k: np.asarray(v) for k, v in inputs.items()}
    if "nc" not in _NC_CACHE:
        import os
        _NC_CACHE["nc"] = build(99, poison=bool(os.environ.get("KPOISON")))
    nc = _NC_CACHE["nc"]
    maps = prep_inputs(inp, 99)
    res = run_bass_kernel_spmd(nc, maps, core_ids=list(range(8)))
    out = np.zeros((NB, S, D), np.float32)
    for c in range(8):
        b, h = c // 2, c % 2
        out[b, h * 1024:(h + 1) * 1024] = res.results[c]["out"]
    return out
```

```python
import numpy as np
import ml_dtypes
import concourse.bass as bass
import concourse.mybir as mybir
from concourse.bass_utils import run_bass_kernel_spmd

F32 = mybir.dt.float32
BF16 = mybir.dt.bfloat16
I32 = mybir.dt.int32
AF = mybir.ActivationFunctionType
ALU = mybir.AluOpType
AX = mybir.AxisListType

D = 2048
S = 2048
NB = 4
HD = 128
NH = 8
EPS = 1e-6
SCALE = HD ** -0.5
NEGB = -30000.0
BIG = 1e30
NEXP = 32
DFF = 512
POOL_SIZES = (2, 4, 8, 16)
DT_SIZE = {F32: 4, BF16: 2, I32: 4}


class Tok:
    __slots__ = ("w", "r")

    def __init__(self, fence=None):
        self.w = None
        self.r = dict(fence) if fence else {}


class KB:
    def __init__(self, nc):
        self.nc = nc
        self.eng = dict(pe=nc.tensor, dve=nc.vector, act=nc.scalar, pool=nc.gpsimd, sp=nc.sync)
        self.sems = {}
        self.cnt = {}
        for k in self.eng:
            self.sems[k] = nc.alloc_semaphore("p_" + k)
            self.cnt[k] = 0
        self.waited = {k: {} for k in self.eng}
        self.dring = {}
        self.dpos = {}
        for q, n in (("sp", 12), ("pool", 12), ("act", 4)):
            ks = []
            for i in range(n):
                key = "d_%s%d" % (q, i)
                self.sems[key] = nc.alloc_semaphore(key)
                self.cnt[key] = 0
                ks.append(key)
            self.dring[q] = ks
            self.dpos[q] = 0
        self.base = (nc.sbuf_base + 63) // 64 * 64
        self.top = nc.sbuf_top
        self.live = []
        self.fences = []
        self.nalloc = 0
        self.ents = {}

    def alloc_tok(self, name, shape, dtype, ntok=1):
        n = 1
        for s in shape[1:]:
            n *= s
        size = (n * DT_SIZE[dtype] + 63) // 64 * 64
        off = self.base
        for (o, s_, _) in sorted(self.live):
            if off + size <= o:
                break
            off = max(off, o + s_)
        if off + size > self.top:
            raise RuntimeError("SBUF overflow allocating %s (%d bytes) live=%s" % (name, size, sorted(self.live)))
        self.nalloc += 1
        ent = (off, size, "%s_%d" % (name, self.nalloc))
        self.live.append(ent)
        t = self.nc.alloc_sbuf_tensor_at(ent[2], list(shape), dtype, offset=off)
        fence = {}
        for (o, s_, deps) in self.fences:
            if o < off + size and off < o + s_:
                for k, v in deps.items():
                    if fence.get(k, 0) < v:
                        fence[k] = v
        toks = [Tok(fence) for _ in range(ntok)]
        self.ents[id(t)] = (ent, t)
        return (t, toks[0]) if ntok == 1 else (t, toks)

    def free(self, t, toks):
        ent, _ = self.ents.pop(id(t))
        self.live.remove(ent)
        deps = {}
        for tk in toks:
            if tk.w:
                k, v = tk.w
                if deps.get(k, 0) < v:
                    deps[k] = v
            for k, v in tk.r.items():
                if deps.get(k, 0) < v:
                    deps[k] = v
        self.fences.append((ent[0], ent[1], deps))

    def _deps(self, reads, writes):
        d = {}
        for b in reads:
            if b.w:
                k, v = b.w
                if d.get(k, 0) < v:
                    d[k] = v
        for b in writes:
            if b.w:
                k, v = b.w
                if d.get(k, 0) < v:
                    d[k] = v
            for k, v in b.r.items():
                if d.get(k, 0) < v:
                    d[k] = v
        return d

    def _wait(self, X, deps):
        w = self.waited[X]
        for key, val in deps.items():
            if val <= 0:
                continue
            if key == X and X == "pe":
                continue
            if w.get(key, 0) >= val:
                continue
            self.eng[X].wait_ge(self.sems[key], val)
            w[key] = val

    def op(self, X, fn, reads=(), writes=(), guard=False):
        self._wait(X, self._deps(reads, writes))
        inst = fn(self.eng[X])
        self.cnt[X] += 1
        inst.then_inc(self.sems[X], 1)
        if guard:
            g = self.gbuf
            inst2 = self.eng[X].copy(out=g[:, 0:1], in_=g[:, 1:2])
            self.cnt[X] += 1
            inst2.then_inc(self.sems[X], 1)
        c = self.cnt[X]
        for b in reads:
            if b.r.get(X, 0) < c:
                b.r[X] = c
        for b in writes:
            b.w = (X, c)
            b.r = {}
        return inst

    def dma(self, Q, out, in_, reads=(), writes=()):
        ring = self.dring[Q]
        i = self.dpos[Q]
        self.dpos[Q] = (i + 1) % len(ring)
        key = ring[i]
        deps = self._deps(reads, writes)
        if self.cnt[key] > 0:
            deps[key] = max(deps.get(key, 0), self.cnt[key])
        self._wait(Q, deps)
        inst = self.eng[Q].dma_start(out=out, in_=in_)
        self.cnt[key] += 16
        inst.then_inc(self.sems[key], 16)
        c = self.cnt[key]
        for b in reads:
            b.r[key] = c
        for b in writes:
            b.w = (key, c)
            b.r = {}
        return inst

    def finish(self):
        deps = {}
        for k, v in self.cnt.items():
            if v > 0:
                deps[k] = v
        self._wait("sp", deps)


def build(stage=99, taps=(), poison=False):
    nc = bass.Bass("TRN2", target_bir_lowering=False)
    kb = KB(nc)
    if poison:
        nel = (kb.top - kb.base) // 4
        parena = nc.alloc_sbuf_tensor_at("poison_arena", [128, nel], F32, offset=kb.base)
        pt_ = Tok()
        kb.op("dve", lambda e: e.memset(parena[:], float("nan")), [], [pt_])
        for X in ("pe", "act", "pool", "sp"):
            kb._wait(X, {"dve": 1})

    def din(name, shape, dt=F32):
        return nc.dram_tensor(name, list(shape), dt, kind="ExternalInput").ap()

    def dout(name, shape, dt=F32):
        return nc.dram_tensor(name, list(shape), dt, kind="ExternalOutput").ap()

    xc = din("xc", [2048, D])
    w_in = din("w_in", [D, 3608])
    posT_d = din("posT", [128, 16], I32)
    invf_d = din("invf", [128, 16])
    valid_d = din("valid", [128, 16])
    identb_d = din("identb", [128, 128], BF16)
    identf_d = din("identf", [128, 128])
    lnmix_d = din("lnmix", [128, D])
    pekT_d = din("pekT", [128, 32])
    pevT_d = din("pevT", [128, 32])
    w1k_d = din("w1k", [4096, 256])
    w1v_d = din("w1v", [4096, 256])
    w2k_d = din("w2k", [256, 128])
    w2v_d = din("w2v", [256, 128])
    cmpbias_d = din("cmpbias", [128, 1024], BF16)
    wcs_d = din("wcs", [128, 32], BF16)
    emat_d = din("emat", [128, 2048], BF16)
    causb_d = din("causb", [128, 4, 512], BF16)
    winb_d = din("winb", [128, 8, 512], BF16)
    amul_d = din("amul", [128, 8, 32])
    aadd_d = din("aadd", [128, 8, 32])
    invcnt_d = din("invcnt", [128, 4, 16])
    wpool_d = din("wpool", [4, 256, 256])
    bpool_d = din("bpool", [128, 1024])
    pscale_d = din("pscale", [128, 1024])
    gnnsa_d = din("gnnsa", [128, 1024])
    gnpool_d = din("gnpool", [128, 1024])
    wout_d = din("wout", [D, D])
    lnmoe_d = din("lnmoe", [128, D])
    wr_d = din("wr", [D, 36])
    br_d = din("br", [128, 36])
    if stage >= 7:
        wg_d = din("wg", [NEXP, D, DFF])
        wu_d = din("wu", [NEXP, D, DFF])
        wd_d = din("wd", [NEXP, DFF, D])
    lnfin_d = din("lnfin", [128, D])
    out_d = dout("out", [1024, D])
    tap_d = {}
    for (nm, shp, tdt) in taps:
        tap_d[nm] = dout("tap_" + nm, shp, tdt)

    def tap(nm, src_ap, toks, dst=None):
        if nm in tap_d:
            kb.dma("sp", out=(tap_d[nm] if dst is None else dst), in_=src_ap, reads=toks)

    gb_, gb_t_ = kb.alloc_tok("gbuf", [128, 2], F32)
    kb.gbuf = gb_
    kb.op("dve", lambda e: e.memset(gb_[:], 0.0), [], [gb_t_])
    kb._wait("act", {"dve": kb.cnt["dve"]})
    psb = [nc.alloc_psum_tensor("ps%d" % i, [128, 512], F32) for i in range(8)]
    pst = [Tok() for _ in range(8)]

    identb, identb_t = kb.alloc_tok("identb", [128, 128], BF16)
    identf, identf_t = kb.alloc_tok("identf", [128, 128], F32)
    kb.dma("sp", out=identb[:], in_=identb_d, writes=[identb_t])
    kb.dma("sp", out=identf[:], in_=identf_d, writes=[identf_t])
    small, small_t = kb.alloc_tok("small", [128, 256], F32)
    valid_sb, valid_t = kb.alloc_tok("valid", [128, 16], F32)
    kb.dma("sp", out=valid_sb[:], in_=valid_d, writes=[valid_t])

    cs4, cs4_t = kb.alloc_tok("cs4", [128, 2, 16, 4, 16], F32)
    rt, rt_t = kb.alloc_tok("ropetmp", [128, 4, 4, 16], F32)

    def rope(ps_ap, H, ti, out_ap, ps_tok, out_tok):
        sin = cs4[:, 0, ti, 0:H, :]
        cos = cs4[:, 1, ti, 0:H, :]
        x1 = ps_ap[:, :, 0:16]
        x2 = ps_ap[:, :, 16:32]
        kb.op("dve", lambda e: e.tensor_tensor(out=rt[:, 0, 0:H, :], in0=x1, in1=cos, op=ALU.mult), [ps_tok, cs4_t], [rt_t])
        kb.op("dve", lambda e: e.tensor_tensor(out=rt[:, 1, 0:H, :], in0=x2, in1=sin, op=ALU.mult), [ps_tok, cs4_t], [rt_t])
        kb.op("dve", lambda e: e.tensor_tensor(out=rt[:, 2, 0:H, :], in0=x2, in1=cos, op=ALU.mult), [ps_tok, cs4_t], [rt_t])
        kb.op("dve", lambda e: e.tensor_tensor(out=rt[:, 3, 0:H, :], in0=x1, in1=sin, op=ALU.mult), [ps_tok, cs4_t], [rt_t])
        kb.op("dve", lambda e: e.tensor_tensor(out=out_ap[:, :, 0:16], in0=rt[:, 0, 0:H, :], in1=rt[:, 1, 0:H, :],
                                               op=ALU.subtract), [rt_t], [out_tok])
        kb.op("dve", lambda e: e.tensor_tensor(out=out_ap[:, :, 16:32], in0=rt[:, 2, 0:H, :], in1=rt[:, 3, 0:H, :],
                                               op=ALU.add), [rt_t], [out_tok])
        kb.op("act", lambda e: e.copy(out=out_ap[:, :, 32:128], in_=ps_ap[:, :, 32:128]), [ps_tok], [out_tok])

    NSLOT = 3
    ring = []
    ring_t = []
    for i in range(NSLOT):
        t, tk = kb.alloc_tok("ring%d" % i, [128, 8192], BF16)
        ring.append(t)
        ring_t.append(tk)
    ring_pos = [0]

    def ring_load(src_ap, shape_str, **kw):
        i = ring_pos[0]
        ring_pos[0] = (i + 1) % NSLOT
        a, b = src_ap.shape[1], src_ap.shape[2]
        view = ring[i][:, 0:a * b].rearrange("p (a b) -> p a b", a=a)
        kb.dma("pool", out=view, in_=src_ap, writes=[ring_t[i]])
        return view, ring_t[i]

    wkv = []
    for cg in range(3):
        v, tk = ring_load(w_in[:, 1024 + cg * 512:1024 + (cg + 1) * 512].rearrange("(kc p) n -> p kc n", p=128), "")
        wkv.append((v, tk))
    junk, junk_t = kb.alloc_tok("junk", [128, D], BF16)
    hb, hb_t = kb.alloc_tok("hb", [128, D], BF16)
    KsT, KsT_t = kb.alloc_tok("KsT", [128, 2, 2048], BF16, ntok=16)
    KwT, KwT_t = kb.alloc_tok("KwT", [128, 2, 2048], BF16, ntok=16)
    Vs, Vs_t = kb.alloc_tok("Vs", [128, 16, 2, 129], BF16, ntok=16)
    Vw, Vw_t = kb.alloc_tok("Vw", [128, 16, 2, 129], BF16, ntok=16)
    tkm, tkm_t = kb.alloc_tok("tkm", [128, 512], BF16)
    tkm2, tkm2_t = kb.alloc_tok("tkm2", [128, 512], BF16)
    hTo, hTo_t = kb.alloc_tok("hTown", [128, 16, 1024], BF16, ntok=8)
    hTh, hTh_t = kb.alloc_tok("hThalo", [128, 16, 16], BF16)
    kvcT, kvcT_t = kb.alloc_tok("kvcT", [128, 4, 2048], BF16)
    lnmix, lnmix_t = kb.alloc_tok("lnmix", [128, D], F32)
    kb.dma("sp", out=lnmix[:], in_=lnmix_d, writes=[lnmix_t])
    xt = []
    xt_t = []
    for i in range(2):
        t, tk = kb.alloc_tok("xt%d" % i, [128, D], F32)
        xt.append(t)
        xt_t.append(tk)
    hTt = []
    hTt_t = []
    for i in range(2):
        t, tk = kb.alloc_tok("hTt%d" % i, [128, 16, 128], BF16)
        hTt.append(t)
        hTt_t.append(tk)
    posi, posi_t = kb.alloc_tok("posi", [128, 16], I32)
    invf, invf_t = kb.alloc_tok("invf", [128, 16], F32)
    kb.dma("sp", out=posi[:], in_=posT_d, writes=[posi_t])
    kb.dma("sp", out=invf[:], in_=invf_d, writes=[invf_t])
    posf, posf_t = kb.alloc_tok("posf", [128, 16], F32)
    ang, ang_t = kb.alloc_tok("ang", [128, 2, 16, 16], F32)
    rtmp, rtmp_t = kb.alloc_tok("rtmp", [128, 2, 16, 16], F32)
    rki, rki_t = kb.alloc_tok("rki", [128, 2, 16, 16], I32)
    kb.op("dve", lambda e: e.tensor_copy(out=posf[:], in_=posi[:]), [posi_t], [posf_t])
    for ti in range(16):
        kb.op("dve", lambda e: e.tensor_scalar(out=ang[:, 0, ti, :], in0=invf[:], scalar1=posf[:, ti:ti + 1],
                                               scalar2=None, op0=ALU.mult), [invf_t, posf_t], [ang_t])
    kb.op("dve", lambda e: e.tensor_scalar(out=ang[:, 1], in0=ang[:, 0], scalar1=float(np.pi / 2), scalar2=None,
                                           op0=ALU.add), [ang_t], [ang_t])
    TWO_PI = float(2 * np.pi)
    kb.op("dve", lambda e: e.tensor_scalar(out=rtmp[:], in0=ang[:], scalar1=1.0 / TWO_PI, scalar2=None,
                                           op0=ALU.mult), [ang_t], [rtmp_t])
    kb.op("dve", lambda e: e.tensor_copy(out=rki[:], in_=rtmp[:]), [rtmp_t], [rki_t])
    kb.op("dve", lambda e: e.tensor_copy(out=rtmp[:], in_=rki[:]), [rki_t], [rtmp_t])
    C1 = 6.28125
    C2 = TWO_PI - C1
    kb.op("dve", lambda e: e.scalar_tensor_tensor(out=ang[:], in0=rtmp[:], scalar=-C1, in1=ang[:],
                                                  op0=ALU.mult, op1=ALU.add), [rtmp_t, ang_t], [ang_t])
    kb.op("dve", lambda e: e.scalar_tensor_tensor(out=ang[:], in0=rtmp[:], scalar=-C2, in1=ang[:],
                                                  op0=ALU.mult, op1=ALU.add), [rtmp_t, ang_t], [ang_t])
    PI = float(np.pi)
    kb.op("dve", lambda e: e.tensor_scalar(out=rtmp[:], in0=ang[:], scalar1=PI, scalar2=-TWO_PI,
                                           op0=ALU.is_gt, op1=ALU.mult), [ang_t], [rtmp_t])
    kb.op("dve", lambda e: e.tensor_tensor(out=ang[:], in0=ang[:], in1=rtmp[:], op=ALU.add), [ang_t, rtmp_t], [ang_t])
    kb.op("dve", lambda e: e.tensor_scalar(out=rtmp[:], in0=ang[:], scalar1=-PI, scalar2=TWO_PI,
                                           op0=ALU.is_lt, op1=ALU.mult), [ang_t], [rtmp_t])
    kb.op("dve", lambda e: e.tensor_tensor(out=ang[:], in0=ang[:], in1=rtmp[:], op=ALU.add), [ang_t, rtmp_t], [ang_t])
    kb.op("dve", lambda e: e.tensor_scalar(out=ang[:], in0=ang[:], scalar1=3.141592, scalar2=-3.141592,
                                           op0=ALU.min, op1=ALU.max), [ang_t], [ang_t])
    kb.op("act", lambda e: e.activation(out=rtmp[:], in_=ang[:], func=AF.Sin), [ang_t], [rtmp_t])
    for hh in range(4):
        kb.op("dve", lambda e: e.tensor_copy(out=cs4[:, :, :, hh, :], in_=rtmp[:]), [rtmp_t], [cs4_t])
    if "cs" in tap_d:
        tap("cs", rtmp[:].rearrange("p a t f -> p (a t f)"), [rtmp_t])

    for _t, _k in ((posi, posi_t), (invf, invf_t), (posf, posf_t), (ang, ang_t), (rtmp, rtmp_t), (rki, rki_t)):
        kb.free(_t, [_k])
    for g in range(2):
        kb.op("dve", lambda e: e.tensor_copy(out=Vs[:, :, g, 128], in_=valid_sb[:]), [valid_t], Vs_t)
        kb.op("dve", lambda e: e.tensor_copy(out=Vw[:, :, g, 128], in_=valid_sb[:]), [valid_t], Vw_t)

    psrot = {}

    def nextps(lo=0, hi=8):
        i = psrot.get((lo, hi), lo)
        psrot[(lo, hi)] = lo + (i + 1 - lo) % (hi - lo)
        return i

    def bfview(bank):
        return psb[bank][:].bitcast(BF16)

    sm1, sm1_t = kb.alloc_tok("sm1", [128, 16, 4], F32, ntok=16)
    hbB, hbB_t = kb.alloc_tok("hbB", [128, D], BF16)
    hbs = [(hb, hb_t), (hbB, hbB_t)]
    tkmB, tkmB_t = kb.alloc_tok("tkmB", [128, 512], BF16)
    tkm2B, tkm2B_t = kb.alloc_tok("tkm2B", [128, 512], BF16)
    tk2s = [(tkm2, tkm2_t), (tkm2B, tkm2B_t)]

    def norm_tile(lnrep, lnrep_t, ti, xbuf, xbuf_t, hbuf, hbuf_t):
        s_, st_ = sm1[:, ti, :], sm1_t[ti]
        kb.op("act", lambda e: e.activation(out=junk[:], in_=xbuf[:], func=AF.Square, accum_out=s_[:, 0:1]),
              [xbuf_t], [junk_t, st_], guard=True)
        kb.op("dve", lambda e: e.tensor_scalar(out=s_[:, 1:2], in0=s_[:, 0:1], scalar1=1.0 / D,
                                               scalar2=EPS, op0=ALU.mult, op1=ALU.add), [st_], [st_])
        kb.op("act", lambda e: e.activation(out=s_[:, 2:3], in_=s_[:, 1:2], func=AF.Sqrt), [st_], [st_])
        kb.op("dve", lambda e: e.reciprocal(out=s_[:, 3:4], in_=s_[:, 2:3]), [st_], [st_])
        kb.op("dve", lambda e: e.scalar_tensor_tensor(out=hbuf[:], in0=xbuf[:], scalar=s_[:, 3:4],
                                                      in1=lnrep[:], op0=ALU.mult, op1=ALU.mult),
              [xbuf_t, st_, lnrep_t], [hbuf_t])

    def transpose16(src, src_t, dst_ap_fn, dst_toks, banks=(6, 7), halves=(0, 1)):
        for half in halves:
            bk = banks[half]
            pv = bfview(bk)
            for j in range(8):
                kc = half * 8 + j
                kb.op("pe", lambda e: e.transpose(out=pv[:, j * 128:(j + 1) * 128], in_=src[:, kc * 128:(kc + 1) * 128],
                                                  identity=identb[:]), [src_t, identb_t], [pst[bk]])
            kb.op("act", lambda e: e.copy(out=dst_ap_fn(half), in_=pv[:, 0:1024].rearrange("p (a b) -> p a b", a=8)),
                  [pst[bk]], dst_toks)

    def p1_norm(ti):
        hbuf, hbuf_t = hbs[ti % 2]
        norm_tile(lnmix, lnmix_t, ti, xt[ti % 2], xt_t[ti % 2], hbuf, hbuf_t)

    def p1_hinfo(ti):
        if ti >= 8:
            ot = ti - 8
            return hTo_t[ot], (lambda kc: hTo[:, kc, ot * 128:(ot + 1) * 128]), (lambda half: hTo[:, half * 8:(half + 1) * 8, ot * 128:(ot + 1) * 128])
        hcur = hTt[ti % 2]
        return hTt_t[ti % 2], (lambda kc: hcur[:, kc, :]), (lambda half: hcur[:, half * 8:(half + 1) * 8, :])

    def p1_tr(ti, halves=(0, 1)):
        hbuf, hbuf_t = hbs[ti % 2]
        hs_t, lhs, dstf = p1_hinfo(ti)
        transpose16(hbuf, hbuf_t, dstf, [hs_t], halves=halves)
        if ti == 7 and 1 in halves:
            kb.op("dve", lambda e: e.tensor_copy(out=hTh[:], in_=hTt[1][:, :, 112:128]), [hs_t], [hTh_t])

    def p1_mm(ti, between=None):
        hs_t, lhs, dstf = p1_hinfo(ti)
        banks = []
        for cg in range(3):
            bk = nextps(0, 6)
            wv, wtk = wkv[cg]
            for kc in range(16):
                kb.op("pe", lambda e: e.matmul(out=psb[bk][:, 0:512], lhsT=lhs(kc), rhs=wv[:, kc, :], start=(kc == 0),
                                               stop=(kc == 15)), [hs_t, wtk], [pst[bk]])
            banks.append(bk)
            if between is not None and cg < 2:
                between(cg)
        return banks

    t2tok = [[Tok(), Tok()], [Tok(), Tok()]]
    tkms = [(tkm, tkm_t), (tkmB, tkmB_t)]

    def p1_evac_a(ti, banks):
        par = ti % 2
        t1, t1_t = tkms[par]
        t2 = tk2s[par][0]
        for cg in range(3):
            bk = banks[cg]
            if cg == 0:
                kb.op("act", lambda e: e.copy(out=t1[:], in_=psb[bk][:, 0:512]), [pst[bk]], [t1_t])
            else:
                V, V_t = (Vs, Vs_t) if cg == 1 else (Vw, Vw_t)
                co = (cg - 1) * 256
                rope(psb[bk][:, 0:256].rearrange("p (h d) -> p h d", h=2), 2, ti,
                     t2[:, co:co + 256].rearrange("p (h d) -> p h d", h=2), pst[bk], t2tok[par][cg - 1])
                kb.op("act", lambda e: e.copy(out=V[:, ti, :, 0:128], in_=psb[bk][:, 256:512].rearrange("p (h d) -> p h d", h=2)),
                      [pst[bk]], [V_t[ti]])

    def p1_evac_b(ti):
        par = ti % 2
        t1, t1_t = tkms[par]
        t2 = tk2s[par][0]
        tb = 7
        pv = bfview(tb)
        for j in range(4):
            kb.op("pe", lambda e: e.transpose(out=pv[:, j * 128:(j + 1) * 128], in_=t1[:, j * 128:(j + 1) * 128],
                                              identity=identb[:]), [t1_t, identb_t], [pst[tb]])
        kb.op("dve", lambda e: e.tensor_copy(out=kvcT[:, :, ti * 128:(ti + 1) * 128],
                                             in_=pv[:, 0:512].rearrange("p (a b) -> p a b", a=4)), [pst[tb]], [kvcT_t])
        tb = 6
        pv = bfview(tb)
        for cg in (1, 2):
            co = (cg - 1) * 256
            for j in range(2):
                kb.op("pe", lambda e: e.transpose(out=pv[:, co + j * 128:co + (j + 1) * 128], in_=t2[:, co + j * 128:co + (j + 1) * 128],
                                                  identity=identb[:]), [t2tok[par][cg - 1], identb_t], [pst[tb]])
        kb.op("dve", lambda e: e.tensor_copy(out=KsT[:, :, ti * 128:(ti + 1) * 128],
                                             in_=pv[:, 0:256].rearrange("p (a b) -> p a b", a=2)), [pst[tb]], [KsT_t[ti]])
        kb.op("dve", lambda e: e.tensor_copy(out=KwT[:, :, ti * 128:(ti + 1) * 128],
                                             in_=pv[:, 256:512].rearrange("p (a b) -> p a b", a=2)), [pst[tb]], [KwT_t[ti]])

    def p1_norm_dma(k):
        p1_norm(k)
        if k + 2 < 16:
            kb.dma("sp", out=xt[k % 2][:], in_=xc[(k + 2) * 128:(k + 3) * 128, :], writes=[xt_t[k % 2]])

    kb.dma("sp", out=xt[0][:], in_=xc[0:128, :], writes=[xt_t[0]])
    kb.dma("sp", out=xt[1][:], in_=xc[128:256, :], writes=[xt_t[1]])
    p1_norm_dma(0)
    p1_tr(0)
    p1_norm_dma(1)
    for ti in range(16):
        banks = p1_mm(ti, (lambda cg, _ti=ti: p1_tr(_ti + 1, halves=(cg,))) if ti + 1 < 16 else None)
        if ti + 2 < 16:
            p1_norm_dma(ti + 2)
        p1_evac_a(ti, banks)
        if ti >= 1:
            p1_evac_b(ti - 1)
    p1_evac_b(15)
    if "kvcT" in tap_d:
        tap("kvcT", kvcT[:].rearrange("p a t -> p (a t)"), [kvcT_t])
        tap("KsT", KsT[:].rearrange("p a t -> p (a t)"), KsT_t)
        tap("Vw", Vw[:].rearrange("p a g d -> p (a g d)"), Vw_t)
    if stage <= 1:
        kb.finish()
        return nc

    for i in range(2):
        kb.free(xt[i], [xt_t[i]])
        kb.free(hTt[i], [hTt_t[i]])
    kb.free(lnmix, [lnmix_t])
    kb.free(hbB, [hbB_t])
    kb.free(sm1, sm1_t)
    w1 = []
    for kind, src_d in ((0, w1k_d), (1, w1v_d)):
        w1.append(ring_load(src_d.rearrange("(l d) n -> d l n", d=128), ""))
    w2sb, w2_t = kb.alloc_tok("w2sb", [128, 2, 2, 128], BF16)
    kb.dma("pool", out=w2sb[:, 0], in_=w2k_d.rearrange("(hc p) n -> p hc n", p=128), writes=[w2_t])
    kb.dma("pool", out=w2sb[:, 1], in_=w2v_d.rearrange("(hc p) n -> p hc n", p=128), writes=[w2_t])
    peT, peT_t = kb.alloc_tok("peT", [128, 2, 32], BF16)
    kb.dma("pool", out=peT[:, 0], in_=pekT_d, writes=[peT_t])
    kb.dma("pool", out=peT[:, 1], in_=pevT_d, writes=[peT_t])
    kcmpT, kcmpT_t = kb.alloc_tok("kcmpT", [128, 2, 128], BF16)
    Vc, Vc_t = kb.alloc_tok("Vc", [128, 2, 162], BF16)
    for g in range(2):
        kb.op("dve", lambda e: e.memset(Vc[:, g, 128:129], 1.0), [], [Vc_t])
        kb.dma("sp", out=Vc[:, g, 129:161], in_=wcs_d, writes=[Vc_t])
    gx, gx_t = kb.alloc_tok("gx", [128, 128], F32)
    gu, gu_t = kb.alloc_tok("gu", [128, 128], F32)
    gs, gs_t = kb.alloc_tok("gs", [128, 128], F32)
    gT, gT_t = kb.alloc_tok("gT", [128, 2, 128], BF16)
    for kind in range(2):
        wv, wtk = w1[kind]
        for g in range(2):
            srcv = kvcT[:, kind * 2 + g, :].rearrange("p (j s) -> p j s", s=16)
            for hc in range(2):
                bk = nextps(0, 4)
                for l in range(32):
                    rhs = srcv[:, 0:127, l] if l < 16 else srcv[:, 1:128, l - 16]
                    kb.op("pe", lambda e: e.matmul(out=psb[bk][:, 0:127], lhsT=wv[:, l, hc * 128:(hc + 1) * 128], rhs=rhs,
                                                   start=(l == 0), stop=(l == 31)), [kvcT_t, wtk], [pst[bk]])
                for l in range(32):
                    kb.op("pe", lambda e: e.matmul(out=psb[bk][:, 128:129], lhsT=wv[:, l, hc * 128:(hc + 1) * 128],
                                                   rhs=peT[:, kind, l:l + 1], start=(l == 0), stop=(l == 31)),
                          [peT_t, wtk], [pst[bk]])
                kb.op("dve", lambda e: e.tensor_copy(out=small[:, 64:65], in_=psb[bk][:, 128:129]), [pst[bk]], [small_t])
                kb.op("dve", lambda e: e.tensor_scalar(out=gx[:, 0:127], in0=psb[bk][:, 0:127], scalar1=small[:, 64:65], scalar2=None,
                                                       op0=ALU.add), [pst[bk], small_t], [gx_t])
                kb.op("dve", lambda e: e.tensor_tensor(out=gu[:, 0:127], in0=gx[:, 0:127], in1=gx[:, 0:127], op=ALU.mult), [gx_t], [gu_t])
                kb.op("dve", lambda e: e.tensor_scalar(out=gu[:, 0:127], in0=gu[:, 0:127], scalar1=0.044715, scalar2=1.0,
                                                       op0=ALU.mult, op1=ALU.add), [gu_t], [gu_t])
                kb.op("dve", lambda e: e.tensor_tensor(out=gu[:, 0:127], in0=gu[:, 0:127], in1=gx[:, 0:127], op=ALU.mult), [gu_t, gx_t], [gu_t])
                kb.op("act", lambda e: e.activation(out=gs[:, 0:127], in_=gu[:, 0:127], func=AF.Sigmoid, scale=1.5957691216057308),
                      [gu_t], [gs_t])
                kb.op("dve", lambda e: e.tensor_tensor(out=gT[:, hc, 0:127], in0=gx[:, 0:127], in1=gs[:, 0:127], op=ALU.mult),
                      [gx_t, gs_t], [gT_t])
            bk = nextps(0, 4)
            if kind == 0:
                for hc in range(2):
                    kb.op("pe", lambda e: e.matmul(out=psb[bk][:, 0:127], lhsT=w2sb[:, 0, hc, :], rhs=gT[:, hc, 0:127],
                                                   start=(hc == 0), stop=(hc == 1)), [w2_t, gT_t], [pst[bk]])
                kb.op("act", lambda e: e.copy(out=kcmpT[:, g, 0:127], in_=psb[bk][:, 0:127]), [pst[bk]], [kcmpT_t])
            else:
                for hc in range(2):
                    kb.op("pe", lambda e: e.matmul(out=psb[bk][0:127, 0:128], lhsT=gT[:, hc, 0:127], rhs=w2sb[:, 1, hc, :],
                                                   start=(hc == 0), stop=(hc == 1)), [w2_t, gT_t], [pst[bk]])
                kb.op("act", lambda e: e.copy(out=Vc[0:127, g, 0:128], in_=psb[bk][0:127, 0:128]), [pst[bk]], [Vc_t])
    kb.free(kvcT, [kvcT_t])
    kb.free(gx, [gx_t])
    kb.free(gu, [gu_t])
    kb.free(gs, [gs_t])
    kb.free(gT, [gT_t])
    QT2, QT2_t = kb.alloc_tok("QT2", [128, 2, 8, 1024], BF16, ntok=4)
    qw = [ring_load(w_in[:, qc * 512:(qc + 1) * 512].rearrange("(kc p) n -> p kc n", p=128), "") for qc in range(2)]

    def q_mm(qc, ot):
        wv, wtk = qw[qc]
        bk = nextps(0, 4)
        for kc in range(16):
            kb.op("pe", lambda e: e.matmul(out=psb[bk][:, 0:512], lhsT=hTo[:, kc, ot * 128:(ot + 1) * 128], rhs=wv[:, kc, :],
                                           start=(kc == 0), stop=(kc == 15)), [hTo_t[ot], wtk], [pst[bk]])
        return bk

    def q_evac(qc, ot, bk, par):
        t1, t1_t = (tkm, tkm_t) if par == 0 else (tkmB, tkmB_t)
        t2, t2_t = tk2s[par]
        rope(psb[bk][:, 0:512].rearrange("p (h d) -> p h d", h=4), 4, 8 + ot,
             t2[:, 0:512].rearrange("p (h d) -> p h d", h=4), pst[bk], t2_t)
        kb.op("act", lambda e: e.copy(out=t1[:], in_=psb[bk][:, 0:512]), [pst[bk]], [t1_t])
        tb = 4 + par
        pv = bfview(tb)
        for j in range(8):
            srcb, srct = (t1, t1_t) if j < 4 else (t2, t2_t)
            jj = j % 4
            kb.op("pe", lambda e: e.transpose(out=pv[:, j * 128:(j + 1) * 128], in_=srcb[:, jj * 128:(jj + 1) * 128],
                                              identity=identb[:]), [srct, identb_t], [pst[tb]])
        kb.op("dve", lambda e: e.tensor_copy(out=QT2[:, :, 4 * qc:4 * qc + 4, ot * 128:(ot + 1) * 128],
                                             in_=pv[:, 0:1024].rearrange("p (a h d) -> p a h d", a=2, h=4)),
              [pst[tb]], [QT2_t[qc * 2 + ot // 4]])

    qitems = [(qc, ot) for qc in range(2) for ot in range(8)]
    pend = q_mm(*qitems[0])
    for i, (qc, ot) in enumerate(qitems):
        bk = pend
        if i + 1 < len(qitems):
            pend = q_mm(*qitems[i + 1])
        q_evac(qc, ot, bk, i % 2)
    wga, wga_t = kb.alloc_tok("wga", [128, 16, 24], BF16)
    kb.dma("pool", out=wga[:], in_=w_in[:, 2560:2584].rearrange("(kc p) n -> p kc n", p=128), writes=[wga_t])
    gate_sb, gate_t = kb.alloc_tok("gate", [128, 8, 24], F32)
    for ot in range(8):
        bk = nextps(0, 4)
        for kc in range(16):
            kb.op("pe", lambda e: e.matmul(out=psb[bk][:, 0:24], lhsT=hTo[:, kc, ot * 128:(ot + 1) * 128], rhs=wga[:, kc, :],
                                           start=(kc == 0), stop=(kc == 15)), [hTo_t[ot], wga_t], [pst[bk]])
        kb.op("act", lambda e: e.activation(out=gate_sb[:, ot, :], in_=psb[bk][:, 0:24], func=AF.Sigmoid), [pst[bk]], [gate_t])
    ub, ub_t = kb.alloc_tok("ub", [128, 1040], F32)
    sa, sa_t = kb.alloc_tok("sa", [128, 1040], F32)
    sbb, sbb_t = kb.alloc_tok("sbb", [128, 1040], F32)
    invcnt, invcnt_t = kb.alloc_tok("invcnt", [128, 4, 16], F32)
    kb.dma("sp", out=invcnt[:], in_=invcnt_d, writes=[invcnt_t])
    dT, dT_t = kb.alloc_tok("dT", [128, 8, 1024], BF16, ntok=8)
    for uc in range(2):
        wv, wtk = ring_load(w_in[:, 2584 + uc * 512:2584 + (uc + 1) * 512].rearrange("(kc p) n -> p kc n", p=128), "")
        for c4 in range(4):
            c8 = uc * 4 + c4
            wi = c8 // 2
            w = POOL_SIZES[wi]
            bk = nextps(0, 4)
            for kc in range(16):
                kb.op("pe", lambda e: e.matmul(out=psb[bk][:, 0:16], lhsT=wv[:, kc, c4 * 128:(c4 + 1) * 128], rhs=hTh[:, kc, :],
                                               start=(kc == 0), stop=(kc == 15)), [hTh_t, wtk], [pst[bk]])
            kb.op("act", lambda e: e.copy(out=ub[:, 0:16], in_=psb[bk][:, 0:16]), [pst[bk]], [ub_t])
            for tq in range(2):
                bk = nextps(0, 4)
                for kc in range(16):
                    kb.op("pe", lambda e: e.matmul(out=psb[bk][:, 0:512], lhsT=wv[:, kc, c4 * 128:(c4 + 1) * 128],
                                                   rhs=hTo[:, kc, tq * 512:(tq + 1) * 512], start=(kc == 0), stop=(kc == 15)),
                          hTo_t[4 * tq:4 * tq + 4] + [wtk], [pst[bk]])
                kb.op("act", lambda e: e.copy(out=ub[:, 16 + tq * 512:16 + (tq + 1) * 512], in_=psb[bk][:, 0:512]), [pst[bk]], [ub_t])
            cur, cur_t = ub, ub_t
            bufs = [(sa, sa_t), (sbb, sbb_t)]
            step = 1
            bi = 0
            while step < w:
                nb, nb_t = bufs[bi]
                lo = 2 * step - 1
                kb.op("dve", lambda e: e.tensor_tensor(out=nb[:, lo:1040], in0=cur[:, lo:1040], in1=cur[:, lo - step:1040 - step],
                                                       op=ALU.add), [cur_t], [nb_t])
                cur, cur_t = nb, nb_t
                bi ^= 1
                step *= 2
            kb.op("dve", lambda e: e.scalar_tensor_tensor(out=dT[:, c8, :], in0=cur[:, 16:1040], scalar=1.0 / w, in1=ub[:, 16:1040],
                                                          op0=ALU.mult, op1=ALU.subtract), [cur_t, ub_t], [dT_t[c8]])
            kb.op("dve", lambda e: e.tensor_tensor(out=rt[:, 0, 0, :], in0=cur[:, 16:32], in1=invcnt[:, wi, :], op=ALU.mult),
                  [cur_t, invcnt_t], [rt_t])
            kb.op("dve", lambda e: e.tensor_tensor(out=dT[:, c8, 0:16], in0=rt[:, 0, 0, :], in1=ub[:, 16:32], op=ALU.subtract),
                  [rt_t, ub_t], [dT_t[c8]])
    kb.free(hTo, hTo_t)
    kb.free(hTh, [hTh_t])
    kb.free(ub, [ub_t])
    kb.free(sa, [sa_t])
    kb.free(sbb, [sbb_t])
    kb.free(wga, [wga_t])
    if "QT2" in tap_d:
        tap("QT2", QT2[:].rearrange("p a h q -> p (a h q)"), QT2_t)
        tap("dT", dT[:].rearrange("p a q -> p (a q)"), dT_t)
        tap("gate", gate_sb[:].rearrange("p a q -> p (a q)"), [gate_t])
        tap("kcmpT", kcmpT[:].rearrange("p a q -> p (a q)"), [kcmpT_t])
        tap("Vc", Vc[:].rearrange("p a q -> p (a q)"), [Vc_t])
    if stage <= 2:
        kb.finish()
        return nc

    for i in range(NSLOT):
        kb.free(ring[i], [ring_t[i]])

    def rms(src_ap, src_toks, n, ln_ap, ln_tok, out_ap, out_toks, sm, sm_t, col):
        kb.op("act", lambda e: e.activation(out=junk[:, 0:n], in_=src_ap, func=AF.Square, accum_out=sm[:, col:col + 1]),
              src_toks, [junk_t, sm_t], guard=True)
        kb.op("dve", lambda e: e.tensor_scalar(out=sm[:, col + 1:col + 2], in0=sm[:, col:col + 1], scalar1=1.0 / n,
                                               scalar2=EPS, op0=ALU.mult, op1=ALU.add), [sm_t], [sm_t])
        kb.op("act", lambda e: e.activation(out=sm[:, col + 2:col + 3], in_=sm[:, col + 1:col + 2], func=AF.Sqrt), [sm_t], [sm_t])
        kb.op("dve", lambda e: e.reciprocal(out=sm[:, col + 3:col + 4], in_=sm[:, col + 2:col + 3]), [sm_t], [sm_t])
        kb.op("dve", lambda e: e.scalar_tensor_tensor(out=out_ap, in0=src_ap, scalar=sm[:, col + 3:col + 4], in1=ln_ap,
                                                      op0=ALU.mult, op1=ALU.mult), list(src_toks) + [sm_t, ln_tok], out_toks)

    mixtok, mixtok_t = kb.alloc_tok("mixtok", [128, 8, 2048], BF16, ntok=8)
    wpool_sb, wpool_t = kb.alloc_tok("wpool", [128, 4, 2, 256], BF16)
    for G in range(4):
        kb.dma("pool", out=wpool_sb[:, G], in_=wpool_d[G].rearrange("(k p) n -> p k n", p=128), writes=[wpool_t])
    bpool_sb, bpool_t = kb.alloc_tok("bpool", [128, 1024], F32)
    pscale_sb, pscale_t = kb.alloc_tok("pscale", [128, 1024], F32)
    gnpool_sb, gnpool_t = kb.alloc_tok("gnpool", [128, 1024], F32)
    gnnsa_sb, gnnsa_t = kb.alloc_tok("gnnsa", [128, 1024], F32)
    kb.dma("sp", out=bpool_sb[:], in_=bpool_d, writes=[bpool_t])
    kb.dma("sp", out=pscale_sb[:], in_=pscale_d, writes=[pscale_t])
    kb.dma("sp", out=gnpool_sb[:], in_=gnpool_d, writes=[gnpool_t])
    kb.dma("sp", out=gnnsa_sb[:], in_=gnnsa_d, writes=[gnnsa_t])
    opool, opool_t = kb.alloc_tok("opool", [128, 1024], F32)
    sm2, sm2_t = kb.alloc_tok("sm2", [128, 64], F32)
    for ot in range(8):
        pb0 = (ot % 2) * 2
        for G in range(4):
            bk = pb0 + G // 2
            for k2 in range(2):
                kb.op("pe", lambda e: e.matmul(out=psb[bk][:, (G % 2) * 256:(G % 2) * 256 + 256],
                                               lhsT=dT[:, 2 * G + k2, ot * 128:(ot + 1) * 128], rhs=wpool_sb[:, G, k2, :],
                                               start=(k2 == 0), stop=(k2 == 1)), [dT_t[2 * G + k2], wpool_t], [pst[bk]])
        for half in range(2):
            kb.op("dve", lambda e: e.tensor_tensor(out=opool[:, half * 512:(half + 1) * 512], in0=psb[pb0 + half][:, 0:512],
                                                   in1=bpool_sb[:, half * 512:(half + 1) * 512], op=ALU.add),
                  [pst[pb0 + half], bpool_t], [opool_t])
        kb.op("dve", lambda e: e.tensor_tensor(out=opool[:], in0=opool[:], in1=pscale_sb[:], op=ALU.mult), [opool_t, pscale_t], [opool_t])
        if ot == 0 and "opool" in tap_d:
            tap("opool", opool[:], [opool_t])
        rms(opool[:], [opool_t], 1024, gnpool_sb[:], gnpool_t, mixtok[:, ot, 1024:2048], [mixtok_t[ot]], sm2, sm2_t, 0)
    if stage <= 2.5:
        kb.finish()
        return nc
    kb.free(dT, dT_t)
    kb.free(bpool_sb, [bpool_t])
    kb.free(pscale_sb, [pscale_t])
    kb.free(wpool_sb, [wpool_t])
    cmpb_sb, cmpb_t = kb.alloc_tok("cmpb", [128, 1024], BF16)
    emat_sb, emat_t = kb.alloc_tok("emat", [128, 2048], BF16)
    causb_sb, causb_t = kb.alloc_tok("causb", [128, 4, 512], BF16)
    winb_sb, winb_t = kb.alloc_tok("winb", [128, 8, 512], BF16)
    amul_sb, amul_t = kb.alloc_tok("amul", [128, 8, 32], F32)
    aadd_sb, aadd_t = kb.alloc_tok("aadd", [128, 8, 32], F32)
    kb.dma("sp", out=cmpb_sb[:], in_=cmpbias_d, writes=[cmpb_t])
    kb.dma("sp", out=emat_sb[:], in_=emat_d, writes=[emat_t])
    kb.dma("sp", out=causb_sb[:], in_=causb_d, writes=[causb_t])
    kb.dma("sp", out=winb_sb[:], in_=winb_d, writes=[winb_t])
    kb.dma("sp", out=amul_sb[:], in_=amul_d, writes=[amul_t])
    kb.dma("sp", out=aadd_sb[:], in_=aadd_d, writes=[aadd_t])
    PT = []
    PT_t = []
    for i in range(3):
        t, tk = kb.alloc_tok("PT%d" % i, [128, 512], BF16)
        PT.append(t)
        PT_t.append(tk)
    ptpos = [0]
    selbT, selbT_t = kb.alloc_tok("selbT", [128, 2, 512], BF16, ntok=2)
    kb.op("dve", lambda e: e.memset(selbT[:], 0.0), [], selbT_t)
    oacc, oacc_t = kb.alloc_tok("oacc", [128, 4, 1024], F32, ntok=4)
    impsb, imp_t = kb.alloc_tok("impsb", [128, 4, 32], F32, ntok=4)
    impm, impm_t = kb.alloc_tok("impm", [128, 32], F32)
    imp2, imp2_t = kb.alloc_tok("imp2", [128, 32], F32)
    m8, m8_t = kb.alloc_tok("m8", [128, 16], F32)
    selb, selb_t = kb.alloc_tok("selb", [128, 32], BF16)
    sm3 = []
    sm3_t = []
    for i in range(4):
        t, tk = kb.alloc_tok("sm3_%d" % i, [128, 8], F32)
        sm3.append(t)
        sm3_t.append(tk)
    smpos = [0]

    def evac_multi(jobs):
        ss = []
        for _ in jobs:
            i = smpos[0]
            smpos[0] = (i + 1) % 4
            ss.append((sm3[i], sm3_t[i]))
        for (job, (s, st)) in zip(jobs, ss):
            kb.op("dve", lambda e: e.tensor_scalar(out=s[:, 0:1], in0=job[1], scalar1=1e-30, scalar2=None, op0=ALU.max), [job[2]], [st])
        for (job, (s, st)) in zip(jobs, ss):
            kb.op("dve", lambda e: e.reciprocal(out=s[:, 1:2], in_=s[:, 0:1]), [st], [st])
        for (job, (s, st)) in zip(jobs, ss):
            tile, gcol = job[4], job[6]
            kb.op("dve", lambda e: e.tensor_tensor(out=s[:, 2:3], in0=s[:, 1:2], in1=gate_sb[:, tile, gcol:gcol + 1], op=ALU.mult),
                  [st, gate_t], [st])
        for (job, (s, st)) in zip(jobs, ss):
            ps_ap_num, _, ps_tok, j, tile, head, gcol, first, imp_ap, imp_first = job
            dst = oacc[:, j, head * 128:(head + 1) * 128]
            if first:
                kb.op("dve", lambda e: e.tensor_scalar(out=dst, in0=ps_ap_num, scalar1=s[:, 2:3], scalar2=None, op0=ALU.mult),
                      [ps_tok, st], [oacc_t[j]])
            else:
                kb.op("dve", lambda e: e.scalar_tensor_tensor(out=dst, in0=ps_ap_num, scalar=s[:, 2:3], in1=dst, op0=ALU.mult,
                                                              op1=ALU.add), [ps_tok, st, oacc_t[j]], [oacc_t[j]])
        for (job, (s, st)) in zip(jobs, ss):
            ps_ap_num, _, ps_tok, j, tile, head, gcol, first, imp_ap, imp_first = job
            if imp_ap is not None:
                if imp_first:
                    kb.op("dve", lambda e: e.tensor_scalar(out=impsb[:, j, :], in0=imp_ap, scalar1=s[:, 1:2], scalar2=None, op0=ALU.mult),
                          [ps_tok, st], [imp_t[j]])
                else:
                    kb.op("dve", lambda e: e.scalar_tensor_tensor(out=impsb[:, j, :], in0=imp_ap, scalar=s[:, 1:2], in1=impsb[:, j, :],
                                                                  op0=ALU.mult, op1=ALU.add), [ps_tok, st, imp_t[j]], [imp_t[j]])

    def expo(bk, np_):
        i = ptpos[0]
        ptpos[0] = (i + 1) % 3
        kb.op("act", lambda e: e.activation(out=PT[i][0:np_, :], in_=psb[bk][0:np_, 0:512], func=AF.Exp, scale=SCALE),
              [pst[bk]], [PT_t[i]])
        return PT[i], PT_t[i]

    accpair = [0]
    for qc in range(2):
        qsl = slice(qc * 512, (qc + 1) * 512)
        qtoks = lambda head: [QT2_t[(head // 4) * 2 + qc]]
        for g in range(2):
            for r in range(4):
                head = 4 * g + r
                bk = nextps(0, 3)
                kb.op("pe", lambda e: e.matmul(out=psb[bk][0:127, 0:512], lhsT=kcmpT[:, g, 0:127], rhs=QT2[:, 0, head, qsl],
                                               start=True, stop=False), [kcmpT_t] + qtoks(head), [pst[bk]])
                kb.op("pe", lambda e: e.matmul(out=psb[bk][0:127, 0:512], lhsT=identb[0:127, 0:127], rhs=cmpb_sb[0:127, qsl],
                                               start=False, stop=True), [identb_t, cmpb_t], [pst[bk]])
                P, P_t = expo(bk, 127)
                jobs = []
                for j in range(4):
                    ob = nextps(3, 8)
                    kb.op("pe", lambda e: e.matmul(out=psb[ob][:, 0:161], lhsT=P[0:127, j * 128:(j + 1) * 128], rhs=Vc[0:127, g, 0:161],
                                                   start=True, stop=True), [P_t, Vc_t], [pst[ob]])
                    jobs.append((psb[ob][:, 0:128], psb[ob][:, 128:129], pst[ob], j, 4 * qc + j, head, 3 * head + 0, True,
                                 psb[ob][:, 129:161], r == 0))
                evac_multi(jobs)
            for j in range(4):
                tile = 4 * qc + j
                kb.op("dve", lambda e: e.tensor_tensor(out=impm[:], in0=impsb[:, j, :], in1=amul_sb[:, tile, :], op=ALU.mult),
                      [imp_t[j], amul_t], [impm_t])
                kb.op("dve", lambda e: e.tensor_tensor(out=impm[:], in0=impm[:], in1=aadd_sb[:, tile, :], op=ALU.add),
                      [impm_t, aadd_t], [impm_t])
                if "impm" in tap_d and g == 0 and j == 0 and qc == 0:
                    tap("impm", impm[:], [impm_t])
                kb.op("dve", lambda e: e.max(out=m8[:, 0:8], in_=impm[:]), [impm_t], [m8_t])
                kb.op("dve", lambda e: e.match_replace(out=imp2[:], in_to_replace=m8[:, 0:8], in_values=impm[:], imm_value=-3.0e38),
                      [m8_t, impm_t], [imp2_t])
                kb.op("dve", lambda e: e.max(out=m8[:, 8:16], in_=imp2[:]), [imp2_t], [m8_t])
                kb.op("dve", lambda e: e.tensor_scalar(out=m8[:, 15:16], in0=m8[:, 15:16], scalar1=0.0, scalar2=None, op0=ALU.max),
                      [m8_t], [m8_t])
                kb.op("dve", lambda e: e.tensor_scalar(out=imp2[:], in0=impm[:], scalar1=m8[:, 15:16], scalar2=None, op0=ALU.is_ge),
                      [impm_t, m8_t], [imp2_t])
                kb.op("dve", lambda e: e.tensor_scalar(out=selb[:], in0=imp2[:], scalar1=-1.0, scalar2=-NEGB, op0=ALU.add, op1=ALU.mult),
                      [imp2_t], [selb_t])
                if "selb" in tap_d and g == 0 and j == 0 and qc == 0:
                    tap("selb", imp2[:], [imp2_t])
                tb = 6
                pv = bfview(tb)
                kb.op("pe", lambda e: e.transpose(out=pv[0:32, 0:128], in_=selb[:, 0:32], identity=identb[:]), [selb_t, identb_t], [pst[tb]])
                kb.op("act", lambda e: e.copy(out=selbT[0:32, g, j * 128:(j + 1) * 128], in_=pv[0:32, 0:128]), [pst[tb]], [selbT_t[g]])
        if "oacc_cmp" in tap_d and qc == 0:
            tap("oacc_cmp", oacc[:].rearrange("p a q -> p (a q)"), oacc_t)
        if stage <= 2.7:
            kb.finish()
            return nc
        items = []
        for g in range(2):
            for r in range(4):
                head = 4 * g + r
                for br in range(2):
                    kts = list(range(12 + 4 * qc)) if br == 0 else [4 + 4 * qc + i for i in range(8)]
                    for ii, kt in enumerate(kts):
                        items.append((g, head, br, ii, kt, ii == 0, ii == len(kts) - 1))

        def emitS(it):
            g, head, br, ii, kt, _, _ = it
            KT, KT_t = (KsT, KsT_t) if br == 0 else (KwT, KwT_t)
            bk = nextps(0, 3)
            kb.op("pe", lambda e: e.matmul(out=psb[bk][:, 0:512], lhsT=KT[:, g, kt * 128:(kt + 1) * 128], rhs=QT2[:, 1, head, qsl],
                                           start=True, stop=False), [KT_t[kt]] + qtoks(head), [pst[bk]])
            if br == 0:
                diag = kt - (8 + 4 * qc)
                kb.op("pe", lambda e: e.matmul(out=psb[bk][:, 0:512], lhsT=emat_sb[:, kt * 128:(kt + 1) * 128],
                                               rhs=selbT[:, g, :], start=False, stop=(diag < 0)),
                      [emat_t, selbT_t[g]], [pst[bk]])
                if diag >= 0:
                    kb.op("pe", lambda e: e.matmul(out=psb[bk][:, 0:512], lhsT=identb[:], rhs=causb_sb[:, diag, :],
                                                   start=False, stop=True), [identb_t, causb_t], [pst[bk]])
            else:
                kb.op("pe", lambda e: e.matmul(out=psb[bk][:, 0:512], lhsT=identb[:], rhs=winb_sb[:, ii, :],
                                               start=False, stop=True), [identb_t, winb_t], [pst[bk]])
            return bk

        pendq = [emitS(items[0]), emitS(items[1])]
        accb = None
        for idx, it in enumerate(items):
            g, head, br, ii, kt, first_, last_ = it
            bk = pendq.pop(0)
            if idx + 2 < len(items):
                pendq.append(emitS(items[idx + 2]))
            if first_:
                accb = [nextps(3, 8) for _ in range(4)]
            V, V_t = (Vs, Vs_t) if br == 0 else (Vw, Vw_t)
            if br == 0:
                jr = [(j, kt == 0, kt == 8 + 4 * qc + j) for j in range(4) if kt <= 8 + 4 * qc + j]
            else:
                jr = [(j, ii == j, ii == j + 4) for j in range(4) if j <= ii <= j + 4]
            P, P_t = expo(bk, 128)
            for (j, st_, sp_) in jr:
                ab = accb[j]
                kb.op("pe", lambda e: e.matmul(out=psb[ab][:, 0:129], lhsT=P[:, j * 128:(j + 1) * 128],
                                               rhs=V[:, kt, g, 0:129], start=st_, stop=sp_), [P_t, V_t[kt]], [pst[ab]])
            if last_:
                evac_multi([(psb[accb[j]][:, 0:128], psb[accb[j]][:, 128:129], pst[accb[j]], j, 4 * qc + j, head, 3 * head + 1 + br,
                             False, None, False) for j in range(4)])
        if "oacc" in tap_d and qc == 0:
            tap("oacc", oacc[:].rearrange("p a q -> p (a q)"), oacc_t)
        for j in range(4):
            tile = 4 * qc + j
            rms(oacc[:, j, :], [oacc_t[j]], 1024, gnnsa_sb[:], gnnsa_t, mixtok[:, tile, 0:1024], [mixtok_t[tile]], sm2, sm2_t, 8)
    if "mixtok" in tap_d:
        tap("mixtok", mixtok[:].rearrange("p a q -> p (a q)"), mixtok_t)
    if stage <= 3:
        kb.finish()
        return nc

    for _t, _k in ((QT2, QT2_t), (KsT, KsT_t), (KwT, KwT_t), (Vs, Vs_t), (Vw, Vw_t), (kcmpT, [kcmpT_t]), (Vc, [Vc_t]),
                   (cmpb_sb, [cmpb_t]), (emat_sb, [emat_t]), (causb_sb, [causb_t]), (winb_sb, [winb_t]), (amul_sb, [amul_t]),
                   (aadd_sb, [aadd_t]), (PT[0], [PT_t[0]]), (PT[1], [PT_t[1]]), (PT[2], [PT_t[2]]), (selbT, selbT_t), (oacc, oacc_t), (impsb, imp_t),
                   (impm, [impm_t]), (imp2, [imp2_t]), (m8, [m8_t]), (selb, [selb_t]), (gate_sb, [gate_t]), (gnpool_sb, [gnpool_t]),
                   (gnnsa_sb, [gnnsa_t]), (opool, [opool_t]), (w2sb, [w2_t]), (peT, [peT_t]), (tkm, [tkm_t]), (tkm2, [tkm2_t]), (tkmB, [tkmB_t]), (tkm2B, [tkm2B_t]),
                   (cs4, [cs4_t]), (rt, [rt_t]), (invcnt, [invcnt_t])):
        kb.free(_t, _k)
    for i in range(4):
        kb.free(sm3[i], [sm3_t[i]])
    x1, x1_t = kb.alloc_tok("x1", [128, 8, D], F32, ntok=8)
    for ot in range(8):
        kb.dma("sp", out=x1[:, ot, :], in_=xc[1024 + ot * 128:1024 + (ot + 1) * 128, :], writes=[x1_t[ot]])
    mixT, mixT_t = kb.alloc_tok("mixT", [128, 16, 1024], BF16, ntok=8)
    ring = []
    ring_t = []
    NSLOT = 3
    for i in range(NSLOT):
        t, tk = kb.alloc_tok("ringb%d" % i, [128, 8192], BF16)
        ring.append(t)
        ring_t.append(tk)
    rp2 = [0]

    def ring_load2(src_ap, nslot):
        i = rp2[0]
        rp2[0] = (i + 1) % nslot
        a, b = src_ap.shape[1], src_ap.shape[2]
        view = ring[i][:, 0:a * b].rearrange("p (a b) -> p a b", a=a)
        kb.dma("pool", out=view, in_=src_ap, writes=[ring_t[i]])
        return view, ring_t[i]

    for ot in range(8):
        transpose16(mixtok[:, ot, :], mixtok_t[ot], lambda half: mixT[:, half * 8:(half + 1) * 8, ot * 128:(ot + 1) * 128], [mixT_t[ot]])
    for oc in range(4):
        wv, wtk = ring_load2(wout_d[:, oc * 512:(oc + 1) * 512].rearrange("(kc p) n -> p kc n", p=128), 3)
        for ot in range(8):
            bk = nextps(0, 4)
            for kc in range(16):
                kb.op("pe", lambda e: e.matmul(out=psb[bk][:, 0:512], lhsT=mixT[:, kc, ot * 128:(ot + 1) * 128], rhs=wv[:, kc, :],
                                               start=(kc == 0), stop=(kc == 15)), [mixT_t[ot], wtk], [pst[bk]])
            kb.op("dve", lambda e: e.tensor_tensor(out=x1[:, ot, oc * 512:(oc + 1) * 512], in0=psb[bk][:, 0:512],
                                                   in1=x1[:, ot, oc * 512:(oc + 1) * 512], op=ALU.add), [pst[bk], x1_t[ot]], [x1_t[ot]])
    if "x1" in tap_d:
        tap("x1", x1[:].rearrange("p a q -> p (a q)"), x1_t)
    if stage <= 4:
        kb.finish()
        return nc
    kb.free(mixtok, mixtok_t)
    kb.free(mixT, mixT_t)
    for i in range(3):
        kb.free(ring[i], [ring_t[i]])
    h2T, h2T_t = kb.alloc_tok("h2T", [128, 16, 1024], BF16, ntok=8)
    ring = []
    ring_t = []
    for i in range(4):
        t, tk = kb.alloc_tok("ringc%d" % i, [128, 8192], BF16)
        ring.append(t)
        ring_t.append(tk)
    rp2[0] = 0
    moe_pref = []
    if stage >= 7:
        moe_pref.append(ring_load2(wg_d[0].rearrange("(kc p) n -> p kc n", p=128), 4))
        moe_pref.append(ring_load2(wu_d[0].rearrange("(kc p) n -> p kc n", p=128), 4))
        moe_pref.append(ring_load2(wd_d[0].rearrange("(fc p) n -> p fc n", p=128), 4))
    lnmoe_sb, lnmoe_t = kb.alloc_tok("lnmoe", [128, D], F32)
    kb.dma("sp", out=lnmoe_sb[:], in_=lnmoe_d, writes=[lnmoe_t])
    wr_sb, wr_t = kb.alloc_tok("wr", [128, 16, 36], BF16)
    kb.dma("pool", out=wr_sb[:], in_=wr_d.rearrange("(kc p) n -> p kc n", p=128), writes=[wr_t])
    br_sb, br_t = kb.alloc_tok("br", [128, 36], F32)
    kb.dma("sp", out=br_sb[:], in_=br_d, writes=[br_t])
    gmat, gmat_t = kb.alloc_tok("gmat", [128, 8, 32], F32)
    lgb, lgb_t = kb.alloc_tok("lgb", [128, 36], F32)
    lem, lem_t = kb.alloc_tok("lem", [128, 32], F32)
    rs, rs_t = kb.alloc_tok("rs", [128, 32], F32)
    g1b, g1b_t = kb.alloc_tok("g1b", [128, 32], F32)
    mr8, mr8_t = kb.alloc_tok("mr8", [128, 8], F32)
    hbC, hbC_t = kb.alloc_tok("hbC", [128, D], BF16)
    hb2 = [(hb, hb_t), (hbC, hbC_t)]
    sm4, sm4_t = kb.alloc_tok("sm4", [128, 8, 4], F32, ntok=8)

    lgbA, lgbA_t = kb.alloc_tok("lgbA", [128, 8, 36], F32, ntok=8)
    def router_front(ot):
        hbx, hbx_t = hb2[ot % 2]
        rms(x1[:, ot, :], [x1_t[ot]], D, lnmoe_sb[:], lnmoe_t, hbx[:], [hbx_t], sm4[:, ot, :], sm4_t[ot], 0)
        transpose16(hbx, hbx_t, lambda half: h2T[:, half * 8:(half + 1) * 8, ot * 128:(ot + 1) * 128], [h2T_t[ot]],
                    banks=(4 + 2 * (ot % 2), 5 + 2 * (ot % 2)))
        bk = nextps(0, 4)
        for kc in range(16):
            kb.op("pe", lambda e: e.matmul(out=psb[bk][:, 0:36], lhsT=h2T[:, kc, ot * 128:(ot + 1) * 128], rhs=wr_sb[:, kc, :],
                                           start=(kc == 0), stop=(kc == 15)), [h2T_t[ot], wr_t], [pst[bk]])
        kb.op("dve", lambda e: e.tensor_tensor(out=lgbA[:, ot, :], in0=psb[bk][:, 0:36], in1=br_sb[:], op=ALU.add),
              [pst[bk], br_t], [lgbA_t[ot]])
        return bk

    lemA, lemA_t = kb.alloc_tok("lemA", [128, 8, 32], F32, ntok=8)
    rsA, rsA_t = kb.alloc_tok("rsA", [128, 8, 32], F32, ntok=8)
    g1A, g1A_t = kb.alloc_tok("g1A", [128, 8, 32], F32, ntok=8)
    mrA, mrA_t = kb.alloc_tok("mrA", [128, 8, 8], F32, ntok=8)
    gmat_tt = [Tok() for _ in range(8)]
    rbanks = [router_front(ot) for ot in range(8)]
    T8 = range(8)
    for ot in T8:
        kb.op("dve", lambda e: e.tensor_reduce(out=rsA[:, ot, 0:1], in_=lgbA[:, ot, 0:4], axis=AX.X, op=ALU.max), [lgbA_t[ot]], [rsA_t[ot]])
    for ot in T8:
        kb.op("dve", lambda e: e.tensor_scalar(out=rsA[:, ot, 4:8], in0=lgbA[:, ot, 0:4], scalar1=rsA[:, ot, 0:1], scalar2=None,
                                               op0=ALU.is_ge), [lgbA_t[ot], rsA_t[ot]], [rsA_t[ot]])
    for ot in T8:
        kb.op("dve", lambda e: e.tensor_scalar(out=rsA[:, ot, 1:2], in0=rsA[:, ot, 0:1], scalar1=-1.0, scalar2=None, op0=ALU.mult),
              [rsA_t[ot]], [rsA_t[ot]])
    for ot in T8:
        kb.op("act", lambda e: e.activation(out=rsA[:, ot, 8:12], in_=lgbA[:, ot, 0:4], func=AF.Exp, bias=rsA[:, ot, 1:2], scale=1.0,
                                            accum_out=rsA[:, ot, 2:3]), [lgbA_t[ot], rsA_t[ot]], [rsA_t[ot]], guard=True)
    for ot in T8:
        kb.op("dve", lambda e: e.reciprocal(out=rsA[:, ot, 3:4], in_=rsA[:, ot, 2:3]), [rsA_t[ot]], [rsA_t[ot]])
    for ot in T8:
        kb.op("dve", lambda e: e.tensor_scalar(out=rsA[:, ot, 12:16], in0=rsA[:, ot, 4:8], scalar1=-1.0, scalar2=BIG, op0=ALU.add,
                                               op1=ALU.mult), [rsA_t[ot]], [rsA_t[ot]])
    for g in range(4):
        for ot in T8:
            kb.op("dve", lambda e: e.tensor_scalar(out=lemA[:, ot, g * 8:(g + 1) * 8], in0=lgbA[:, ot, 4 + 8 * g:12 + 8 * g],
                                                   scalar1=rsA[:, ot, 12 + g:13 + g], scalar2=None, op0=ALU.add),
                  [lgbA_t[ot], rsA_t[ot]], [lemA_t[ot]])
    for ot in T8:
        kb.op("dve", lambda e: e.max(out=mrA[:, ot, :], in_=lemA[:, ot, :]), [lemA_t[ot]], [mrA_t[ot]])
    for ot in T8:
        kb.op("dve", lambda e: e.tensor_tensor(out=rsA[:, ot, 16:17], in0=mrA[:, ot, 1:2], in1=mrA[:, ot, 0:1], op=ALU.subtract),
              [mrA_t[ot]], [rsA_t[ot]])
    for ot in T8:
        kb.op("act", lambda e: e.activation(out=rsA[:, ot, 17:18], in_=rsA[:, ot, 16:17], func=AF.Exp), [rsA_t[ot]], [rsA_t[ot]])
    for ot in T8:
        kb.op("dve", lambda e: e.tensor_scalar(out=rsA[:, ot, 18:19], in0=rsA[:, ot, 17:18], scalar1=1.0, scalar2=None, op0=ALU.add),
              [rsA_t[ot]], [rsA_t[ot]])
    for ot in T8:
        kb.op("dve", lambda e: e.reciprocal(out=rsA[:, ot, 19:20], in_=rsA[:, ot, 18:19]), [rsA_t[ot]], [rsA_t[ot]])
    for ot in T8:
        kb.op("dve", lambda e: e.tensor_tensor(out=rsA[:, ot, 20:21], in0=rsA[:, ot, 19:20], in1=rsA[:, ot, 3:4], op=ALU.mult),
              [rsA_t[ot]], [rsA_t[ot]])
    for ot in T8:
        kb.op("dve", lambda e: e.tensor_tensor(out=rsA[:, ot, 21:22], in0=rsA[:, ot, 3:4], in1=rsA[:, ot, 20:21], op=ALU.subtract),
              [rsA_t[ot]], [rsA_t[ot]])
    for ot in T8:
        kb.op("dve", lambda e: e.tensor_scalar(out=g1A[:, ot, :], in0=lemA[:, ot, :], scalar1=mrA[:, ot, 0:1], scalar2=rsA[:, ot, 20:21],
                                               op0=ALU.is_equal, op1=ALU.mult), [lemA_t[ot], mrA_t[ot], rsA_t[ot]], [g1A_t[ot]])
    for ot in T8:
        kb.op("dve", lambda e: e.tensor_scalar(out=gmat[:, ot, :], in0=lemA[:, ot, :], scalar1=mrA[:, ot, 1:2], scalar2=rsA[:, ot, 21:22],
                                               op0=ALU.is_equal, op1=ALU.mult), [lemA_t[ot], mrA_t[ot], rsA_t[ot]], [gmat_tt[ot]])
    for ot in T8:
        kb.op("dve", lambda e: e.tensor_tensor(out=gmat[:, ot, :], in0=gmat[:, ot, :], in1=g1A[:, ot, :], op=ALU.add),
              [gmat_tt[ot], g1A_t[ot]], [gmat_tt[ot]])
    gmat_t.w = gmat_tt[7].w
    if "gmat" in tap_d:
        tap("gmat", gmat[:].rearrange("p a q -> p (a q)"), [gmat_t])
    if stage <= 5:
        kb.finish()
        return nc
    kb.free(lnmoe_sb, [lnmoe_t])
    kb.free(hbC, [hbC_t])
    kb.free(sm4, sm4_t)
    kb.free(lgbA, lgbA_t)
    kb.free(lemA, lemA_t)
    kb.free(rsA, rsA_t)
    kb.free(g1A, g1A_t)
    kb.free(mrA, mrA_t)
    AT, AT_t = kb.alloc_tok("AT", [128, 4, 1024], BF16, ntok=8)
    sgt = []
    sgt_t = []
    for i in range(2):
        t, tk = kb.alloc_tok("sg%d" % i, [128, 512], F32)
        sgt.append(t)
        sgt_t.append(tk)
    sgp = 0
    nexp = NEXP if stage >= 7 else 0
    for ex in range(nexp):
        if ex == 0:
            (wgv, wg_t), (wuv, wu_t), (wdv, wd_t) = moe_pref
        else:
            wgv, wg_t = ring_load2(wg_d[ex].rearrange("(kc p) n -> p kc n", p=128), 4)
            wuv, wu_t = ring_load2(wu_d[ex].rearrange("(kc p) n -> p kc n", p=128), 4)
            wdv, wd_t = ring_load2(wd_d[ex].rearrange("(fc p) n -> p fc n", p=128), 4)
        for fc in range(4):
            for tq in range(2):
                bg = nextps(0, 4)
                bu = nextps(0, 4)
                for (bk_, wv_, wt_) in ((bg, wgv, wg_t), (bu, wuv, wu_t)):
                    for kc in range(16):
                        kb.op("pe", lambda e: e.matmul(out=psb[bk_][:, 0:512], lhsT=wv_[:, kc, fc * 128:(fc + 1) * 128],
                                                       rhs=h2T[:, kc, tq * 512:(tq + 1) * 512], start=(kc == 0), stop=(kc == 15)),
                              h2T_t[4 * tq:4 * tq + 4] + [wt_], [pst[bk_]])
                sg, sg_t = sgt[sgp], sgt_t[sgp]
                sgp ^= 1
                kb.op("act", lambda e: e.activation(out=sg[:], in_=psb[bg][:, 0:512], func=AF.Silu), [pst[bg]], [sg_t])
                kb.op("dve", lambda e: e.tensor_tensor(out=AT[:, fc, tq * 512:(tq + 1) * 512], in0=sg[:], in1=psb[bu][:, 0:512],
                                                       op=ALU.mult), [sg_t, pst[bu]], [AT_t[fc * 2 + tq]])
        for ot in range(8):
            for dc in range(4):
                by = nextps(4, 8)
                for fc in range(4):
                    kb.op("pe", lambda e: e.matmul(out=psb[by][:, 0:512], lhsT=AT[:, fc, ot * 128:(ot + 1) * 128],
                                                   rhs=wdv[:, fc, dc * 512:(dc + 1) * 512], start=(fc == 0), stop=(fc == 3)),
                          [AT_t[fc * 2 + ot // 4], wd_t], [pst[by]])
                kb.op("dve", lambda e: e.scalar_tensor_tensor(out=x1[:, ot, dc * 512:(dc + 1) * 512], in0=psb[by][:, 0:512],
                                                              scalar=gmat[:, ot, ex:ex + 1], in1=x1[:, ot, dc * 512:(dc + 1) * 512],
                                                              op0=ALU.mult, op1=ALU.add), [pst[by], gmat_t, x1_t[ot]], [x1_t[ot]])
    for i in range(4):
        kb.free(ring[i], [ring_t[i]])
    kb.free(AT, AT_t)
    kb.free(h2T, h2T_t)
    lnfin_sb, lnfin_t = kb.alloc_tok("lnfin", [128, D], F32)
    kb.dma("sp", out=lnfin_sb[:], in_=lnfin_d, writes=[lnfin_t])
    of = []
    of_t = []
    for i in range(2):
        t, tk = kb.alloc_tok("of%d" % i, [128, D], F32)
        of.append(t)
        of_t.append(tk)
    for ot in range(8):
        rms(x1[:, ot, :], [x1_t[ot]], D, lnfin_sb[:], lnfin_t, of[ot % 2][:], [of_t[ot % 2]], sm2, sm2_t, 24)
        kb.dma("sp", out=out_d[ot * 128:(ot + 1) * 128, :], in_=of[ot % 2][:], reads=[of_t[ot % 2]])
    kb.finish()
    return nc


def _bf(a):
    return np.ascontiguousarray(a.astype(ml_dtypes.bfloat16))


def _rep(v, n=128):
    return np.ascontiguousarray(np.broadcast_to(np.asarray(v, np.float32).reshape(1, -1), (n, v.size)))


def const_tables(h):
    off = -1024 + 1024 * h
    t = {}
    c = np.arange(2048)
    t["valid"] = np.ascontiguousarray(((c + off) >= 0).astype(np.float32).reshape(16, 128).T)
    t["identb"] = _bf(np.eye(128, dtype=np.float32))
    t["identf"] = np.eye(128, dtype=np.float32)
    inv = (500000.0 ** (-np.arange(0, 32, 2, dtype=np.float32) / np.float32(32))).astype(np.float32)
    t["invf"] = _rep(inv)
    j = np.arange(128)[:, None]
    cq = 1024 + np.arange(1024)[None, :]
    ok = (16 * j + off >= 0) & (16 * j + 31 <= cq) & (j < 127)
    t["cmpbias"] = _bf(np.where(ok, 0.0, NEGB).astype(np.float32))
    cs = np.arange(128)[:, None] * 16
    ss = np.arange(32)[None, :] * 64
    ov = np.clip(np.minimum(cs + 32, ss + 64) - np.maximum(cs, ss), 0, None)
    t["wcs"] = _bf((ov / 32.0).astype(np.float32))
    key = np.arange(2048)[None, :]
    t["emat"] = _bf((key // 64 == np.arange(128)[:, None]).astype(np.float32))
    k = np.arange(128)[:, None, None]
    q = np.arange(512)[None, None, :]
    i4 = np.arange(4)[None, :, None]
    t["causb"] = _bf(np.where(128 * i4 + k <= q, 0.0, NEGB).astype(np.float32))
    i8 = np.arange(8)[None, :, None]
    kk = 128 * i8 + k
    t["winb"] = _bf(np.where((kk > q) & (kk <= q + 512), 0.0, NEGB).astype(np.float32))
    tq = (1024 + np.arange(1024) + off)[:, None]
    jg = np.arange(32)[None, :] + off // 64
    okb = (jg >= 0) & (jg * 64 <= tq)
    cur = tq // 64
    forced = (jg == 0) | ((cur - jg >= 0) & (cur - jg < 2))
    amul = (okb & ~forced).astype(np.float32)
    aadd = np.where(okb & forced, BIG, np.where(okb, 0.0, -BIG)).astype(np.float32)
    t["amul"] = np.ascontiguousarray(amul.reshape(8, 128, 32).transpose(1, 0, 2))
    t["aadd"] = np.ascontiguousarray(aadd.reshape(8, 128, 32).transpose(1, 0, 2))
    tg = 1024 + np.arange(16) + off
    ic = np.stack([1.0 / np.minimum(tg + 1, w) for w in POOL_SIZES], 0).astype(np.float32)
    t["invcnt"] = np.ascontiguousarray(np.broadcast_to(ic[None], (128, 4, 16)))
    return t


def prep_inputs(inp, stage=99):
    L = 0
    shared = {}
    shared["w_in"] = np.ascontiguousarray(inp["w_in"][L])
    shared["lnmix"] = _rep(inp["ln_mix"][L])
    shared["pekT"] = np.ascontiguousarray(inp["pe_cmp_k"][L].T)
    shared["pevT"] = np.ascontiguousarray(inp["pe_cmp_v"][L].T)
    shared["w1k"] = np.ascontiguousarray(inp["w_cmp_k1"][L])
    shared["w1v"] = np.ascontiguousarray(inp["w_cmp_v1"][L])
    shared["w2k"] = np.ascontiguousarray(inp["w_cmp_k2"][L])
    shared["w2v"] = np.ascontiguousarray(inp["w_cmp_v2"][L])
    shared["wpool"] = np.ascontiguousarray(inp["w_pool"][L])
    shared["bpool"] = _rep(inp["b_pool"][L])
    shared["pscale"] = _rep(inp["pool_scale"][L])
    shared["gnnsa"] = _rep(inp["gn_nsa"][L])
    shared["gnpool"] = _rep(inp["gn_pool"][L])
    shared["wout"] = np.ascontiguousarray(inp["w_out"][L])
    shared["lnmoe"] = _rep(inp["ln_moe"][L])
    wr = np.concatenate([inp["w_router_group"][L]] + [inp["w_router_expert"][L][g] for g in range(4)], axis=1)
    shared["wr"] = np.ascontiguousarray(wr.astype(np.float32))
    br = np.concatenate([inp["b_router_group"][L].reshape(-1), inp["b_router_expert"][L].reshape(-1)])
    shared["br"] = _rep(br)
    if stage >= 7:
        shared["wg"] = np.ascontiguousarray(inp["w_gate"][L])
        shared["wu"] = np.ascontiguousarray(inp["w_up"][L])
        shared["wd"] = np.ascontiguousarray(inp["w_down"][L])
    shared["lnfin"] = _rep(inp["ln_final"])
    tabs = [const_tables(0), const_tables(1)]
    x = np.asarray(inp["x"], np.float32)
    pos = np.asarray(inp["positions"], np.int32)
    maps = []
    for c in range(8):
        b, h = c // 2, c % 2
        m = dict(shared)
        m.update(tabs[h])
        if h == 0:
            xcx = np.concatenate([np.zeros((1024, D), np.float32), x[b, 0:1024]], 0)
            pc = np.concatenate([np.zeros((1024,), np.int32), pos[b, 0:1024]], 0)
        else:
            xcx = x[b]
            pc = pos[b]
        m["xc"] = np.ascontiguousarray(xcx)
        m["posT"] = np.ascontiguousarray(pc.reshape(16, 128).T.astype(np.int32))
        maps.append(m)
    return maps


_NC_CACHE = {}


def kernel(**inputs):
    inp = {k: np.asarray(v) for k, v in inputs.items()}
    if "nc" not in _NC_CACHE:
        import os
        _NC_CACHE["nc"] = build(99, poison=bool(os.environ.get("KPOISON")))
    nc = _NC_CACHE["nc"]
    maps = prep_inputs(inp, 99)
    res = run_bass_kernel_spmd(nc, maps, core_ids=list(range(8)))
    out = np.zeros((NB, S, D), np.float32)
    for c in range(8):
        b, h = c // 2, c % 2
        out[b, h * 1024:(h + 1) * 1024] = res.results[c]["out"]
    return out
```

```python
import numpy as np
import ml_dtypes
import concourse.bass as bass
import concourse.mybir as mybir
from concourse.bass_utils import run_bass_kernel_spmd

F32 = mybir.dt.float32
BF16 = mybir.dt.bfloat16
I32 = mybir.dt.int32
AF = mybir.ActivationFunctionType
ALU = mybir.AluOpType
AX = mybir.AxisListType

D = 2048
S = 2048
NB = 4
HD = 128
NH = 8
EPS = 1e-6
SCALE = HD ** -0.5
NEGB = -30000.0
BIG = 1e30
NEXP = 32
DFF = 512
POOL_SIZES = (2, 4, 8, 16)
DT_SIZE = {F32: 4, BF16: 2, I32: 4}


class Tok:
    __slots__ = ("w", "r")

    def __init__(self, fence=None):
        self.w = None
        self.r = dict(fence) if fence else {}


class KB:
    def __init__(self, nc):
        self.nc = nc
        self.eng = dict(pe=nc.tensor, dve=nc.vector, act=nc.scalar, pool=nc.gpsimd, sp=nc.sync)
        self.sems = {}
        self.cnt = {}
        for k in self.eng:
            self.sems[k] = nc.alloc_semaphore("p_" + k)
            self.cnt[k] = 0
        self.waited = {k: {} for k in self.eng}
        self.dring = {}
        self.dpos = {}
        for q, n in (("sp", 12), ("pool", 12), ("act", 4)):
            ks = []
            for i in range(n):
                key = "d_%s%d" % (q, i)
                self.sems[key] = nc.alloc_semaphore(key)
                self.cnt[key] = 0
                ks.append(key)
            self.dring[q] = ks
            self.dpos[q] = 0
        self.base = (nc.sbuf_base + 63) // 64 * 64
        self.top = nc.sbuf_top
        self.live = []
        self.fences = []
        self.nalloc = 0
        self.ents = {}

    def alloc_tok(self, name, shape, dtype, ntok=1):
        n = 1
        for s in shape[1:]:
            n *= s
        size = (n * DT_SIZE[dtype] + 63) // 64 * 64
        off = self.base
        for (o, s_, _) in sorted(self.live):
            if off + size <= o:
                break
            off = max(off, o + s_)
        if off + size > self.top:
            raise RuntimeError("SBUF overflow allocating %s (%d bytes) live=%s" % (name, size, sorted(self.live)))
        self.nalloc += 1
        ent = (off, size, "%s_%d" % (name, self.nalloc))
        self.live.append(ent)
        t = self.nc.alloc_sbuf_tensor_at(ent[2], list(shape), dtype, offset=off)
        fence = {}
        for (o, s_, deps) in self.fences:
            if o < off + size and off < o + s_:
                for k, v in deps.items():
                    if fence.get(k, 0) < v:
                        fence[k] = v
        toks = [Tok(fence) for _ in range(ntok)]
        self.ents[id(t)] = (ent, t)
        return (t, toks[0]) if ntok == 1 else (t, toks)

    def free(self, t, toks):
        ent, _ = self.ents.pop(id(t))
        self.live.remove(ent)
        deps = {}
        for tk in toks:
            if tk.w:
                k, v = tk.w
                if deps.get(k, 0) < v:
                    deps[k] = v
            for k, v in tk.r.items():
                if deps.get(k, 0) < v:
                    deps[k] = v
        self.fences.append((ent[0], ent[1], deps))

    def _deps(self, reads, writes):
        d = {}
        for b in reads:
            if b.w:
                k, v = b.w
                if d.get(k, 0) < v:
                    d[k] = v
        for b in writes:
            if b.w:
                k, v = b.w
                if d.get(k, 0) < v:
                    d[k] = v
            for k, v in b.r.items():
                if d.get(k, 0) < v:
                    d[k] = v
        return d

    def _wait(self, X, deps):
        w = self.waited[X]
        for key, val in deps.items():
            if val <= 0:
                continue
            if key == X and X == "pe":
                continue
            if w.get(key, 0) >= val:
                continue
            self.eng[X].wait_ge(self.sems[key], val)
            w[key] = val

    def op(self, X, fn, reads=(), writes=(), guard=False):
        self._wait(X, self._deps(reads, writes))
        inst = fn(self.eng[X])
        self.cnt[X] += 1
        inst.then_inc(self.sems[X], 1)
        if guard:
            self.eng[X].wait_ge(self.sems[X], self.cnt[X])
            self.waited[X][X] = self.cnt[X]
            g = self.gbuf[X]
            if X == "act":
                inst2 = self.eng[X].copy(out=g[:, 0:1], in_=g[:, 1:2])
            else:
                inst2 = self.eng[X].tensor_copy(out=g[:, 0:1], in_=g[:, 1:2])
            self.cnt[X] += 1
            inst2.then_inc(self.sems[X], 1)
        c = self.cnt[X]
        for b in reads:
            if b.r.get(X, 0) < c:
                b.r[X] = c
        for b in writes:
            b.w = (X, c)
            b.r = {}
        return inst

    def dma(self, Q, out, in_, reads=(), writes=()):
        ring = self.dring[Q]
        i = self.dpos[Q]
        self.dpos[Q] = (i + 1) % len(ring)
        key = ring[i]
        deps = self._deps(reads, writes)
        if self.cnt[key] > 0:
            deps[key] = max(deps.get(key, 0), self.cnt[key])
        self._wait(Q, deps)
        inst = self.eng[Q].dma_start(out=out, in_=in_)
        self.cnt[key] += 16
        inst.then_inc(self.sems[key], 16)
        c = self.cnt[key]
        for b in reads:
            b.r[key] = c
        for b in writes:
            b.w = (key, c)
            b.r = {}
        return inst

    def finish(self):
        deps = {}
        for k, v in self.cnt.items():
            if v > 0:
                deps[k] = v
        self._wait("sp", deps)


def build(stage=99, taps=(), poison=False):
    nc = bass.Bass("TRN2", target_bir_lowering=False)
    kb = KB(nc)
    if poison:
        nel = (kb.top - kb.base) // 4
        parena = nc.alloc_sbuf_tensor_at("poison_arena", [128, nel], F32, offset=kb.base)
        pt_ = Tok()
        kb.op("dve", lambda e: e.memset(parena[:], float("nan")), [], [pt_])
        for X in ("pe", "act", "pool", "sp"):
            kb._wait(X, {"dve": 1})

    def din(name, shape, dt=F32):
        return nc.dram_tensor(name, list(shape), dt, kind="ExternalInput").ap()

    def dout(name, shape, dt=F32):
        return nc.dram_tensor(name, list(shape), dt, kind="ExternalOutput").ap()

    xc = din("xc", [2048, D])
    w_in = din("w_in", [D, 3608])
    posT_d = din("posT", [128, 16], I32)
    invf_d = din("invf", [128, 16])
    valid_d = din("valid", [128, 16])
    identb_d = din("identb", [128, 128], BF16)
    identf_d = din("identf", [128, 128])
    lnmix_d = din("lnmix", [128, D])
    pekT_d = din("pekT", [128, 32])
    pevT_d = din("pevT", [128, 32])
    w1k_d = din("w1k", [4096, 256])
    w1v_d = din("w1v", [4096, 256])
    w2k_d = din("w2k", [256, 128])
    w2v_d = din("w2v", [256, 128])
    cmpbias_d = din("cmpbias", [128, 1024], BF16)
    wcs_d = din("wcs", [128, 32], BF16)
    emat_d = din("emat", [128, 2048], BF16)
    causb_d = din("causb", [128, 4, 512], BF16)
    winb_d = din("winb", [128, 8, 512], BF16)
    amul_d = din("amul", [128, 8, 32])
    aadd_d = din("aadd", [128, 8, 32])
    invcnt_d = din("invcnt", [128, 4, 16])
    wpool_d = din("wpool", [4, 256, 256])
    bpool_d = din("bpool", [128, 1024])
    pscale_d = din("pscale", [128, 1024])
    gnnsa_d = din("gnnsa", [128, 1024])
    gnpool_d = din("gnpool", [128, 1024])
    wout_d = din("wout", [D, D])
    lnmoe_d = din("lnmoe", [128, D])
    wr_d = din("wr", [D, 36])
    br_d = din("br", [128, 36])
    if stage >= 7:
        wg_d = din("wg", [NEXP, D, DFF])
        wu_d = din("wu", [NEXP, D, DFF])
        wd_d = din("wd", [NEXP, DFF, D])
    lnfin_d = din("lnfin", [128, D])
    out_d = dout("out", [1024, D])
    tap_d = {}
    for (nm, shp, tdt) in taps:
        tap_d[nm] = dout("tap_" + nm, shp, tdt)

    def tap(nm, src_ap, toks, dst=None):
        if nm in tap_d:
            kb.dma("sp", out=(tap_d[nm] if dst is None else dst), in_=src_ap, reads=toks)

    gba_, gba_t_ = kb.alloc_tok("gbufa", [128, 2], F32)
    gbd_, gbd_t_ = kb.alloc_tok("gbufd", [128, 2], F32)
    kb.gbuf = {"act": gba_, "dve": gbd_}
    kb.op("dve", lambda e: e.memset(gba_[:], 0.0), [], [gba_t_])
    kb.op("dve", lambda e: e.memset(gbd_[:], 0.0), [], [gbd_t_])
    kb._wait("act", {"dve": kb.cnt["dve"]})
    kb._wait("dve", {"dve": kb.cnt["dve"]})
    psb = [nc.alloc_psum_tensor("ps%d" % i, [128, 512], F32) for i in range(8)]
    pst = [Tok() for _ in range(8)]

    identb, identb_t = kb.alloc_tok("identb", [128, 128], BF16)
    identf, identf_t = kb.alloc_tok("identf", [128, 128], F32)
    kb.dma("sp", out=identb[:], in_=identb_d, writes=[identb_t])
    kb.dma("sp", out=identf[:], in_=identf_d, writes=[identf_t])
    small, small_t = kb.alloc_tok("small", [128, 256], F32)
    valid_sb, valid_t = kb.alloc_tok("valid", [128, 16], F32)
    kb.dma("sp", out=valid_sb[:], in_=valid_d, writes=[valid_t])

    cs4, cs4_t = kb.alloc_tok("cs4", [128, 2, 16, 4, 16], F32)
    rt, rt_t = kb.alloc_tok("ropetmp", [128, 4, 4, 16], F32)

    def rope(ps_ap, H, ti, out_ap, ps_tok, out_tok):
        sin = cs4[:, 0, ti, 0:H, :]
        cos = cs4[:, 1, ti, 0:H, :]
        x1 = ps_ap[:, :, 0:16]
        x2 = ps_ap[:, :, 16:32]
        kb.op("dve", lambda e: e.tensor_tensor(out=rt[:, 0, 0:H, :], in0=x1, in1=cos, op=ALU.mult), [ps_tok, cs4_t], [rt_t])
        kb.op("dve", lambda e: e.tensor_tensor(out=rt[:, 1, 0:H, :], in0=x2, in1=sin, op=ALU.mult), [ps_tok, cs4_t], [rt_t])
        kb.op("dve", lambda e: e.tensor_tensor(out=rt[:, 2, 0:H, :], in0=x2, in1=cos, op=ALU.mult), [ps_tok, cs4_t], [rt_t])
        kb.op("dve", lambda e: e.tensor_tensor(out=rt[:, 3, 0:H, :], in0=x1, in1=sin, op=ALU.mult), [ps_tok, cs4_t], [rt_t])
        kb.op("dve", lambda e: e.tensor_tensor(out=out_ap[:, :, 0:16], in0=rt[:, 0, 0:H, :], in1=rt[:, 1, 0:H, :],
                                               op=ALU.subtract), [rt_t], [out_tok])
        kb.op("dve", lambda e: e.tensor_tensor(out=out_ap[:, :, 16:32], in0=rt[:, 2, 0:H, :], in1=rt[:, 3, 0:H, :],
                                               op=ALU.add), [rt_t], [out_tok])
        kb.op("act", lambda e: e.copy(out=out_ap[:, :, 32:128], in_=ps_ap[:, :, 32:128]), [ps_tok], [out_tok])

    NSLOT = 3
    ring = []
    ring_t = []
    for i in range(NSLOT):
        t, tk = kb.alloc_tok("ring%d" % i, [128, 8192], BF16)
        ring.append(t)
        ring_t.append(tk)
    ring_pos = [0]

    def ring_load(src_ap, shape_str, **kw):
        i = ring_pos[0]
        ring_pos[0] = (i + 1) % NSLOT
        a, b = src_ap.shape[1], src_ap.shape[2]
        view = ring[i][:, 0:a * b].rearrange("p (a b) -> p a b", a=a)
        kb.dma("pool", out=view, in_=src_ap, writes=[ring_t[i]])
        return view, ring_t[i]

    wkv = []
    for cg in range(3):
        v, tk = ring_load(w_in[:, 1024 + cg * 512:1024 + (cg + 1) * 512].rearrange("(kc p) n -> p kc n", p=128), "")
        wkv.append((v, tk))
    junk, junk_t = kb.alloc_tok("junk", [128, D], BF16)
    hb, hb_t = kb.alloc_tok("hb", [128, D], BF16)
    KsT, KsT_t = kb.alloc_tok("KsT", [128, 2, 2048], BF16, ntok=16)
    KwT, KwT_t = kb.alloc_tok("KwT", [128, 2, 2048], BF16, ntok=16)
    Vs, Vs_t = kb.alloc_tok("Vs", [128, 16, 2, 129], BF16, ntok=16)
    Vw, Vw_t = kb.alloc_tok("Vw", [128, 16, 2, 129], BF16, ntok=16)
    tkm, tkm_t = kb.alloc_tok("tkm", [128, 512], BF16)
    tkm2, tkm2_t = kb.alloc_tok("tkm2", [128, 512], BF16)
    hTo, hTo_t = kb.alloc_tok("hTown", [128, 16, 1024], BF16, ntok=8)
    hTh, hTh_t = kb.alloc_tok("hThalo", [128, 16, 16], BF16)
    kvcT, kvcT_t = kb.alloc_tok("kvcT", [128, 4, 2048], BF16)
    lnmix, lnmix_t = kb.alloc_tok("lnmix", [128, D], F32)
    kb.dma("sp", out=lnmix[:], in_=lnmix_d, writes=[lnmix_t])
    xt = []
    xt_t = []
    for i in range(2):
        t, tk = kb.alloc_tok("xt%d" % i, [128, D], F32)
        xt.append(t)
        xt_t.append(tk)
    hTt = []
    hTt_t = []
    for i in range(2):
        t, tk = kb.alloc_tok("hTt%d" % i, [128, 16, 128], BF16)
        hTt.append(t)
        hTt_t.append(tk)
    posi, posi_t = kb.alloc_tok("posi", [128, 16], I32)
    invf, invf_t = kb.alloc_tok("invf", [128, 16], F32)
    kb.dma("sp", out=posi[:], in_=posT_d, writes=[posi_t])
    kb.dma("sp", out=invf[:], in_=invf_d, writes=[invf_t])
    posf, posf_t = kb.alloc_tok("posf", [128, 16], F32)
    ang, ang_t = kb.alloc_tok("ang", [128, 2, 16, 16], F32)
    rtmp, rtmp_t = kb.alloc_tok("rtmp", [128, 2, 16, 16], F32)
    rki, rki_t = kb.alloc_tok("rki", [128, 2, 16, 16], I32)
    kb.op("dve", lambda e: e.tensor_copy(out=posf[:], in_=posi[:]), [posi_t], [posf_t])
    for ti in range(16):
        kb.op("dve", lambda e: e.tensor_scalar(out=ang[:, 0, ti, :], in0=invf[:], scalar1=posf[:, ti:ti + 1],
                                               scalar2=None, op0=ALU.mult), [invf_t, posf_t], [ang_t])
    kb.op("dve", lambda e: e.tensor_scalar(out=ang[:, 1], in0=ang[:, 0], scalar1=float(np.pi / 2), scalar2=None,
                                           op0=ALU.add), [ang_t], [ang_t])
    TWO_PI = float(2 * np.pi)
    kb.op("dve", lambda e: e.tensor_scalar(out=rtmp[:], in0=ang[:], scalar1=1.0 / TWO_PI, scalar2=None,
                                           op0=ALU.mult), [ang_t], [rtmp_t])
    kb.op("dve", lambda e: e.tensor_copy(out=rki[:], in_=rtmp[:]), [rtmp_t], [rki_t])
    kb.op("dve", lambda e: e.tensor_copy(out=rtmp[:], in_=rki[:]), [rki_t], [rtmp_t])
    C1 = 6.28125
    C2 = TWO_PI - C1
    kb.op("dve", lambda e: e.scalar_tensor_tensor(out=ang[:], in0=rtmp[:], scalar=-C1, in1=ang[:],
                                                  op0=ALU.mult, op1=ALU.add), [rtmp_t, ang_t], [ang_t])
    kb.op("dve", lambda e: e.scalar_tensor_tensor(out=ang[:], in0=rtmp[:], scalar=-C2, in1=ang[:],
                                                  op0=ALU.mult, op1=ALU.add), [rtmp_t, ang_t], [ang_t])
    PI = float(np.pi)
    kb.op("dve", lambda e: e.tensor_scalar(out=rtmp[:], in0=ang[:], scalar1=PI, scalar2=-TWO_PI,
                                           op0=ALU.is_gt, op1=ALU.mult), [ang_t], [rtmp_t])
    kb.op("dve", lambda e: e.tensor_tensor(out=ang[:], in0=ang[:], in1=rtmp[:], op=ALU.add), [ang_t, rtmp_t], [ang_t])
    kb.op("dve", lambda e: e.tensor_scalar(out=rtmp[:], in0=ang[:], scalar1=-PI, scalar2=TWO_PI,
                                           op0=ALU.is_lt, op1=ALU.mult), [ang_t], [rtmp_t])
    kb.op("dve", lambda e: e.tensor_tensor(out=ang[:], in0=ang[:], in1=rtmp[:], op=ALU.add), [ang_t, rtmp_t], [ang_t])
    kb.op("dve", lambda e: e.tensor_scalar(out=ang[:], in0=ang[:], scalar1=3.141592, scalar2=-3.141592,
                                           op0=ALU.min, op1=ALU.max), [ang_t], [ang_t])
    kb.op("act", lambda e: e.activation(out=rtmp[:], in_=ang[:], func=AF.Sin), [ang_t], [rtmp_t])
    for hh in range(4):
        kb.op("dve", lambda e: e.tensor_copy(out=cs4[:, :, :, hh, :], in_=rtmp[:]), [rtmp_t], [cs4_t])
    if "cs" in tap_d:
        tap("cs", rtmp[:].rearrange("p a t f -> p (a t f)"), [rtmp_t])

    for _t, _k in ((posi, posi_t), (invf, invf_t), (posf, posf_t), (ang, ang_t), (rtmp, rtmp_t), (rki, rki_t)):
        kb.free(_t, [_k])
    for g in range(2):
        kb.op("dve", lambda e: e.tensor_copy(out=Vs[:, :, g, 128], in_=valid_sb[:]), [valid_t], Vs_t)
        kb.op("dve", lambda e: e.tensor_copy(out=Vw[:, :, g, 128], in_=valid_sb[:]), [valid_t], Vw_t)

    psrot = {}

    def nextps(lo=0, hi=8):
        i = psrot.get((lo, hi), lo)
        psrot[(lo, hi)] = lo + (i + 1 - lo) % (hi - lo)
        return i

    def bfview(bank):
        return psb[bank][:].bitcast(BF16)

    sm1, sm1_t = kb.alloc_tok("sm1", [128, 16, 4], F32, ntok=16)
    hbB, hbB_t = kb.alloc_tok("hbB", [128, D], BF16)
    hbs = [(hb, hb_t), (hbB, hbB_t)]
    tkmB, tkmB_t = kb.alloc_tok("tkmB", [128, 512], BF16)
    tkm2B, tkm2B_t = kb.alloc_tok("tkm2B", [128, 512], BF16)
    tk2s = [(tkm2, tkm2_t), (tkm2B, tkm2B_t)]

    def norm_tile(lnrep, lnrep_t, ti, xbuf, xbuf_t, hbuf, hbuf_t):
        s_, st_ = sm1[:, ti, :], sm1_t[ti]
        kb.op("act", lambda e: e.activation(out=junk[:], in_=xbuf[:], func=AF.Square, accum_out=s_[:, 0:1]),
              [xbuf_t], [junk_t, st_], guard=True)
        kb.op("dve", lambda e: e.tensor_scalar(out=s_[:, 1:2], in0=s_[:, 0:1], scalar1=1.0 / D,
                                               scalar2=EPS, op0=ALU.mult, op1=ALU.add), [st_], [st_])
        kb.op("act", lambda e: e.activation(out=s_[:, 2:3], in_=s_[:, 1:2], func=AF.Sqrt), [st_], [st_])
        kb.op("dve", lambda e: e.reciprocal(out=s_[:, 3:4], in_=s_[:, 2:3]), [st_], [st_])
        kb.op("dve", lambda e: e.scalar_tensor_tensor(out=hbuf[:], in0=xbuf[:], scalar=s_[:, 3:4],
                                                      in1=lnrep[:], op0=ALU.mult, op1=ALU.mult),
              [xbuf_t, st_, lnrep_t], [hbuf_t])

    def transpose16(src, src_t, dst_ap_fn, dst_toks, banks=(6, 7), halves=(0, 1)):
        for half in halves:
            bk = banks[half]
            pv = bfview(bk)
            for j in range(8):
                kc = half * 8 + j
                kb.op("pe", lambda e: e.transpose(out=pv[:, j * 128:(j + 1) * 128], in_=src[:, kc * 128:(kc + 1) * 128],
                                                  identity=identb[:]), [src_t, identb_t], [pst[bk]])
            kb.op("act", lambda e: e.copy(out=dst_ap_fn(half), in_=pv[:, 0:1024].rearrange("p (a b) -> p a b", a=8)),
                  [pst[bk]], dst_toks)

    def p1_norm(ti):
        hbuf, hbuf_t = hbs[ti % 2]
        norm_tile(lnmix, lnmix_t, ti, xt[ti % 2], xt_t[ti % 2], hbuf, hbuf_t)

    def p1_hinfo(ti):
        if ti >= 8:
            ot = ti - 8
            return hTo_t[ot], (lambda kc: hTo[:, kc, ot * 128:(ot + 1) * 128]), (lambda half: hTo[:, half * 8:(half + 1) * 8, ot * 128:(ot + 1) * 128])
        hcur = hTt[ti % 2]
        return hTt_t[ti % 2], (lambda kc: hcur[:, kc, :]), (lambda half: hcur[:, half * 8:(half + 1) * 8, :])

    def p1_tr(ti, halves=(0, 1)):
        hbuf, hbuf_t = hbs[ti % 2]
        hs_t, lhs, dstf = p1_hinfo(ti)
        transpose16(hbuf, hbuf_t, dstf, [hs_t], halves=halves)
        if ti == 7 and 1 in halves:
            kb.op("dve", lambda e: e.tensor_copy(out=hTh[:], in_=hTt[1][:, :, 112:128]), [hs_t], [hTh_t])

    def p1_mm(ti, between=None):
        hs_t, lhs, dstf = p1_hinfo(ti)
        banks = []
        for cg in range(3):
            bk = nextps(0, 6)
            wv, wtk = wkv[cg]
            for kc in range(16):
                kb.op("pe", lambda e: e.matmul(out=psb[bk][:, 0:512], lhsT=lhs(kc), rhs=wv[:, kc, :], start=(kc == 0),
                                               stop=(kc == 15)), [hs_t, wtk], [pst[bk]])
            banks.append(bk)
            if between is not None and cg < 2:
                between(cg)
        return banks

    t2tok = [[Tok(), Tok()], [Tok(), Tok()]]
    tkms = [(tkm, tkm_t), (tkmB, tkmB_t)]

    def p1_evac_a(ti, banks):
        par = ti % 2
        t1, t1_t = tkms[par]
        t2 = tk2s[par][0]
        for cg in range(3):
            bk = banks[cg]
            if cg == 0:
                kb.op("act", lambda e: e.copy(out=t1[:], in_=psb[bk][:, 0:512]), [pst[bk]], [t1_t])
            else:
                V, V_t = (Vs, Vs_t) if cg == 1 else (Vw, Vw_t)
                co = (cg - 1) * 256
                rope(psb[bk][:, 0:256].rearrange("p (h d) -> p h d", h=2), 2, ti,
                     t2[:, co:co + 256].rearrange("p (h d) -> p h d", h=2), pst[bk], t2tok[par][cg - 1])
                kb.op("act", lambda e: e.copy(out=V[:, ti, :, 0:128], in_=psb[bk][:, 256:512].rearrange("p (h d) -> p h d", h=2)),
                      [pst[bk]], [V_t[ti]])

    def p1_evac_b(ti):
        par = ti % 2
        t1, t1_t = tkms[par]
        t2 = tk2s[par][0]
        tb = 7
        pv = bfview(tb)
        for j in range(4):
            kb.op("pe", lambda e: e.transpose(out=pv[:, j * 128:(j + 1) * 128], in_=t1[:, j * 128:(j + 1) * 128],
                                              identity=identb[:]), [t1_t, identb_t], [pst[tb]])
        kb.op("dve", lambda e: e.tensor_copy(out=kvcT[:, :, ti * 128:(ti + 1) * 128],
                                             in_=pv[:, 0:512].rearrange("p (a b) -> p a b", a=4)), [pst[tb]], [kvcT_t])
        tb = 6
        pv = bfview(tb)
        for cg in (1, 2):
            co = (cg - 1) * 256
            for j in range(2):
                kb.op("pe", lambda e: e.transpose(out=pv[:, co + j * 128:co + (j + 1) * 128], in_=t2[:, co + j * 128:co + (j + 1) * 128],
                                                  identity=identb[:]), [t2tok[par][cg - 1], identb_t], [pst[tb]])
        kb.op("dve", lambda e: e.tensor_copy(out=KsT[:, :, ti * 128:(ti + 1) * 128],
                                             in_=pv[:, 0:256].rearrange("p (a b) -> p a b", a=2)), [pst[tb]], [KsT_t[ti]])
        kb.op("dve", lambda e: e.tensor_copy(out=KwT[:, :, ti * 128:(ti + 1) * 128],
                                             in_=pv[:, 256:512].rearrange("p (a b) -> p a b", a=2)), [pst[tb]], [KwT_t[ti]])

    def p1_norm_dma(k):
        p1_norm(k)
        if k + 2 < 16:
            kb.dma("sp", out=xt[k % 2][:], in_=xc[(k + 2) * 128:(k + 3) * 128, :], writes=[xt_t[k % 2]])

    kb.dma("sp", out=xt[0][:], in_=xc[0:128, :], writes=[xt_t[0]])
    kb.dma("sp", out=xt[1][:], in_=xc[128:256, :], writes=[xt_t[1]])
    p1_norm_dma(0)
    p1_tr(0)
    p1_norm_dma(1)
    for ti in range(16):
        banks = p1_mm(ti, (lambda cg, _ti=ti: p1_tr(_ti + 1, halves=(cg,))) if ti + 1 < 16 else None)
        if ti + 2 < 16:
            p1_norm_dma(ti + 2)
        p1_evac_a(ti, banks)
        if ti >= 1:
            p1_evac_b(ti - 1)
    p1_evac_b(15)
    if "kvcT" in tap_d:
        tap("kvcT", kvcT[:].rearrange("p a t -> p (a t)"), [kvcT_t])
        tap("KsT", KsT[:].rearrange("p a t -> p (a t)"), KsT_t)
        tap("Vw", Vw[:].rearrange("p a g d -> p (a g d)"), Vw_t)
    if stage <= 1:
        kb.finish()
        return nc

    for i in range(2):
        kb.free(xt[i], [xt_t[i]])
        kb.free(hTt[i], [hTt_t[i]])
    kb.free(lnmix, [lnmix_t])
    kb.free(hbB, [hbB_t])
    kb.free(sm1, sm1_t)
    w1 = []
    for kind, src_d in ((0, w1k_d), (1, w1v_d)):
        w1.append(ring_load(src_d.rearrange("(l d) n -> d l n", d=128), ""))
    w2sb, w2_t = kb.alloc_tok("w2sb", [128, 2, 2, 128], BF16)
    kb.dma("pool", out=w2sb[:, 0], in_=w2k_d.rearrange("(hc p) n -> p hc n", p=128), writes=[w2_t])
    kb.dma("pool", out=w2sb[:, 1], in_=w2v_d.rearrange("(hc p) n -> p hc n", p=128), writes=[w2_t])
    peT, peT_t = kb.alloc_tok("peT", [128, 2, 32], BF16)
    kb.dma("pool", out=peT[:, 0], in_=pekT_d, writes=[peT_t])
    kb.dma("pool", out=peT[:, 1], in_=pevT_d, writes=[peT_t])
    kcmpT, kcmpT_t = kb.alloc_tok("kcmpT", [128, 2, 128], BF16)
    Vc, Vc_t = kb.alloc_tok("Vc", [128, 2, 162], BF16)
    for g in range(2):
        kb.op("dve", lambda e: e.memset(Vc[:, g, 128:129], 1.0), [], [Vc_t])
        kb.dma("sp", out=Vc[:, g, 129:161], in_=wcs_d, writes=[Vc_t])
    gx, gx_t = kb.alloc_tok("gx", [128, 128], F32)
    gu, gu_t = kb.alloc_tok("gu", [128, 128], F32)
    gs, gs_t = kb.alloc_tok("gs", [128, 128], F32)
    gT, gT_t = kb.alloc_tok("gT", [128, 2, 128], BF16)
    for kind in range(2):
        wv, wtk = w1[kind]
        for g in range(2):
            srcv = kvcT[:, kind * 2 + g, :].rearrange("p (j s) -> p j s", s=16)
            for hc in range(2):
                bk = nextps(0, 4)
                for l in range(32):
                    rhs = srcv[:, 0:127, l] if l < 16 else srcv[:, 1:128, l - 16]
                    kb.op("pe", lambda e: e.matmul(out=psb[bk][:, 0:127], lhsT=wv[:, l, hc * 128:(hc + 1) * 128], rhs=rhs,
                                                   start=(l == 0), stop=(l == 31)), [kvcT_t, wtk], [pst[bk]])
                for l in range(32):
                    kb.op("pe", lambda e: e.matmul(out=psb[bk][:, 128:129], lhsT=wv[:, l, hc * 128:(hc + 1) * 128],
                                                   rhs=peT[:, kind, l:l + 1], start=(l == 0), stop=(l == 31)),
                          [peT_t, wtk], [pst[bk]])
                kb.op("dve", lambda e: e.tensor_copy(out=small[:, 64:65], in_=psb[bk][:, 128:129]), [pst[bk]], [small_t])
                kb.op("dve", lambda e: e.tensor_scalar(out=gx[:, 0:127], in0=psb[bk][:, 0:127], scalar1=small[:, 64:65], scalar2=None,
                                                       op0=ALU.add), [pst[bk], small_t], [gx_t])
                kb.op("dve", lambda e: e.tensor_tensor(out=gu[:, 0:127], in0=gx[:, 0:127], in1=gx[:, 0:127], op=ALU.mult), [gx_t], [gu_t])
                kb.op("dve", lambda e: e.tensor_scalar(out=gu[:, 0:127], in0=gu[:, 0:127], scalar1=0.044715, scalar2=1.0,
                                                       op0=ALU.mult, op1=ALU.add), [gu_t], [gu_t])
                kb.op("dve", lambda e: e.tensor_tensor(out=gu[:, 0:127], in0=gu[:, 0:127], in1=gx[:, 0:127], op=ALU.mult), [gu_t, gx_t], [gu_t])
                kb.op("act", lambda e: e.activation(out=gs[:, 0:127], in_=gu[:, 0:127], func=AF.Sigmoid, scale=1.5957691216057308),
                      [gu_t], [gs_t])
                kb.op("dve", lambda e: e.tensor_tensor(out=gT[:, hc, 0:127], in0=gx[:, 0:127], in1=gs[:, 0:127], op=ALU.mult),
                      [gx_t, gs_t], [gT_t])
            bk = nextps(0, 4)
            if kind == 0:
                for hc in range(2):
                    kb.op("pe", lambda e: e.matmul(out=psb[bk][:, 0:127], lhsT=w2sb[:, 0, hc, :], rhs=gT[:, hc, 0:127],
                                                   start=(hc == 0), stop=(hc == 1)), [w2_t, gT_t], [pst[bk]])
                kb.op("act", lambda e: e.copy(out=kcmpT[:, g, 0:127], in_=psb[bk][:, 0:127]), [pst[bk]], [kcmpT_t])
            else:
                for hc in range(2):
                    kb.op("pe", lambda e: e.matmul(out=psb[bk][0:127, 0:128], lhsT=gT[:, hc, 0:127], rhs=w2sb[:, 1, hc, :],
                                                   start=(hc == 0), stop=(hc == 1)), [w2_t, gT_t], [pst[bk]])
                kb.op("act", lambda e: e.copy(out=Vc[0:127, g, 0:128], in_=psb[bk][0:127, 0:128]), [pst[bk]], [Vc_t])
    kb.free(kvcT, [kvcT_t])
    kb.free(gx, [gx_t])
    kb.free(gu, [gu_t])
    kb.free(gs, [gs_t])
    kb.free(gT, [gT_t])
    QT2, QT2_t = kb.alloc_tok("QT2", [128, 2, 8, 1024], BF16, ntok=4)
    qw = [ring_load(w_in[:, qc * 512:(qc + 1) * 512].rearrange("(kc p) n -> p kc n", p=128), "") for qc in range(2)]

    def q_mm(qc, ot):
        wv, wtk = qw[qc]
        bk = nextps(0, 4)
        for kc in range(16):
            kb.op("pe", lambda e: e.matmul(out=psb[bk][:, 0:512], lhsT=hTo[:, kc, ot * 128:(ot + 1) * 128], rhs=wv[:, kc, :],
                                           start=(kc == 0), stop=(kc == 15)), [hTo_t[ot], wtk], [pst[bk]])
        return bk

    def q_evac(qc, ot, bk, par):
        t1, t1_t = (tkm, tkm_t) if par == 0 else (tkmB, tkmB_t)
        t2, t2_t = tk2s[par]
        rope(psb[bk][:, 0:512].rearrange("p (h d) -> p h d", h=4), 4, 8 + ot,
             t2[:, 0:512].rearrange("p (h d) -> p h d", h=4), pst[bk], t2_t)
        kb.op("act", lambda e: e.copy(out=t1[:], in_=psb[bk][:, 0:512]), [pst[bk]], [t1_t])
        tb = 4 + par
        pv = bfview(tb)
        for j in range(8):
            srcb, srct = (t1, t1_t) if j < 4 else (t2, t2_t)
            jj = j % 4
            kb.op("pe", lambda e: e.transpose(out=pv[:, j * 128:(j + 1) * 128], in_=srcb[:, jj * 128:(jj + 1) * 128],
                                              identity=identb[:]), [srct, identb_t], [pst[tb]])
        kb.op("dve", lambda e: e.tensor_copy(out=QT2[:, :, 4 * qc:4 * qc + 4, ot * 128:(ot + 1) * 128],
                                             in_=pv[:, 0:1024].rearrange("p (a h d) -> p a h d", a=2, h=4)),
              [pst[tb]], [QT2_t[qc * 2 + ot // 4]])

    qitems = [(qc, ot) for qc in range(2) for ot in range(8)]
    pend = q_mm(*qitems[0])
    for i, (qc, ot) in enumerate(qitems):
        bk = pend
        if i + 1 < len(qitems):
            pend = q_mm(*qitems[i + 1])
        q_evac(qc, ot, bk, i % 2)
    wga, wga_t = kb.alloc_tok("wga", [128, 16, 24], BF16)
    kb.dma("pool", out=wga[:], in_=w_in[:, 2560:2584].rearrange("(kc p) n -> p kc n", p=128), writes=[wga_t])
    gate_sb, gate_t = kb.alloc_tok("gate", [128, 8, 24], F32)
    for ot in range(8):
        bk = nextps(0, 4)
        for kc in range(16):
            kb.op("pe", lambda e: e.matmul(out=psb[bk][:, 0:24], lhsT=hTo[:, kc, ot * 128:(ot + 1) * 128], rhs=wga[:, kc, :],
                                           start=(kc == 0), stop=(kc == 15)), [hTo_t[ot], wga_t], [pst[bk]])
        kb.op("act", lambda e: e.activation(out=gate_sb[:, ot, :], in_=psb[bk][:, 0:24], func=AF.Sigmoid), [pst[bk]], [gate_t])
    ub, ub_t = kb.alloc_tok("ub", [128, 1040], F32)
    sa, sa_t = kb.alloc_tok("sa", [128, 1040], F32)
    sbb, sbb_t = kb.alloc_tok("sbb", [128, 1040], F32)
    invcnt, invcnt_t = kb.alloc_tok("invcnt", [128, 4, 16], F32)
    kb.dma("sp", out=invcnt[:], in_=invcnt_d, writes=[invcnt_t])
    dT, dT_t = kb.alloc_tok("dT", [128, 8, 1024], BF16, ntok=8)
    for uc in range(2):
        wv, wtk = ring_load(w_in[:, 2584 + uc * 512:2584 + (uc + 1) * 512].rearrange("(kc p) n -> p kc n", p=128), "")
        for c4 in range(4):
            c8 = uc * 4 + c4
            wi = c8 // 2
            w = POOL_SIZES[wi]
            bk = nextps(0, 4)
            for kc in range(16):
                kb.op("pe", lambda e: e.matmul(out=psb[bk][:, 0:16], lhsT=wv[:, kc, c4 * 128:(c4 + 1) * 128], rhs=hTh[:, kc, :],
                                               start=(kc == 0), stop=(kc == 15)), [hTh_t, wtk], [pst[bk]])
            kb.op("act", lambda e: e.copy(out=ub[:, 0:16], in_=psb[bk][:, 0:16]), [pst[bk]], [ub_t])
            for tq in range(2):
                bk = nextps(0, 4)
                for kc in range(16):
                    kb.op("pe", lambda e: e.matmul(out=psb[bk][:, 0:512], lhsT=wv[:, kc, c4 * 128:(c4 + 1) * 128],
                                                   rhs=hTo[:, kc, tq * 512:(tq + 1) * 512], start=(kc == 0), stop=(kc == 15)),
                          hTo_t[4 * tq:4 * tq + 4] + [wtk], [pst[bk]])
                kb.op("act", lambda e: e.copy(out=ub[:, 16 + tq * 512:16 + (tq + 1) * 512], in_=psb[bk][:, 0:512]), [pst[bk]], [ub_t])
            cur, cur_t = ub, ub_t
            bufs = [(sa, sa_t), (sbb, sbb_t)]
            step = 1
            bi = 0
            while step < w:
                nb, nb_t = bufs[bi]
                lo = 2 * step - 1
                kb.op("dve", lambda e: e.tensor_tensor(out=nb[:, lo:1040], in0=cur[:, lo:1040], in1=cur[:, lo - step:1040 - step],
                                                       op=ALU.add), [cur_t], [nb_t])
                cur, cur_t = nb, nb_t
                bi ^= 1
                step *= 2
            kb.op("dve", lambda e: e.scalar_tensor_tensor(out=dT[:, c8, :], in0=cur[:, 16:1040], scalar=1.0 / w, in1=ub[:, 16:1040],
                                                          op0=ALU.mult, op1=ALU.subtract), [cur_t, ub_t], [dT_t[c8]])
            kb.op("dve", lambda e: e.tensor_tensor(out=rt[:, 0, 0, :], in0=cur[:, 16:32], in1=invcnt[:, wi, :], op=ALU.mult),
                  [cur_t, invcnt_t], [rt_t])
            kb.op("dve", lambda e: e.tensor_tensor(out=dT[:, c8, 0:16], in0=rt[:, 0, 0, :], in1=ub[:, 16:32], op=ALU.subtract),
                  [rt_t, ub_t], [dT_t[c8]])
    kb.free(hTo, hTo_t)
    kb.free(hTh, [hTh_t])
    kb.free(ub, [ub_t])
    kb.free(sa, [sa_t])
    kb.free(sbb, [sbb_t])
    kb.free(wga, [wga_t])
    if "QT2" in tap_d:
        tap("QT2", QT2[:].rearrange("p a h q -> p (a h q)"), QT2_t)
        tap("dT", dT[:].rearrange("p a q -> p (a q)"), dT_t)
        tap("gate", gate_sb[:].rearrange("p a q -> p (a q)"), [gate_t])
        tap("kcmpT", kcmpT[:].rearrange("p a q -> p (a q)"), [kcmpT_t])
        tap("Vc", Vc[:].rearrange("p a q -> p (a q)"), [Vc_t])
    if stage <= 2:
        kb.finish()
        return nc

    for i in range(NSLOT):
        kb.free(ring[i], [ring_t[i]])

    def rms(src_ap, src_toks, n, ln_ap, ln_tok, out_ap, out_toks, sm, sm_t, col):
        kb.op("act", lambda e: e.activation(out=junk[:, 0:n], in_=src_ap, func=AF.Square, accum_out=sm[:, col:col + 1]),
              src_toks, [junk_t, sm_t], guard=True)
        kb.op("dve", lambda e: e.tensor_scalar(out=sm[:, col + 1:col + 2], in0=sm[:, col:col + 1], scalar1=1.0 / n,
                                               scalar2=EPS, op0=ALU.mult, op1=ALU.add), [sm_t], [sm_t])
        kb.op("act", lambda e: e.activation(out=sm[:, col + 2:col + 3], in_=sm[:, col + 1:col + 2], func=AF.Sqrt), [sm_t], [sm_t])
        kb.op("dve", lambda e: e.reciprocal(out=sm[:, col + 3:col + 4], in_=sm[:, col + 2:col + 3]), [sm_t], [sm_t])
        kb.op("dve", lambda e: e.scalar_tensor_tensor(out=out_ap, in0=src_ap, scalar=sm[:, col + 3:col + 4], in1=ln_ap,
                                                      op0=ALU.mult, op1=ALU.mult), list(src_toks) + [sm_t, ln_tok], out_toks)

    mixtok, mixtok_t = kb.alloc_tok("mixtok", [128, 8, 2048], BF16, ntok=8)
    wpool_sb, wpool_t = kb.alloc_tok("wpool", [128, 4, 2, 256], BF16)
    for G in range(4):
        kb.dma("pool", out=wpool_sb[:, G], in_=wpool_d[G].rearrange("(k p) n -> p k n", p=128), writes=[wpool_t])
    bpool_sb, bpool_t = kb.alloc_tok("bpool", [128, 1024], F32)
    pscale_sb, pscale_t = kb.alloc_tok("pscale", [128, 1024], F32)
    gnpool_sb, gnpool_t = kb.alloc_tok("gnpool", [128, 1024], F32)
    gnnsa_sb, gnnsa_t = kb.alloc_tok("gnnsa", [128, 1024], F32)
    kb.dma("sp", out=bpool_sb[:], in_=bpool_d, writes=[bpool_t])
    kb.dma("sp", out=pscale_sb[:], in_=pscale_d, writes=[pscale_t])
    kb.dma("sp", out=gnpool_sb[:], in_=gnpool_d, writes=[gnpool_t])
    kb.dma("sp", out=gnnsa_sb[:], in_=gnnsa_d, writes=[gnnsa_t])
    opool, opool_t = kb.alloc_tok("opool", [128, 1024], F32)
    sm2, sm2_t = kb.alloc_tok("sm2", [128, 64], F32)
    for ot in range(8):
        pb0 = (ot % 2) * 2
        for G in range(4):
            bk = pb0 + G // 2
            for k2 in range(2):
                kb.op("pe", lambda e: e.matmul(out=psb[bk][:, (G % 2) * 256:(G % 2) * 256 + 256],
                                               lhsT=dT[:, 2 * G + k2, ot * 128:(ot + 1) * 128], rhs=wpool_sb[:, G, k2, :],
                                               start=(k2 == 0), stop=(k2 == 1)), [dT_t[2 * G + k2], wpool_t], [pst[bk]])
        for half in range(2):
            kb.op("dve", lambda e: e.tensor_tensor(out=opool[:, half * 512:(half + 1) * 512], in0=psb[pb0 + half][:, 0:512],
                                                   in1=bpool_sb[:, half * 512:(half + 1) * 512], op=ALU.add),
                  [pst[pb0 + half], bpool_t], [opool_t])
        kb.op("dve", lambda e: e.tensor_tensor(out=opool[:], in0=opool[:], in1=pscale_sb[:], op=ALU.mult), [opool_t, pscale_t], [opool_t])
        if ot == 0 and "opool" in tap_d:
            tap("opool", opool[:], [opool_t])
        rms(opool[:], [opool_t], 1024, gnpool_sb[:], gnpool_t, mixtok[:, ot, 1024:2048], [mixtok_t[ot]], sm2, sm2_t, 0)
    if stage <= 2.5:
        kb.finish()
        return nc
    kb.free(dT, dT_t)
    kb.free(bpool_sb, [bpool_t])
    kb.free(pscale_sb, [pscale_t])
    kb.free(wpool_sb, [wpool_t])
    cmpb_sb, cmpb_t = kb.alloc_tok("cmpb", [128, 1024], BF16)
    emat_sb, emat_t = kb.alloc_tok("emat", [128, 2048], BF16)
    causb_sb, causb_t = kb.alloc_tok("causb", [128, 4, 512], BF16)
    winb_sb, winb_t = kb.alloc_tok("winb", [128, 8, 512], BF16)
    amul_sb, amul_t = kb.alloc_tok("amul", [128, 8, 32], F32)
    aadd_sb, aadd_t = kb.alloc_tok("aadd", [128, 8, 32], F32)
    kb.dma("sp", out=cmpb_sb[:], in_=cmpbias_d, writes=[cmpb_t])
    kb.dma("sp", out=emat_sb[:], in_=emat_d, writes=[emat_t])
    kb.dma("sp", out=causb_sb[:], in_=causb_d, writes=[causb_t])
    kb.dma("sp", out=winb_sb[:], in_=winb_d, writes=[winb_t])
    kb.dma("sp", out=amul_sb[:], in_=amul_d, writes=[amul_t])
    kb.dma("sp", out=aadd_sb[:], in_=aadd_d, writes=[aadd_t])
    PT = []
    PT_t = []
    for i in range(3):
        t, tk = kb.alloc_tok("PT%d" % i, [128, 512], BF16)
        PT.append(t)
        PT_t.append(tk)
    ptpos = [0]
    selbT, selbT_t = kb.alloc_tok("selbT", [128, 2, 512], BF16, ntok=2)
    kb.op("dve", lambda e: e.memset(selbT[:], 0.0), [], selbT_t)
    oacc, oacc_t = kb.alloc_tok("oacc", [128, 4, 1024], F32, ntok=4)
    impsb, imp_t = kb.alloc_tok("impsb", [128, 4, 32], F32, ntok=4)
    impm, impm_t = kb.alloc_tok("impm", [128, 32], F32)
    imp2, imp2_t = kb.alloc_tok("imp2", [128, 32], F32)
    m8, m8_t = kb.alloc_tok("m8", [128, 16], F32)
    selb, selb_t = kb.alloc_tok("selb", [128, 32], BF16)
    sm3 = []
    sm3_t = []
    for i in range(4):
        t, tk = kb.alloc_tok("sm3_%d" % i, [128, 8], F32)
        sm3.append(t)
        sm3_t.append(tk)
    smpos = [0]

    def evac_multi(jobs):
        ss = []
        for _ in jobs:
            i = smpos[0]
            smpos[0] = (i + 1) % 4
            ss.append((sm3[i], sm3_t[i]))
        for (job, (s, st)) in zip(jobs, ss):
            kb.op("dve", lambda e: e.tensor_scalar(out=s[:, 0:1], in0=job[1], scalar1=1e-30, scalar2=None, op0=ALU.max), [job[2]], [st])
        for (job, (s, st)) in zip(jobs, ss):
            kb.op("dve", lambda e: e.reciprocal(out=s[:, 1:2], in_=s[:, 0:1]), [st], [st])
        for (job, (s, st)) in zip(jobs, ss):
            tile, gcol = job[4], job[6]
            kb.op("dve", lambda e: e.tensor_tensor(out=s[:, 2:3], in0=s[:, 1:2], in1=gate_sb[:, tile, gcol:gcol + 1], op=ALU.mult),
                  [st, gate_t], [st])
        for (job, (s, st)) in zip(jobs, ss):
            ps_ap_num, _, ps_tok, j, tile, head, gcol, first, imp_ap, imp_first = job
            dst = oacc[:, j, head * 128:(head + 1) * 128]
            if first:
                kb.op("dve", lambda e: e.tensor_scalar(out=dst, in0=ps_ap_num, scalar1=s[:, 2:3], scalar2=None, op0=ALU.mult),
                      [ps_tok, st], [oacc_t[j]])
            else:
                kb.op("dve", lambda e: e.scalar_tensor_tensor(out=dst, in0=ps_ap_num, scalar=s[:, 2:3], in1=dst, op0=ALU.mult,
                                                              op1=ALU.add), [ps_tok, st, oacc_t[j]], [oacc_t[j]])
        for (job, (s, st)) in zip(jobs, ss):
            ps_ap_num, _, ps_tok, j, tile, head, gcol, first, imp_ap, imp_first = job
            if imp_ap is not None:
                if imp_first:
                    kb.op("dve", lambda e: e.tensor_scalar(out=impsb[:, j, :], in0=imp_ap, scalar1=s[:, 1:2], scalar2=None, op0=ALU.mult),
                          [ps_tok, st], [imp_t[j]])
                else:
                    kb.op("dve", lambda e: e.scalar_tensor_tensor(out=impsb[:, j, :], in0=imp_ap, scalar=s[:, 1:2], in1=impsb[:, j, :],
                                                                  op0=ALU.mult, op1=ALU.add), [ps_tok, st, imp_t[j]], [imp_t[j]])

    def expo(bk, np_):
        i = ptpos[0]
        ptpos[0] = (i + 1) % 3
        kb.op("act", lambda e: e.activation(out=PT[i][0:np_, :], in_=psb[bk][0:np_, 0:512], func=AF.Exp, scale=SCALE),
              [pst[bk]], [PT_t[i]])
        return PT[i], PT_t[i]

    accpair = [0]
    for qc in range(2):
        qsl = slice(qc * 512, (qc + 1) * 512)
        qtoks = lambda head: [QT2_t[(head // 4) * 2 + qc]]
        for g in range(2):
            for r in range(4):
                head = 4 * g + r
                bk = nextps(0, 3)
                kb.op("pe", lambda e: e.matmul(out=psb[bk][0:127, 0:512], lhsT=kcmpT[:, g, 0:127], rhs=QT2[:, 0, head, qsl],
                                               start=True, stop=False), [kcmpT_t] + qtoks(head), [pst[bk]])
                kb.op("pe", lambda e: e.matmul(out=psb[bk][0:127, 0:512], lhsT=identb[0:127, 0:127], rhs=cmpb_sb[0:127, qsl],
                                               start=False, stop=True), [identb_t, cmpb_t], [pst[bk]])
                P, P_t = expo(bk, 127)
                jobs = []
                for j in range(4):
                    ob = nextps(3, 8)
                    kb.op("pe", lambda e: e.matmul(out=psb[ob][:, 0:161], lhsT=P[0:127, j * 128:(j + 1) * 128], rhs=Vc[0:127, g, 0:161],
                                                   start=True, stop=True), [P_t, Vc_t], [pst[ob]])
                    jobs.append((psb[ob][:, 0:128], psb[ob][:, 128:129], pst[ob], j, 4 * qc + j, head, 3 * head + 0, True,
                                 psb[ob][:, 129:161], r == 0))
                evac_multi(jobs)
            for j in range(4):
                tile = 4 * qc + j
                kb.op("dve", lambda e: e.tensor_tensor(out=impm[:], in0=impsb[:, j, :], in1=amul_sb[:, tile, :], op=ALU.mult),
                      [imp_t[j], amul_t], [impm_t])
                kb.op("dve", lambda e: e.tensor_tensor(out=impm[:], in0=impm[:], in1=aadd_sb[:, tile, :], op=ALU.add),
                      [impm_t, aadd_t], [impm_t])
                if "impm" in tap_d and g == 0 and j == 0 and qc == 0:
                    tap("impm", impm[:], [impm_t])
                kb.op("dve", lambda e: e.max(out=m8[:, 0:8], in_=impm[:]), [impm_t], [m8_t])
                kb.op("dve", lambda e: e.match_replace(out=imp2[:], in_to_replace=m8[:, 0:8], in_values=impm[:], imm_value=-3.0e38),
                      [m8_t, impm_t], [imp2_t], guard=True)
                kb.op("dve", lambda e: e.max(out=m8[:, 8:16], in_=imp2[:]), [imp2_t], [m8_t])
                kb.op("dve", lambda e: e.tensor_scalar(out=m8[:, 15:16], in0=m8[:, 15:16], scalar1=0.0, scalar2=None, op0=ALU.max),
                      [m8_t], [m8_t])
                kb.op("dve", lambda e: e.tensor_scalar(out=imp2[:], in0=impm[:], scalar1=m8[:, 15:16], scalar2=None, op0=ALU.is_ge),
                      [impm_t, m8_t], [imp2_t])
                kb.op("dve", lambda e: e.tensor_scalar(out=selb[:], in0=imp2[:], scalar1=-1.0, scalar2=-NEGB, op0=ALU.add, op1=ALU.mult),
                      [imp2_t], [selb_t])
                if "selb" in tap_d and g == 0 and j == 0 and qc == 0:
                    tap("selb", imp2[:], [imp2_t])
                tb = 6
                pv = bfview(tb)
                kb.op("pe", lambda e: e.transpose(out=pv[0:32, 0:128], in_=selb[:, 0:32], identity=identb[:]), [selb_t, identb_t], [pst[tb]])
                kb.op("act", lambda e: e.copy(out=selbT[0:32, g, j * 128:(j + 1) * 128], in_=pv[0:32, 0:128]), [pst[tb]], [selbT_t[g]])
        if "oacc_cmp" in tap_d and qc == 0:
            tap("oacc_cmp", oacc[:].rearrange("p a q -> p (a q)"), oacc_t)
        if stage <= 2.7:
            kb.finish()
            return nc
        items = []
        for g in range(2):
            for r in range(4):
                head = 4 * g + r
                for br in range(2):
                    kts = list(range(12 + 4 * qc)) if br == 0 else [4 + 4 * qc + i for i in range(8)]
                    for ii, kt in enumerate(kts):
                        items.append((g, head, br, ii, kt, ii == 0, ii == len(kts) - 1))

        def emitS(it):
            g, head, br, ii, kt, _, _ = it
            KT, KT_t = (KsT, KsT_t) if br == 0 else (KwT, KwT_t)
            bk = nextps(0, 3)
            kb.op("pe", lambda e: e.matmul(out=psb[bk][:, 0:512], lhsT=KT[:, g, kt * 128:(kt + 1) * 128], rhs=QT2[:, 1, head, qsl],
                                           start=True, stop=False), [KT_t[kt]] + qtoks(head), [pst[bk]])
            if br == 0:
                diag = kt - (8 + 4 * qc)
                kb.op("pe", lambda e: e.matmul(out=psb[bk][:, 0:512], lhsT=emat_sb[:, kt * 128:(kt + 1) * 128],
                                               rhs=selbT[:, g, :], start=False, stop=(diag < 0)),
                      [emat_t, selbT_t[g]], [pst[bk]])
                if diag >= 0:
                    kb.op("pe", lambda e: e.matmul(out=psb[bk][:, 0:512], lhsT=identb[:], rhs=causb_sb[:, diag, :],
                                                   start=False, stop=True), [identb_t, causb_t], [pst[bk]])
            else:
                kb.op("pe", lambda e: e.matmul(out=psb[bk][:, 0:512], lhsT=identb[:], rhs=winb_sb[:, ii, :],
                                               start=False, stop=True), [identb_t, winb_t], [pst[bk]])
            return bk

        pendq = [emitS(items[0]), emitS(items[1])]
        accb = None
        for idx, it in enumerate(items):
            g, head, br, ii, kt, first_, last_ = it
            bk = pendq.pop(0)
            if idx + 2 < len(items):
                pendq.append(emitS(items[idx + 2]))
            if first_:
                accb = [nextps(3, 8) for _ in range(4)]
            V, V_t = (Vs, Vs_t) if br == 0 else (Vw, Vw_t)
            if br == 0:
                jr = [(j, kt == 0, kt == 8 + 4 * qc + j) for j in range(4) if kt <= 8 + 4 * qc + j]
            else:
                jr = [(j, ii == j, ii == j + 4) for j in range(4) if j <= ii <= j + 4]
            P, P_t = expo(bk, 128)
            for (j, st_, sp_) in jr:
                ab = accb[j]
                kb.op("pe", lambda e: e.matmul(out=psb[ab][:, 0:129], lhsT=P[:, j * 128:(j + 1) * 128],
                                               rhs=V[:, kt, g, 0:129], start=st_, stop=sp_), [P_t, V_t[kt]], [pst[ab]])
            if last_:
                evac_multi([(psb[accb[j]][:, 0:128], psb[accb[j]][:, 128:129], pst[accb[j]], j, 4 * qc + j, head, 3 * head + 1 + br,
                             False, None, False) for j in range(4)])
        if "oacc" in tap_d and qc == 0:
            tap("oacc", oacc[:].rearrange("p a q -> p (a q)"), oacc_t)
        for j in range(4):
            tile = 4 * qc + j
            rms(oacc[:, j, :], [oacc_t[j]], 1024, gnnsa_sb[:], gnnsa_t, mixtok[:, tile, 0:1024], [mixtok_t[tile]], sm2, sm2_t, 8)
    if "mixtok" in tap_d:
        tap("mixtok", mixtok[:].rearrange("p a q -> p (a q)"), mixtok_t)
    if stage <= 3:
        kb.finish()
        return nc

    for _t, _k in ((QT2, QT2_t), (KsT, KsT_t), (KwT, KwT_t), (Vs, Vs_t), (Vw, Vw_t), (kcmpT, [kcmpT_t]), (Vc, [Vc_t]),
                   (cmpb_sb, [cmpb_t]), (emat_sb, [emat_t]), (causb_sb, [causb_t]), (winb_sb, [winb_t]), (amul_sb, [amul_t]),
                   (aadd_sb, [aadd_t]), (PT[0], [PT_t[0]]), (PT[1], [PT_t[1]]), (PT[2], [PT_t[2]]), (selbT, selbT_t), (oacc, oacc_t), (impsb, imp_t),
                   (impm, [impm_t]), (imp2, [imp2_t]), (m8, [m8_t]), (selb, [selb_t]), (gate_sb, [gate_t]), (gnpool_sb, [gnpool_t]),
                   (gnnsa_sb, [gnnsa_t]), (opool, [opool_t]), (w2sb, [w2_t]), (peT, [peT_t]), (tkm, [tkm_t]), (tkm2, [tkm2_t]), (tkmB, [tkmB_t]), (tkm2B, [tkm2B_t]),
                   (cs4, [cs4_t]), (rt, [rt_t]), (invcnt, [invcnt_t])):
        kb.free(_t, _k)
    for i in range(4):
        kb.free(sm3[i], [sm3_t[i]])
    x1, x1_t = kb.alloc_tok("x1", [128, 8, D], F32, ntok=8)
    for ot in range(8):
        kb.dma("sp", out=x1[:, ot, :], in_=xc[1024 + ot * 128:1024 + (ot + 1) * 128, :], writes=[x1_t[ot]])
    mixT, mixT_t = kb.alloc_tok("mixT", [128, 16, 1024], BF16, ntok=8)
    ring = []
    ring_t = []
    NSLOT = 3
    for i in range(NSLOT):
        t, tk = kb.alloc_tok("ringb%d" % i, [128, 8192], BF16)
        ring.append(t)
        ring_t.append(tk)
    rp2 = [0]

    def ring_load2(src_ap, nslot):
        i = rp2[0]
        rp2[0] = (i + 1) % nslot
        a, b = src_ap.shape[1], src_ap.shape[2]
        view = ring[i][:, 0:a * b].rearrange("p (a b) -> p a b", a=a)
        kb.dma("pool", out=view, in_=src_ap, writes=[ring_t[i]])
        return view, ring_t[i]

    for ot in range(8):
        transpose16(mixtok[:, ot, :], mixtok_t[ot], lambda half: mixT[:, half * 8:(half + 1) * 8, ot * 128:(ot + 1) * 128], [mixT_t[ot]])
    for oc in range(4):
        wv, wtk = ring_load2(wout_d[:, oc * 512:(oc + 1) * 512].rearrange("(kc p) n -> p kc n", p=128), 3)
        for ot in range(8):
            bk = nextps(0, 4)
            for kc in range(16):
                kb.op("pe", lambda e: e.matmul(out=psb[bk][:, 0:512], lhsT=mixT[:, kc, ot * 128:(ot + 1) * 128], rhs=wv[:, kc, :],
                                               start=(kc == 0), stop=(kc == 15)), [mixT_t[ot], wtk], [pst[bk]])
            kb.op("dve", lambda e: e.tensor_tensor(out=x1[:, ot, oc * 512:(oc + 1) * 512], in0=psb[bk][:, 0:512],
                                                   in1=x1[:, ot, oc * 512:(oc + 1) * 512], op=ALU.add), [pst[bk], x1_t[ot]], [x1_t[ot]])
    if "x1" in tap_d:
        tap("x1", x1[:].rearrange("p a q -> p (a q)"), x1_t)
    if stage <= 4:
        kb.finish()
        return nc
    kb.free(mixtok, mixtok_t)
    kb.free(mixT, mixT_t)
    for i in range(3):
        kb.free(ring[i], [ring_t[i]])
    h2T, h2T_t = kb.alloc_tok("h2T", [128, 16, 1024], BF16, ntok=8)
    ring = []
    ring_t = []
    for i in range(4):
        t, tk = kb.alloc_tok("ringc%d" % i, [128, 8192], BF16)
        ring.append(t)
        ring_t.append(tk)
    rp2[0] = 0
    moe_pref = []
    if stage >= 7:
        moe_pref.append(ring_load2(wg_d[0].rearrange("(kc p) n -> p kc n", p=128), 4))
        moe_pref.append(ring_load2(wu_d[0].rearrange("(kc p) n -> p kc n", p=128), 4))
        moe_pref.append(ring_load2(wd_d[0].rearrange("(fc p) n -> p fc n", p=128), 4))
    lnmoe_sb, lnmoe_t = kb.alloc_tok("lnmoe", [128, D], F32)
    kb.dma("sp", out=lnmoe_sb[:], in_=lnmoe_d, writes=[lnmoe_t])
    wr_sb, wr_t = kb.alloc_tok("wr", [128, 16, 36], BF16)
    kb.dma("pool", out=wr_sb[:], in_=wr_d.rearrange("(kc p) n -> p kc n", p=128), writes=[wr_t])
    br_sb, br_t = kb.alloc_tok("br", [128, 36], F32)
    kb.dma("sp", out=br_sb[:], in_=br_d, writes=[br_t])
    gmat, gmat_t = kb.alloc_tok("gmat", [128, 8, 32], F32)
    lgb, lgb_t = kb.alloc_tok("lgb", [128, 36], F32)
    lem, lem_t = kb.alloc_tok("lem", [128, 32], F32)
    rs, rs_t = kb.alloc_tok("rs", [128, 32], F32)
    g1b, g1b_t = kb.alloc_tok("g1b", [128, 32], F32)
    mr8, mr8_t = kb.alloc_tok("mr8", [128, 8], F32)
    hbC, hbC_t = kb.alloc_tok("hbC", [128, D], BF16)
    hb2 = [(hb, hb_t), (hbC, hbC_t)]
    sm4, sm4_t = kb.alloc_tok("sm4", [128, 8, 4], F32, ntok=8)

    lgbA, lgbA_t = kb.alloc_tok("lgbA", [128, 8, 36], F32, ntok=8)
    def router_front(ot):
        hbx, hbx_t = hb2[ot % 2]
        rms(x1[:, ot, :], [x1_t[ot]], D, lnmoe_sb[:], lnmoe_t, hbx[:], [hbx_t], sm4[:, ot, :], sm4_t[ot], 0)
        transpose16(hbx, hbx_t, lambda half: h2T[:, half * 8:(half + 1) * 8, ot * 128:(ot + 1) * 128], [h2T_t[ot]],
                    banks=(4 + 2 * (ot % 2), 5 + 2 * (ot % 2)))
        bk = nextps(0, 4)
        for kc in range(16):
            kb.op("pe", lambda e: e.matmul(out=psb[bk][:, 0:36], lhsT=h2T[:, kc, ot * 128:(ot + 1) * 128], rhs=wr_sb[:, kc, :],
                                           start=(kc == 0), stop=(kc == 15)), [h2T_t[ot], wr_t], [pst[bk]])
        kb.op("dve", lambda e: e.tensor_tensor(out=lgbA[:, ot, :], in0=psb[bk][:, 0:36], in1=br_sb[:], op=ALU.add),
              [pst[bk], br_t], [lgbA_t[ot]])
        return bk

    lemA, lemA_t = kb.alloc_tok("lemA", [128, 8, 32], F32, ntok=8)
    rsA, rsA_t = kb.alloc_tok("rsA", [128, 8, 32], F32, ntok=8)
    g1A, g1A_t = kb.alloc_tok("g1A", [128, 8, 32], F32, ntok=8)
    mrA, mrA_t = kb.alloc_tok("mrA", [128, 8, 8], F32, ntok=8)
    gmat_tt = [Tok() for _ in range(8)]
    rbanks = [router_front(ot) for ot in range(8)]
    T8 = range(8)
    for ot in T8:
        kb.op("dve", lambda e: e.tensor_reduce(out=rsA[:, ot, 0:1], in_=lgbA[:, ot, 0:4], axis=AX.X, op=ALU.max), [lgbA_t[ot]], [rsA_t[ot]])
    for ot in T8:
        kb.op("dve", lambda e: e.tensor_scalar(out=rsA[:, ot, 4:8], in0=lgbA[:, ot, 0:4], scalar1=rsA[:, ot, 0:1], scalar2=None,
                                               op0=ALU.is_ge), [lgbA_t[ot], rsA_t[ot]], [rsA_t[ot]])
    for ot in T8:
        kb.op("dve", lambda e: e.tensor_scalar(out=rsA[:, ot, 1:2], in0=rsA[:, ot, 0:1], scalar1=-1.0, scalar2=None, op0=ALU.mult),
              [rsA_t[ot]], [rsA_t[ot]])
    for ot in T8:
        kb.op("act", lambda e: e.activation(out=rsA[:, ot, 8:12], in_=lgbA[:, ot, 0:4], func=AF.Exp, bias=rsA[:, ot, 1:2], scale=1.0,
                                            accum_out=rsA[:, ot, 2:3]), [lgbA_t[ot], rsA_t[ot]], [rsA_t[ot]], guard=True)
    for ot in T8:
        kb.op("dve", lambda e: e.reciprocal(out=rsA[:, ot, 3:4], in_=rsA[:, ot, 2:3]), [rsA_t[ot]], [rsA_t[ot]])
    for ot in T8:
        kb.op("dve", lambda e: e.tensor_scalar(out=rsA[:, ot, 12:16], in0=rsA[:, ot, 4:8], scalar1=-1.0, scalar2=BIG, op0=ALU.add,
                                               op1=ALU.mult), [rsA_t[ot]], [rsA_t[ot]])
    for g in range(4):
        for ot in T8:
            kb.op("dve", lambda e: e.tensor_scalar(out=lemA[:, ot, g * 8:(g + 1) * 8], in0=lgbA[:, ot, 4 + 8 * g:12 + 8 * g],
                                                   scalar1=rsA[:, ot, 12 + g:13 + g], scalar2=None, op0=ALU.add),
                  [lgbA_t[ot], rsA_t[ot]], [lemA_t[ot]])
    for ot in T8:
        kb.op("dve", lambda e: e.max(out=mrA[:, ot, :], in_=lemA[:, ot, :]), [lemA_t[ot]], [mrA_t[ot]])
    for ot in T8:
        kb.op("dve", lambda e: e.tensor_tensor(out=rsA[:, ot, 16:17], in0=mrA[:, ot, 1:2], in1=mrA[:, ot, 0:1], op=ALU.subtract),
              [mrA_t[ot]], [rsA_t[ot]])
    for ot in T8:
        kb.op("act", lambda e: e.activation(out=rsA[:, ot, 17:18], in_=rsA[:, ot, 16:17], func=AF.Exp), [rsA_t[ot]], [rsA_t[ot]])
    for ot in T8:
        kb.op("dve", lambda e: e.tensor_scalar(out=rsA[:, ot, 18:19], in0=rsA[:, ot, 17:18], scalar1=1.0, scalar2=None, op0=ALU.add),
              [rsA_t[ot]], [rsA_t[ot]])
    for ot in T8:
        kb.op("dve", lambda e: e.reciprocal(out=rsA[:, ot, 19:20], in_=rsA[:, ot, 18:19]), [rsA_t[ot]], [rsA_t[ot]])
    for ot in T8:
        kb.op("dve", lambda e: e.tensor_tensor(out=rsA[:, ot, 20:21], in0=rsA[:, ot, 19:20], in1=rsA[:, ot, 3:4], op=ALU.mult),
              [rsA_t[ot]], [rsA_t[ot]])
    for ot in T8:
        kb.op("dve", lambda e: e.tensor_tensor(out=rsA[:, ot, 21:22], in0=rsA[:, ot, 3:4], in1=rsA[:, ot, 20:21], op=ALU.subtract),
              [rsA_t[ot]], [rsA_t[ot]])
    for ot in T8:
        kb.op("dve", lambda e: e.tensor_scalar(out=g1A[:, ot, :], in0=lemA[:, ot, :], scalar1=mrA[:, ot, 0:1], scalar2=rsA[:, ot, 20:21],
                                               op0=ALU.is_equal, op1=ALU.mult), [lemA_t[ot], mrA_t[ot], rsA_t[ot]], [g1A_t[ot]])
    for ot in T8:
        kb.op("dve", lambda e: e.tensor_scalar(out=gmat[:, ot, :], in0=lemA[:, ot, :], scalar1=mrA[:, ot, 1:2], scalar2=rsA[:, ot, 21:22],
                                               op0=ALU.is_equal, op1=ALU.mult), [lemA_t[ot], mrA_t[ot], rsA_t[ot]], [gmat_tt[ot]])
    for ot in T8:
        kb.op("dve", lambda e: e.tensor_tensor(out=gmat[:, ot, :], in0=gmat[:, ot, :], in1=g1A[:, ot, :], op=ALU.add),
              [gmat_tt[ot], g1A_t[ot]], [gmat_tt[ot]])
    gmat_t.w = gmat_tt[7].w
    if "gmat" in tap_d:
        tap("gmat", gmat[:].rearrange("p a q -> p (a q)"), [gmat_t])
    if stage <= 5:
        kb.finish()
        return nc
    kb.free(lnmoe_sb, [lnmoe_t])
    kb.free(hbC, [hbC_t])
    kb.free(sm4, sm4_t)
    kb.free(lgbA, lgbA_t)
    kb.free(lemA, lemA_t)
    kb.free(rsA, rsA_t)
    kb.free(g1A, g1A_t)
    kb.free(mrA, mrA_t)
    AT, AT_t = kb.alloc_tok("AT", [128, 4, 1024], BF16, ntok=8)
    sgt = []
    sgt_t = []
    for i in range(2):
        t, tk = kb.alloc_tok("sg%d" % i, [128, 512], F32)
        sgt.append(t)
        sgt_t.append(tk)
    sgp = 0
    nexp = NEXP if stage >= 7 else 0
    for ex in range(nexp):
        if ex == 0:
            (wgv, wg_t), (wuv, wu_t), (wdv, wd_t) = moe_pref
        else:
            wgv, wg_t = ring_load2(wg_d[ex].rearrange("(kc p) n -> p kc n", p=128), 4)
            wuv, wu_t = ring_load2(wu_d[ex].rearrange("(kc p) n -> p kc n", p=128), 4)
            wdv, wd_t = ring_load2(wd_d[ex].rearrange("(fc p) n -> p fc n", p=128), 4)
        for fc in range(4):
            for tq in range(2):
                bg = nextps(0, 4)
                bu = nextps(0, 4)
                for (bk_, wv_, wt_) in ((bg, wgv, wg_t), (bu, wuv, wu_t)):
                    for kc in range(16):
                        kb.op("pe", lambda e: e.matmul(out=psb[bk_][:, 0:512], lhsT=wv_[:, kc, fc * 128:(fc + 1) * 128],
                                                       rhs=h2T[:, kc, tq * 512:(tq + 1) * 512], start=(kc == 0), stop=(kc == 15)),
                              h2T_t[4 * tq:4 * tq + 4] + [wt_], [pst[bk_]])
                sg, sg_t = sgt[sgp], sgt_t[sgp]
                sgp ^= 1
                kb.op("act", lambda e: e.activation(out=sg[:], in_=psb[bg][:, 0:512], func=AF.Silu), [pst[bg]], [sg_t])
                kb.op("dve", lambda e: e.tensor_tensor(out=AT[:, fc, tq * 512:(tq + 1) * 512], in0=sg[:], in1=psb[bu][:, 0:512],
                                                       op=ALU.mult), [sg_t, pst[bu]], [AT_t[fc * 2 + tq]])
        for ot in range(8):
            for dc in range(4):
                by = nextps(4, 8)
                for fc in range(4):
                    kb.op("pe", lambda e: e.matmul(out=psb[by][:, 0:512], lhsT=AT[:, fc, ot * 128:(ot + 1) * 128],
                                                   rhs=wdv[:, fc, dc * 512:(dc + 1) * 512], start=(fc == 0), stop=(fc == 3)),
                          [AT_t[fc * 2 + ot // 4], wd_t], [pst[by]])
                kb.op("dve", lambda e: e.scalar_tensor_tensor(out=x1[:, ot, dc * 512:(dc + 1) * 512], in0=psb[by][:, 0:512],
                                                              scalar=gmat[:, ot, ex:ex + 1], in1=x1[:, ot, dc * 512:(dc + 1) * 512],
                                                              op0=ALU.mult, op1=ALU.add), [pst[by], gmat_t, x1_t[ot]], [x1_t[ot]])
    for i in range(4):
        kb.free(ring[i], [ring_t[i]])
    kb.free(AT, AT_t)
    kb.free(h2T, h2T_t)
    lnfin_sb, lnfin_t = kb.alloc_tok("lnfin", [128, D], F32)
    kb.dma("sp", out=lnfin_sb[:], in_=lnfin_d, writes=[lnfin_t])
    of = []
    of_t = []
    for i in range(2):
        t, tk = kb.alloc_tok("of%d" % i, [128, D], F32)
        of.append(t)
        of_t.append(tk)
    for ot in range(8):
        rms(x1[:, ot, :], [x1_t[ot]], D, lnfin_sb[:], lnfin_t, of[ot % 2][:], [of_t[ot % 2]], sm2, sm2_t, 24)
        kb.dma("sp", out=out_d[ot * 128:(ot + 1) * 128, :], in_=of[ot % 2][:], reads=[of_t[ot % 2]])
    kb.finish()
    return nc


def _bf(a):
    return np.ascontiguousarray(a.astype(ml_dtypes.bfloat16))


def _rep(v, n=128):
    return np.ascontiguousarray(np.broadcast_to(np.asarray(v, np.float32).reshape(1, -1), (n, v.size)))


def const_tables(h):
    off = -1024 + 1024 * h
    t = {}
    c = np.arange(2048)
    t["valid"] = np.ascontiguousarray(((c + off) >= 0).astype(np.float32).reshape(16, 128).T)
    t["identb"] = _bf(np.eye(128, dtype=np.float32))
    t["identf"] = np.eye(128, dtype=np.float32)
    inv = (500000.0 ** (-np.arange(0, 32, 2, dtype=np.float32) / np.float32(32))).astype(np.float32)
    t["invf"] = _rep(inv)
    j = np.arange(128)[:, None]
    cq = 1024 + np.arange(1024)[None, :]
    ok = (16 * j + off >= 0) & (16 * j + 31 <= cq) & (j < 127)
    t["cmpbias"] = _bf(np.where(ok, 0.0, NEGB).astype(np.float32))
    cs = np.arange(128)[:, None] * 16
    ss = np.arange(32)[None, :] * 64
    ov = np.clip(np.minimum(cs + 32, ss + 64) - np.maximum(cs, ss), 0, None)
    t["wcs"] = _bf((ov / 32.0).astype(np.float32))
    key = np.arange(2048)[None, :]
    t["emat"] = _bf((key // 64 == np.arange(128)[:, None]).astype(np.float32))
    k = np.arange(128)[:, None, None]
    q = np.arange(512)[None, None, :]
    i4 = np.arange(4)[None, :, None]
    t["causb"] = _bf(np.where(128 * i4 + k <= q, 0.0, NEGB).astype(np.float32))
    i8 = np.arange(8)[None, :, None]
    kk = 128 * i8 + k
    t["winb"] = _bf(np.where((kk > q) & (kk <= q + 512), 0.0, NEGB).astype(np.float32))
    tq = (1024 + np.arange(1024) + off)[:, None]
    jg = np.arange(32)[None, :] + off // 64
    okb = (jg >= 0) & (jg * 64 <= tq)
    cur = tq // 64
    forced = (jg == 0) | ((cur - jg >= 0) & (cur - jg < 2))
    amul = (okb & ~forced).astype(np.float32)
    aadd = np.where(okb & forced, BIG, np.where(okb, 0.0, -BIG)).astype(np.float32)
    t["amul"] = np.ascontiguousarray(amul.reshape(8, 128, 32).transpose(1, 0, 2))
    t["aadd"] = np.ascontiguousarray(aadd.reshape(8, 128, 32).transpose(1, 0, 2))
    tg = 1024 + np.arange(16) + off
    ic = np.stack([1.0 / np.minimum(tg + 1, w) for w in POOL_SIZES], 0).astype(np.float32)
    t["invcnt"] = np.ascontiguousarray(np.broadcast_to(ic[None], (128, 4, 16)))
    return t


def prep_inputs(inp, stage=99):
    L = 0
    shared = {}
    shared["w_in"] = np.ascontiguousarray(inp["w_in"][L])
    shared["lnmix"] = _rep(inp["ln_mix"][L])
    shared["pekT"] = np.ascontiguousarray(inp["pe_cmp_k"][L].T)
    shared["pevT"] = np.ascontiguousarray(inp["pe_cmp_v"][L].T)
    shared["w1k"] = np.ascontiguousarray(inp["w_cmp_k1"][L])
    shared["w1v"] = np.ascontiguousarray(inp["w_cmp_v1"][L])
    shared["w2k"] = np.ascontiguousarray(inp["w_cmp_k2"][L])
    shared["w2v"] = np.ascontiguousarray(inp["w_cmp_v2"][L])
    shared["wpool"] = np.ascontiguousarray(inp["w_pool"][L])
    shared["bpool"] = _rep(inp["b_pool"][L])
    shared["pscale"] = _rep(inp["pool_scale"][L])
    shared["gnnsa"] = _rep(inp["gn_nsa"][L])
    shared["gnpool"] = _rep(inp["gn_pool"][L])
    shared["wout"] = np.ascontiguousarray(inp["w_out"][L])
    shared["lnmoe"] = _rep(inp["ln_moe"][L])
    wr = np.concatenate([inp["w_router_group"][L]] + [inp["w_router_expert"][L][g] for g in range(4)], axis=1)
    shared["wr"] = np.ascontiguousarray(wr.astype(np.float32))
    br = np.concatenate([inp["b_router_group"][L].reshape(-1), inp["b_router_expert"][L].reshape(-1)])
    shared["br"] = _rep(br)
    if stage >= 7:
        shared["wg"] = np.ascontiguousarray(inp["w_gate"][L])
        shared["wu"] = np.ascontiguousarray(inp["w_up"][L])
        shared["wd"] = np.ascontiguousarray(inp["w_down"][L])
    shared["lnfin"] = _rep(inp["ln_final"])
    tabs = [const_tables(0), const_tables(1)]
    x = np.asarray(inp["x"], np.float32)
    pos = np.asarray(inp["positions"], np.int32)
    maps = []
    for c in range(8):
        b, h = c // 2, c % 2
        m = dict(shared)
        m.update(tabs[h])
        if h == 0:
            xcx = np.concatenate([np.zeros((1024, D), np.float32), x[b, 0:1024]], 0)
            pc = np.concatenate([np.zeros((1024,), np.int32), pos[b, 0:1024]], 0)
        else:
            xcx = x[b]
            pc = pos[b]
        m["xc"] = np.ascontiguousarray(xcx)
        m["posT"] = np.ascontiguousarray(pc.reshape(16, 128).T.astype(np.int32))
        maps.append(m)
    return maps


_NC_CACHE = {}


def kernel(**inputs):
    inp = {k: np.asarray(v) for k, v in inputs.items()}
    if "nc" not in _NC_CACHE:
        import os
        _NC_CACHE["nc"] = build(99, poison=bool(os.environ.get("KPOISON")))
    nc = _NC_CACHE["nc"]
    maps = prep_inputs(inp, 99)
    res = run_bass_kernel_spmd(nc, maps, core_ids=list(range(8)))
    out = np.zeros((NB, S, D), np.float32)
    for c in range(8):
        b, h = c // 2, c % 2
        out[b, h * 1024:(h + 1) * 1024] = res.results[c]["out"]
    return out
```
